# Optimizing a Trainium2 kernel written in Bass

```python
import math
import jax, jax.numpy as jnp
from jax import lax
import numpy as np

D_MODEL = 1024
BATCH = 4
SEQ = 4096
DEPTH = 1

EPS = 1e-6
PLE_DIM = 256
GRID_W = 64
D_MIX = D_MODEL
N_HEADS = 8
N_KV_HEADS = 2
HEAD_DIM = 64
D_ATTN = N_HEADS * HEAD_DIM
D_KV = N_KV_HEADS * HEAD_DIM
Q_BLOCK = 128
ROPE_THETA = 10000.0
D_HYENA = D_MIX - D_ATTN
HYENA_HEAD = 64
HYENA_ORDER = 2
SHORT_CONV = 3
FILTER_EMB = 33
FILTER_HIDDEN = 64
FAST_DECAY_PCT = 0.3
SLOW_DECAY_PCT = 1.5
DECAY_TARGET = 1e-2
D_IN = D_ATTN + 2 * D_KV + (HYENA_ORDER + 1) * D_HYENA
N_GROUPS = 4
EXPERTS_PER_GROUP = 8
N_EXPERTS = N_GROUPS * EXPERTS_PER_GROUP
TOP_K = 2
D_EXPERT = 512
MOE_BLOCK = 128

kernel_name = "hymba_attn_hyena_hier_moe_block"


def rms_norm(x, gain):
    xf = x.astype(jnp.float32)
    y = xf * lax.rsqrt(jnp.mean(xf * xf, axis=-1, keepdims=True) + EPS)
    return (y * gain.astype(jnp.float32)).astype(x.dtype)


def group_rms(y, gain, width):
    B, S, W = y.shape
    yg = y.reshape(B, S, W // width, width)
    return rms_norm(yg, gain.reshape(W // width, width)).reshape(B, S, W)


def rope_tables_2d(S):
    rows = S // GRID_W
    r_idx, c_idx = jnp.meshgrid(jnp.arange(rows, dtype=jnp.float32),
                                jnp.arange(GRID_W, dtype=jnp.float32), indexing="ij")
    r_idx, c_idx = r_idx.reshape(S), c_idx.reshape(S)
    half = HEAD_DIM // 2
    inv = ROPE_THETA ** (-jnp.arange(0, half, 2, dtype=jnp.float32) / half)
    ang_r = r_idx[:, None] * inv[None]
    ang_c = c_idx[:, None] * inv[None]
    return jnp.cos(ang_r), jnp.sin(ang_r), jnp.cos(ang_c), jnp.sin(ang_c)


def apply_rope_2d(x, cos_r, sin_r, cos_c, sin_c):
    half = HEAD_DIM // 2
    xf = x.astype(jnp.float32)

    def rot(a, c, s):
        n = a.shape[-1] // 2
        a1, a2 = a[..., :n], a[..., n:]
        c = c[None, :, None, :]
        s = s[None, :, None, :]
        return jnp.concatenate([a1 * c - a2 * s, a2 * c + a1 * s], axis=-1)

    out = jnp.concatenate([rot(xf[..., :half], cos_r, sin_r),
                           rot(xf[..., half:], cos_c, sin_c)], axis=-1)
    return out.astype(x.dtype)


def attention_group(q, k, v, q_gain, k_gain):
    B, S, _ = q.shape
    G = N_HEADS // N_KV_HEADS
    q = rms_norm(q.reshape(B, S, N_HEADS, HEAD_DIM), q_gain)
    k = rms_norm(k.reshape(B, S, N_KV_HEADS, HEAD_DIM), k_gain)
    v = v.reshape(B, S, N_KV_HEADS, HEAD_DIM)
    cos_r, sin_r, cos_c, sin_c = rope_tables_2d(S)
    q = apply_rope_2d(q, cos_r, sin_r, cos_c, sin_c)
    k = apply_rope_2d(k, cos_r, sin_r, cos_c, sin_c)
    n_blk = S // Q_BLOCK
    qb = q.reshape(B, n_blk, Q_BLOCK, N_KV_HEADS, G, HEAD_DIM).transpose(1, 0, 3, 4, 2, 5)
    kt = k.transpose(0, 2, 1, 3)
    vt = v.transpose(0, 2, 1, 3)
    scale = HEAD_DIM ** -0.5

    def one_block(qi):
        s = jnp.einsum("bkgqd,bksd->bkgqs", qi, kt,
                       preferred_element_type=jnp.float32) * scale
        pr = jax.nn.softmax(s, axis=-1)
        return jnp.einsum("bkgqs,bksd->bkgqd", pr.astype(vt.dtype), vt)

    o = lax.map(one_block, qb)
    return o.transpose(1, 0, 4, 2, 3, 5).reshape(B, S, D_ATTN)


def filter_position_features(L):
    bands = (FILTER_EMB - 1) // 2
    t = jnp.linspace(0.0, 1.0, L, dtype=jnp.float32)[:, None]
    w = (2.0 * math.pi / L) * jnp.arange(L, dtype=jnp.float32)[:, None]
    f = jnp.linspace(1e-4, bands - 1, bands, dtype=jnp.float32)[None]
    z = f * w
    return jnp.concatenate([t, jnp.cos(z), -jnp.sin(z)], axis=-1)


def implicit_filters_freq(L, w_f1, b_f1, freq1, w_f2, b_f2, freq2, w_f3):
    z = filter_position_features(L)
    h = jnp.sin(freq1 * (z @ w_f1 + b_f1))
    h = jnp.sin(freq2 * (h @ w_f2 + b_f2))
    h = (h @ w_f3).astype(jnp.float32).reshape(L, HYENA_ORDER, 2, D_HYENA)
    max_decay = math.log(DECAY_TARGET) / FAST_DECAY_PCT
    min_decay = math.log(DECAY_TARGET) / SLOW_DECAY_PCT
    deltas = jnp.linspace(min_decay, max_decay, D_HYENA, dtype=jnp.float32)
    t = jnp.linspace(0.0, 1.0, L, dtype=jnp.float32)[:, None]
    decay = jnp.exp(-t * jnp.abs(deltas)[None])
    h = h * decay[:, None, None, :]
    h = h / (jnp.sum(jnp.abs(h), axis=0, keepdims=True) + EPS)
    hf, hb = h[:, :, 0], h[:, :, 1]
    circ = jnp.concatenate([hf[:1] + hb[:1], hf[1:], jnp.zeros_like(hf[:1]), hb[:0:-1]], axis=0)
    return jnp.fft.rfft(circ, n=2 * L, axis=0)


def long_conv(z, H, bias):
    L = z.shape[1]
    zf = z.astype(jnp.float32)
    Z = jnp.fft.rfft(zf, n=2 * L, axis=1)
    y = jnp.fft.irfft(Z * H[None], n=2 * L, axis=1)[:, :L]
    return (y + zf * bias.astype(jnp.float32)).astype(z.dtype)


def hyena_group(u, conv_w, conv_b, w_f1, b_f1, freq1, w_f2, b_f2, freq2, w_f3, filt_bias):
    L = u.shape[1]
    pad = SHORT_CONV // 2
    up = jnp.pad(u, ((0, 0), (pad, pad), (0, 0)))
    uc = conv_b + sum(up[:, j:j + L] * conv_w[j] for j in range(SHORT_CONV))
    v, x1, x2 = jnp.split(uc, HYENA_ORDER + 1, axis=-1)
    Hf = implicit_filters_freq(L, w_f1, b_f1, freq1, w_f2, b_f2, freq2, w_f3)
    z = v
    for o, gate in enumerate((x1, x2)):
        z = gate * long_conv(z, Hf[:, o], filt_bias[o])
    return z


def hierarchical_moe(h, w_group, b_group, w_router, b_router, w_gate, w_up, w_down):
    B, S, D = h.shape
    N = B * S
    hf = h.reshape(N, D)
    g_logits = (hf @ w_group).astype(jnp.float32) + b_group.astype(jnp.float32)
    g_prob = jax.nn.softmax(g_logits, axis=-1)
    g_top_p, g_sel = lax.top_k(g_prob, 1)
    e_logits = ((hf @ w_router).astype(jnp.float32) + b_router.astype(jnp.float32)
                ).reshape(N, N_GROUPS, EXPERTS_PER_GROUP)
    e_logits = jnp.take_along_axis(e_logits, g_sel[:, :, None], axis=1)[:, 0]
    e_prob = jax.nn.softmax(e_logits, axis=-1)
    top_p, top_i = lax.top_k(e_prob, TOP_K)
    top_p = top_p / jnp.sum(top_p, axis=-1, keepdims=True)
    weights = g_top_p * top_p
    expert = g_sel * EXPERTS_PER_GROUP + top_i

    T = MOE_BLOCK
    NK = N * TOP_K
    e_flat = expert.reshape(NK).astype(jnp.int32)
    w_flat = weights.reshape(NK)
    order = jnp.argsort(e_flat)
    e_sorted = e_flat[order]
    tok_sorted = (order // TOP_K).astype(jnp.int32)
    counts = jnp.bincount(e_flat, length=N_EXPERTS)
    starts = jnp.cumsum(counts) - counts
    padded = (counts + T - 1) // T * T
    pends = jnp.cumsum(padded)
    pstarts = pends - padded
    dest = pstarts[e_sorted] + jnp.arange(NK, dtype=jnp.int32) - starts[e_sorted]
    n_rows = -(-(NK + N_EXPERTS * (T - 1)) // T) * T
    n_blocks = n_rows // T
    row_tok = jnp.full((n_rows,), N, jnp.int32).at[dest].set(tok_sorted)
    row_w = jnp.zeros((n_rows,), jnp.float32).at[dest].set(w_flat[order])
    block_e = jnp.clip(jnp.searchsorted(pends, jnp.arange(n_blocks) * T, side="right"),
                       0, N_EXPERTS - 1)
    x_pad = jnp.concatenate([hf, jnp.zeros((1, D), hf.dtype)], axis=0)
    xb = x_pad[row_tok].reshape(n_blocks, T, D)

    def expert_block(args):
        xblk, e = args
        a = xblk @ w_gate[e]
        b = xblk @ w_up[e]
        return (jax.nn.silu(a) * b) @ w_down[e]

    yb = lax.map(expert_block, (xb, block_e)).reshape(n_rows, D)
    y = jax.ops.segment_sum(yb * row_w[:, None].astype(yb.dtype), row_tok, num_segments=N + 1)[:N]
    return y.reshape(B, S, D).astype(h.dtype)


def setup_inputs(seed: int = 0) -> dict:
    key = jax.random.key(seed)
    ks = iter(jax.random.split(key, 40))

    def nrm(shape, scale):
        return jax.random.normal(next(ks), shape, jnp.float32) * scale

    def gain(shape):
        return 1.0 + nrm(shape, 0.02)

    L = DEPTH
    return {
        "x": nrm((BATCH, SEQ, D_MODEL), 1.0),
        "p": nrm((DEPTH, BATCH, SEQ, PLE_DIM), 1.0),
        "g_mix": gain((L, D_MODEL)),
        "w_in": nrm((L, D_MODEL, D_IN), D_MODEL ** -0.5),
        "q_gain": gain((L, HEAD_DIM)),
        "k_gain": gain((L, HEAD_DIM)),
        "conv_w": nrm((L, SHORT_CONV, (HYENA_ORDER + 1) * D_HYENA), SHORT_CONV ** -0.5),
        "conv_b": nrm((L, (HYENA_ORDER + 1) * D_HYENA), 0.01),
        "w_f1": nrm((L, FILTER_EMB, FILTER_HIDDEN), FILTER_EMB ** -0.5),
        "b_f1": nrm((L, FILTER_HIDDEN), 0.1),
        "freq1": gain((L, FILTER_HIDDEN)),
        "w_f2": nrm((L, FILTER_HIDDEN, FILTER_HIDDEN), FILTER_HIDDEN ** -0.5),
        "b_f2": nrm((L, FILTER_HIDDEN), 0.1),
        "freq2": gain((L, FILTER_HIDDEN)),
        "w_f3": nrm((L, FILTER_HIDDEN, HYENA_ORDER * 2 * D_HYENA), FILTER_HIDDEN ** -0.5),
        "filt_bias": nrm((L, HYENA_ORDER, D_HYENA), 1.0),
        "g_attn_out": gain((L, D_ATTN)),
        "g_hyena_out": gain((L, D_HYENA)),
        "w_out": nrm((L, D_MIX, D_MODEL), D_MIX ** -0.5),
        "g_moe": gain((L, D_MODEL)),
        "w_group": nrm((L, D_MODEL, N_GROUPS), D_MODEL ** -0.5),
        "b_group": nrm((L, N_GROUPS), 0.01),
        "w_router": nrm((L, D_MODEL, N_EXPERTS), D_MODEL ** -0.5),
        "b_router": nrm((L, N_EXPERTS), 0.01),
        "w_gate": nrm((L, N_EXPERTS, D_MODEL, D_EXPERT), D_MODEL ** -0.5),
        "w_up": nrm((L, N_EXPERTS, D_MODEL, D_EXPERT), D_MODEL ** -0.5),
        "w_down": nrm((L, N_EXPERTS, D_EXPERT, D_MODEL), D_EXPERT ** -0.5),
        "g_ple": gain((L, D_MODEL)),
        "w_ple_gate": nrm((L, D_MODEL, D_MODEL), D_MODEL ** -0.5),
        "b_ple_gate": nrm((L, D_MODEL), 0.01),
        "w_ple": nrm((L, PLE_DIM, D_MODEL), PLE_DIM ** -0.5),
        "g_final": gain((D_MODEL,)),
    }


def reference(x, p, g_mix, w_in, q_gain, k_gain, conv_w, conv_b, w_f1, b_f1, freq1,
              w_f2, b_f2, freq2, w_f3, filt_bias, g_attn_out, g_hyena_out, w_out,
              g_moe, w_group, b_group, w_router, b_router, w_gate, w_up, w_down,
              g_ple, w_ple_gate, b_ple_gate, w_ple, g_final):
    for i in range(DEPTH):
        h = rms_norm(x, g_mix[i])
        proj = h @ w_in[i]
        q = proj[..., :D_ATTN]
        k = proj[..., D_ATTN:D_ATTN + D_KV]
        v = proj[..., D_ATTN + D_KV:D_ATTN + 2 * D_KV]
        u = proj[..., D_ATTN + 2 * D_KV:]
        ya = attention_group(q, k, v, q_gain[i], k_gain[i])
        yh = hyena_group(u, conv_w[i], conv_b[i], w_f1[i], b_f1[i], freq1[i],
                         w_f2[i], b_f2[i], freq2[i], w_f3[i], filt_bias[i])
        ya = group_rms(ya, g_attn_out[i], HEAD_DIM)
        yh = group_rms(yh, g_hyena_out[i], HYENA_HEAD)
        x = x + jnp.concatenate([ya, yh], axis=-1) @ w_out[i]
        x = x + hierarchical_moe(rms_norm(x, g_moe[i]), w_group[i], b_group[i],
                                 w_router[i], b_router[i], w_gate[i], w_up[i], w_down[i])
        gate = jax.nn.sigmoid(rms_norm(x, g_ple[i]) @ w_ple_gate[i] + b_ple_gate[i])
        x = x + (p[i] @ w_ple[i]) * gate
    return rms_norm(x, g_final)
```

```python
import math
import numpy as np
import ml_dtypes
from contextlib import ExitStack
import concourse.bass as bass
import concourse.mybir as mybir
from concourse.bass_utils import run_bass_kernel_spmd

F32 = mybir.dt.float32
BF16 = mybir.dt.bfloat16
I32 = mybir.dt.int32
ACT = mybir.ActivationFunctionType
ALU = mybir.AluOpType
AX = mybir.AxisListType

ENGS = ("pe", "act", "dve", "pool", "sp")
L = 4096
D = 1024
NT = 32
NO = 16
LO = 2048
EPS = 1e-6
CC = 32
GK = 512 // (2 * CC)
SB = 512 // CC
NCH = 512 // CC
K1 = 33
STAGE = 99
SAME_ENG_FIFO = False


class Sched:
    SEM_LIMIT = 20000
    NDMA = 24

    def __init__(self, nc, stack):
        self.nc = nc
        self.stack = stack
        self.q = {e: [] for e in ENGS}
        self.cur_sem = {}
        self.cur_cnt = {}
        self.nsem = 0
        for e in ENGS:
            self._new_eng_sem(e)
        self.dma_sems = [self._sem("dma%d" % i) for i in range(self.NDMA)]
        self.dma_tgt = [0] * self.NDMA
        self.dma_k = 0
        self.last_w = {}
        self.readers = {}
        self.seen = {e: {} for e in ENGS}

    def _sem(self, name):
        self.nsem += 1
        return self.stack.enter_context(self.nc.semaphore(name))

    def _new_eng_sem(self, e):
        self.cur_sem[e] = self._sem("c_%s_%d" % (e, self.nsem))
        self.cur_cnt[e] = 0

    def _waits_for(self, eng, toks):
        out = []
        seen = self.seen[eng]
        for t in toks:
            if t is None:
                continue
            sem, val, teng = t
            if teng == eng and (eng == "pe" or SAME_ENG_FIFO):
                continue
            k = id(sem)
            if seen.get(k, 0) >= val:
                continue
            seen[k] = val
            out.append((sem, val))
        return out

    @staticmethod
    def _excl(reads, writes):
        r2 = [b for b in reads if not b.startswith("ps")]
        w2 = list(writes) + [b for b in reads if b.startswith("ps")]
        return r2, w2

    def _deps(self, reads, writes):
        toks = []
        for b in reads:
            toks.append(self.last_w.get(b))
        for b in writes:
            toks.append(self.last_w.get(b))
            toks.extend(self.readers.get(b, ()))
        return toks

    def _commit(self, tok, reads, writes):
        for b in reads:
            self.readers.setdefault(b, []).append(tok)
        for b in writes:
            self.last_w[b] = tok
            self.readers[b] = []

    def op(self, eng, fn, reads=(), writes=()):
        reads, writes = self._excl(reads, writes)
        toks = self._deps(reads, writes)
        waits = self._waits_for(eng, toks)
        if self.cur_cnt[eng] >= self.SEM_LIMIT:
            self._new_eng_sem(eng)
        self.cur_cnt[eng] += 1
        sem = self.cur_sem[eng]
        tok = (sem, self.cur_cnt[eng], eng)
        self.q[eng].append((waits, fn, sem, 1))
        self._commit(tok, reads, writes)
        return tok

    def dma(self, eng, fn, reads=(), writes=()):
        reads, writes = self._excl(reads, writes)
        toks = self._deps(reads, writes)
        i = self.dma_k % self.NDMA
        self.dma_k += 1
        sem = self.dma_sems[i]
        if self.dma_tgt[i] > 0:
            toks.append((sem, self.dma_tgt[i], "dma"))
        waits = self._waits_for(eng, toks)
        self.dma_tgt[i] += 16
        tok = (sem, self.dma_tgt[i], "dma")
        self.q[eng].append((waits, fn, sem, 16))
        self._commit(tok, reads, writes)
        return tok

    def barrier(self):
        toks = [(self.cur_sem[e], self.cur_cnt[e], e) for e in ENGS if self.cur_cnt[e] > 0]
        toks += [(self.dma_sems[i], self.dma_tgt[i], "dma") for i in range(self.NDMA) if self.dma_tgt[i] > 0]
        for e in ENGS:
            tk = toks
            waits = self._waits_for(e, tk)
            self.q[e].append((waits, None, None, 0))
        self.last_w = {}
        self.readers = {}

    def final_wait(self, eng, bufs):
        toks = [self.last_w.get(b) for b in bufs]
        waits = self._waits_for(eng, toks)
        self.q[eng].append((waits, None, None, 0))

    def emit(self):
        nc = self.nc
        q = self.q

        def run(engobj, lst):
            for waits, fn, sem, inc in lst:
                for (s, v) in waits:
                    engobj.wait_ge(s, v)
                if fn is not None:
                    meth, kw = fn
                    ins = getattr(engobj, meth)(**kw)
                    ins.then_inc(sem, inc)

        with nc.Block() as block:
            @block.tensor
            def _(e):
                run(e, q["pe"])

            @block.scalar
            def _(e):
                run(e, q["act"])

            @block.vector
            def _(e):
                run(e, q["dve"])

            @block.gpsimd
            def _(e):
                run(e, q["pool"])

            @block.sync
            def _(e):
                run(e, q["sp"])


def _bf(a):
    return np.ascontiguousarray(a.astype(np.float32)).astype(ml_dtypes.bfloat16)


_CONST = None


def host_consts():
    global _CONST
    if _CONST is not None:
        return _CONST
    c = {}
    c["ident"] = np.eye(128, dtype=np.float32)
    bo = np.zeros((128, 128), np.float32)
    bo[:64, :64] = 1.0
    bo[64:, 64:] = 1.0
    c["bones"] = bo
    c["ones32"] = np.ones((32, 32), np.float32)
    S = L
    rows = S // 64
    r_idx, c_idx = np.meshgrid(np.arange(rows, dtype=np.float32), np.arange(64, dtype=np.float32), indexing="ij")
    r_idx, c_idx = r_idx.reshape(S), c_idx.reshape(S)
    half = 32
    inv = (10000.0 ** (-np.arange(0, half, 2, dtype=np.float32) / half)).astype(np.float32)
    ang_r = (r_idx[:, None] * inv[None]).astype(np.float32)
    ang_c = (c_idx[:, None] * inv[None]).astype(np.float32)
    cr, sr, cc_, sc = np.cos(ang_r), np.sin(ang_r), np.cos(ang_c), np.sin(ang_c)
    c["ropeC"] = np.concatenate([cr, cr, cc_, cc_], axis=1).astype(np.float32)
    c["ropeS"] = np.concatenate([-sr, sr, -sc, sc], axis=1).astype(np.float32)
    bands = 16
    t = np.linspace(0.0, 1.0, L, dtype=np.float32)[:, None]
    w = ((2.0 * math.pi / L) * np.arange(L, dtype=np.float32))[:, None]
    f = np.linspace(1e-4, bands - 1, bands, dtype=np.float32)[None]
    z = (f * w).astype(np.float32)
    zz = np.concatenate([t, np.cos(z), -np.sin(z)], axis=-1).astype(np.float32)
    c["zT"] = np.ascontiguousarray(zz.T)
    max_decay = math.log(1e-2) / 0.3
    min_decay = math.log(1e-2) / 1.5
    deltas = np.linspace(min_decay, max_decay, 512, dtype=np.float32)
    dec = np.exp(-t * np.abs(deltas)[None]).astype(np.float32)
    c["dec"] = _bf(dec.reshape(32, 128, 512).transpose(0, 2, 1))
    a = np.arange(32)[:, None].astype(np.float64)
    k1 = np.arange(K1)[None, :].astype(np.float64)
    th = 2 * np.pi * a * k1 / 64.0
    c["F64"] = _bf(np.concatenate([np.cos(th), -np.sin(th)], axis=1))
    b = np.arange(128).astype(np.float64)
    kk = (np.arange(K1)[:, None] + 64 * np.arange(128)[None, :]).astype(np.float64)
    th = 2 * np.pi * b[:, None, None] * kk[None] / 8192.0
    c["Gr"] = _bf(np.cos(th).reshape(128, K1 * 128))
    c["Gi"] = _bf((-np.sin(th)).reshape(128, K1 * 128))
    k2 = np.arange(128).astype(np.float64)
    th = 2 * np.pi * k2[:, None] * b[None, :] / 128.0
    wr_, wi_ = np.cos(th), np.sin(th)
    c["Wi1"] = _bf(np.concatenate([wr_, wi_], axis=1))
    c["Wi2"] = _bf(np.concatenate([-wi_, wr_], axis=1))
    k1v = np.arange(K1).astype(np.float64)
    wgt = np.full(K1, 2.0); wgt[0] = 1.0; wgt[32] = 1.0
    av = np.arange(32).astype(np.float64)
    th = 2 * np.pi * (k1v[:, None, None] * av[None, None, :] / 64.0 + k1v[:, None, None] * b[None, :, None] / 8192.0)
    gr_ = (np.cos(th) * wgt[:, None, None] / 8192.0).reshape(K1, 128 * 32)
    gi_ = (-np.sin(th) * wgt[:, None, None] / 8192.0).reshape(K1, 128 * 32)
    c["GiC"] = _bf(np.concatenate([gr_, gi_], axis=0))
    _CONST = c
    return c


ROWV = {}
_off = 0
for _n, _l in (("q_gain", 64), ("k_gain", 64), ("g_attn", 512), ("g_final", 1024), ("b_pg", 1024), ("br", 36), ("fbias", 1024)):
    ROWV[_n] = (_off, _l)
    _off += _l
ROWV_N = _off
V128 = {"g_mix": (0, 8), "g_moe": (8, 8), "g_ple": (16, 8), "conv_w": (24, 36), "conv_b": (60, 12), "g_hy": (72, 4)}
V128_N = 76


def build_nc():
    nc = bass.Bass("TRN2", target_bir_lowering=False)

    def din(name, shape, dt=F32):
        return nc.dram_tensor(name, list(shape), dt, kind="ExternalInput").ap()

    xb = din("xb", [L, D])
    pb = din("pb", [LO, 256])
    w_in = din("w_in", [D, 2304])
    w_out = din("w_out", [D, D])
    if STAGE > 3:
        wgate = din("wgate", [32, D, 512])
        wup = din("wup", [32, D, 512])
        wdown = din("wdown", [32, 512, D])
    wpg = din("wpg", [D, D])
    wple = din("wple", [256, D])
    wf1 = din("wf1", [33, 64])
    wf2 = din("wf2", [64, 64])
    wf3 = din("wf3", [64, 2048])
    wr = din("wr", [D, 36])
    v128 = din("v128", [128, V128_N])
    v64 = din("v64", [64, 4])
    rowv = din("rowv", [1, ROWV_N])
    ident_d = din("ident", [128, 128])
    bones_d = din("bones", [128, 128])
    ones32_d = din("ones32", [32, 32])
    ropeC_d = din("ropeC", [L, 64])
    ropeS_d = din("ropeS", [L, 64])
    zT_d = din("zT", [33, L])
    dec_d = din("dec", [32, 512, 128], BF16)
    F64_d = din("F64", [32, 2 * K1], BF16)
    Gr_d = din("Gr", [128, K1 * 128], BF16)
    Gi_d = din("Gi", [128, K1 * 128], BF16)
    Wi1_d = din("Wi1", [128, 256], BF16)
    Wi2_d = din("Wi2", [128, 256], BF16)
    GiC_d = din("GiC", [2 * K1, 128 * 32], BF16)
    out_d = nc.dram_tensor("out", [LO, D], F32, kind="ExternalOutput").ap()
    ucd = nc.dram_tensor("ucd", [1536, L], BF16, kind="Internal").ap()
    yhd = nc.dram_tensor("yhd", [512, LO], BF16, kind="Internal").ap()

    with ExitStack() as st:
        S = Sched(nc, st)

        def sb(name, shape, dt=F32):
            return st.enter_context(nc.sbuf_tensor("s_" + name, list(shape), dt))

        PS = [st.enter_context(nc.psum_tensor("ps%d" % i, [128, 512], F32)) for i in range(8)]
        psk = [0]

        def nextps(lo=0, hi=8):
            i = lo + psk[0] % (hi - lo)
            psk[0] += 1
            return PS[i], "ps%d" % i

        def T(eng, meth, reads, writes, **kw):
            return S.op(eng, (meth, kw), list(reads), list(writes))

        def DMA(eng, reads, writes, **kw):
            return S.dma(eng, ("dma_start", kw), list(reads), list(writes))

        def load(dst, src, key, reads=()):
            DMA("sp", reads, [key], out=dst, in_=src)


        ident = sb("ident", [128, 128])
        load(ident[:], ident_d, "ident")
        v128t = sb("v128t", [128, V128_N])
        load(v128t[:], v128, "v128")
        v64t = sb("v64t", [64, 4])
        load(v64t[:], v64, "v64")
        RA = ROWV["g_final"][0]
        RB0 = ROWV["br"][0]
        RBN = ROWV_N - RB0
        rowt = sb("rowt", [128, RA + RBN])
        load(rowt[:, 0:RA], rowv[:, 0:RA].partition_broadcast(128), "rowt")
        load(rowt[:, RA:RA + RBN], rowv[:, RB0:ROWV_N].partition_broadcast(128), "rowt")

        def rcol(off):
            return off if off < RA else off - RB0 + RA
        epst = sb("epst", [128, 1])
        T("dve", "memset", [], ["epst"], ap=epst[:], constant=EPS)
        negpi = sb("negpi", [128, 1])
        T("dve", "memset", [], ["negpi"], ap=negpi[:], constant=-math.pi)
        junk = sb("junk", [128, 1024])
        ssq = sb("ssq", [128, 16])

        def rv(name):
            o, l = ROWV[name]
            return rowt[:, rcol(o):rcol(o) + l]

        def pv(name, j=0, n=1):
            o, l = V128[name]
            return v128t[:, o + j:o + j + n]

        def rstd_of(src_ap, width, key_src, dst_ap, key_dst):
            T("dve", "tensor_tensor", [key_src], ["junk"], out=junk[:, 0:width], in0=src_ap, in1=src_ap, op=ALU.mult)
            T("dve", "reduce_sum", ["junk"], ["ssq"], out=ssq[:, 0:1], in_=junk[:, 0:width], axis=AX.X)
            T("act", "activation", ["ssq", "epst"], ["ssq"], out=ssq[:, 1:2], in_=ssq[:, 0:1], func=ACT.Sqrt, bias=epst[:], scale=1.0 / width)
            T("dve", "reciprocal", ["ssq"], [key_dst], out=dst_ap, in_=ssq[:, 1:2])

        def norm_transpose(xt, xk, rs, rk, gname, dstT, tcol, tkey):
            rstd_of(xt[:], D, xk, rs[:], rk)
            T("act", "activation", [xk, rk], [xk], out=xt[:], in_=xt[:], func=ACT.Copy, scale=rs[:, 0:1])
            for g in range(2):
                ps, pk = nextps(0, 4)
                for kq in range(4):
                    k = g * 4 + kq
                    T("pe", "transpose", [xk, "ident"], [pk], out=ps[:, kq * 128:(kq + 1) * 128], in_=xt[:, k * 128:(k + 1) * 128], identity=ident[:])
                for kq in range(4):
                    k = g * 4 + kq
                    if kq % 2 == 0:
                        T("dve", "tensor_scalar", [pk, "v128"], [tkey], out=dstT[:, k, tcol:tcol + 128], in0=ps[:, kq * 128:(kq + 1) * 128],
                          scalar1=pv(gname, k), scalar2=None, op0=ALU.mult)
                    else:
                        T("act", "activation", [pk, "v128"], [tkey], out=dstT[:, k, tcol:tcol + 128], in_=ps[:, kq * 128:(kq + 1) * 128],
                          func=ACT.Copy, scale=pv(gname, k))

        mixd = nc.dram_tensor("mixd", [LO, 512], F32, kind="Internal").ap()
        ph1 = ExitStack()
        sb1 = lambda name, shape, dt=F32: ph1.enter_context(nc.sbuf_tensor("s_" + name, list(shape), dt))
        QTz = [sb1("QT0", [128, 4, LO], BF16), sb1("QT1", [128, 4, LO], BF16)]
        T("pool", "memset", [], ["QTz0"], ap=QTz[0][64:128, :, :], constant=0.0)
        T("pool", "memset", [], ["QTz1"], ap=QTz[1][0:64, :, :], constant=0.0)
        KT2 = sb1("KT2", [128, 2, L], BF16)
        Vx = sb1("Vx", [128, NT, 2, 65], BF16)
        T("pool", "memset", [], ["Vx"], ap=Vx[:], constant=1.0)
        phT = ExitStack()
        hT = phT.enter_context(nc.sbuf_tensor("s_hT", [128, 8, L], BF16))
        phA = ExitStack()
        NXB = 4
        xt2 = [phA.enter_context(nc.sbuf_tensor("s_xt%d" % i, [128, D], F32)) for i in range(NXB)]
        rs2 = [phA.enter_context(nc.sbuf_tensor("s_rs%d" % i, [128, 1], F32)) for i in range(NXB)]

        def A_tile(i):
            xt, xk = xt2[i % NXB], "xt%d" % (i % NXB)
            load(xt[:], xb[i * 128:(i + 1) * 128, :], xk)
            norm_transpose(xt, xk, rs2[i % NXB], "rs%d" % (i % NXB), "g_mix", hT, i * 128, "hT:%d" % i)

        phB = ExitStack()
        sbB = lambda name, shape, dt=F32: phB.enter_context(nc.sbuf_tensor("s_" + name, list(shape), dt))
        wst = sbB("wst", [128, 8, 256])
        wqkv = sbB("wqkv", [128, 8, 768], BF16)
        for j in range(3):
            load(wst[:], w_in[:, j * 256:(j + 1) * 256].rearrange("(k p) n -> p k n", p=128), "wst")
            T("pool", "tensor_copy", ["wst"], ["wqkv"], out=wqkv[:, :, j * 256:(j + 1) * 256], in_=wst[:])
        ropeC = sbB("ropeC", [128, NT, 64])
        ropeS = sbB("ropeS", [128, NT, 64])
        load(ropeC[:], ropeC_d.rearrange("(n p) f -> p n f", p=128), "ropeC")
        load(ropeS[:], ropeS_d.rearrange("(n p) f -> p n f", p=128), "ropeS")
        qk = sbB("qk", [128, 10, 64])
        qk2 = sbB("qk2", [128, 10, 64])
        qk3 = sbB("qk3", [128, 12, 64])
        hs10 = sbB("hs10", [128, 10])

        def B1_tile(i):
            own = i < NO
            nh = 10 if own else 2
            h0 = 0 if own else 8
            hk = ["hT:%d" % i, "wqkv"]
            ps2, pk2 = nextps(0, 4)
            if own:
                ps, pk = nextps(0, 4)
                for k in range(8):
                    T("pe", "matmul", hk, [pk], out=ps[:, 0:512], lhsT=hT[:, k, i * 128:(i + 1) * 128], rhs=wqkv[:, k, 0:512], start=(k == 0), stop=(k == 7))
            for k in range(8):
                T("pe", "matmul", hk, [pk2], out=ps2[:, 0:256], lhsT=hT[:, k, i * 128:(i + 1) * 128], rhs=wqkv[:, k, 512:768], start=(k == 0), stop=(k == 7))
            if own:
                T("act", "activation", [pk], ["qk"], out=qk[:, 0:8, :], in_=ps[:, 0:512].rearrange("p (h d) -> p h d", d=64), func=ACT.Copy)
            T("act", "activation", [pk2], ["qk"], out=qk[:, 8:10, :], in_=ps2[:, 0:128].rearrange("p (h d) -> p h d", d=64), func=ACT.Copy)
            T("dve", "tensor_copy", [pk2], ["Vx"], out=Vx[:, i, :, 0:64], in_=ps2[:, 128:256].rearrange("p (h d) -> p h d", d=64))
            T("dve", "tensor_tensor", ["qk"], ["qk2"], out=qk2[:, h0:10, :], in0=qk[:, h0:10, :], in1=qk[:, h0:10, :], op=ALU.mult)
            T("dve", "reduce_sum", ["qk2"], ["hs10"], out=hs10[:, h0:10], in_=qk2[:, h0:10, :], axis=AX.X)
            T("act", "activation", ["hs10", "epst"], ["hs10"], out=hs10[:, h0:10], in_=hs10[:, h0:10], func=ACT.Sqrt, bias=epst[:], scale=1.0 / 64)
            T("dve", "reciprocal", ["hs10"], ["hs10"], out=hs10[:, h0:10], in_=hs10[:, h0:10])
            T("dve", "tensor_tensor", ["qk", "hs10"], ["qk"], out=qk[:, h0:10, :], in0=qk[:, h0:10, :],
              in1=hs10[:, h0:10].unsqueeze(2).broadcast_to([128, nh, 64]), op=ALU.mult)
            if own:
                T("dve", "tensor_tensor", ["qk", "rowt"], ["qk"], out=qk[:, 0:8, :], in0=qk[:, 0:8, :],
                  in1=rv("q_gain").unsqueeze(1).broadcast_to([128, 8, 64]), op=ALU.mult)
            T("dve", "tensor_tensor", ["qk", "rowt"], ["qk"], out=qk[:, 8:10, :], in0=qk[:, 8:10, :],
              in1=rv("k_gain").unsqueeze(1).broadcast_to([128, 2, 64]), op=ALU.mult)
            v5 = qk[:, h0:10, :].rearrange("p h (g t n) -> p h g t n", g=2, t=2, n=16)
            w5 = qk2[:, h0:10, :].rearrange("p h (g t n) -> p h g t n", g=2, t=2, n=16)
            T("pool", "tensor_copy", ["qk"], ["qk2"], out=w5[:, :, :, 0, :], in_=v5[:, :, :, 1, :])
            T("pool", "tensor_copy", ["qk"], ["qk2"], out=w5[:, :, :, 1, :], in_=v5[:, :, :, 0, :])
            T("dve", "tensor_tensor", ["qk", "ropeC"], ["qk"], out=qk[:, h0:10, :], in0=qk[:, h0:10, :],
              in1=ropeC[:, i, :].unsqueeze(1).broadcast_to([128, nh, 64]), op=ALU.mult)
            T("dve", "tensor_tensor", ["qk2", "ropeS"], ["qk2"], out=qk2[:, h0:10, :], in0=qk2[:, h0:10, :],
              in1=ropeS[:, i, :].unsqueeze(1).broadcast_to([128, nh, 64]), op=ALU.mult)
            if own:
                T("dve", "tensor_tensor", ["qk", "qk2"], ["qk3"], out=qk3[:, 0:8, :], in0=qk[:, 0:8, :], in1=qk2[:, 0:8, :], op=ALU.add)
            for kv in range(2):
                for dup in range(2):
                    T("dve", "tensor_tensor", ["qk", "qk2"], ["qk3"], out=qk3[:, 8 + 2 * kv + dup, :], in0=qk[:, 8 + kv, :], in1=qk2[:, 8 + kv, :], op=ALU.add)
            if own:
                ps, pk = nextps(0, 4)
                for blk in range(4):
                    T("pe", "transpose", ["qk3", "ident"], [pk], out=ps[:, blk * 128:(blk + 1) * 128],
                      in_=qk3[:, 2 * blk:2 * blk + 2, :].rearrange("p h d -> p (h d)"), identity=ident[:])
                T("act", "activation", [pk, "QTz0"], ["QT:%d" % i], out=QTz[0][0:64, :, i * 128:(i + 1) * 128], in_=ps[0:64, 0:512].rearrange("p (h t) -> p h t", t=128), func=ACT.Copy)
                T("act", "activation", [pk, "QTz1"], ["QT:%d" % i], out=QTz[1][64:128, :, i * 128:(i + 1) * 128], in_=ps[64:128, 0:512].rearrange("p (h t) -> p h t", t=128), func=ACT.Copy)
            ps3, pk3 = nextps(0, 4)
            for kv in range(2):
                T("pe", "transpose", ["qk3", "ident"], [pk3], out=ps3[:, kv * 128:(kv + 1) * 128],
                  in_=qk3[:, 8 + 2 * kv:10 + 2 * kv, :].rearrange("p h d -> p (h d)"), identity=ident[:])
            T("act", "activation", [pk3], ["KT2:%d" % i], out=KT2[:, :, i * 128:(i + 1) * 128], in_=ps3[:, 0:256].rearrange("p (h t) -> p h t", t=128), func=ACT.Copy)
        A_tile(0)
        A_tile(1)
        for i in range(NT):
            if i + 2 < NT:
                A_tile(i + 2)
            B1_tile(i)
        S.barrier()
        phB.close()
        phA.close()
        phB2 = ExitStack()
        sbB2 = lambda name, shape, dt=F32: phB2.enter_context(nc.sbuf_tensor("s_" + name, list(shape), dt))
        wst2 = sbB2("wst2", [128, 8, 128])
        wct = [sbB2("wct0", [128, 8, 128], BF16), sbB2("wct1", [128, 8, 128], BF16)]
        U = sbB2("U", [128, L + 2])
        T("pool", "memset", [], ["U"], ap=U[:], constant=0.0)
        HL = L // 2
        uc32 = sbB2("uc32", [128, HL])
        ucb = sbB2("ucb", [128, L], BF16)
        PT = [sbB2("PT0", [128, 512], BF16), sbB2("PT1", [128, 512], BF16), sbB2("PT2", [128, 512], BF16)]
        STB = [0, 1, 7]
        rden = sbB2("rden", [128, 4])
        accS = [sbB2("accS0", [65, 512]), sbB2("accS1", [65, 512])]
        stg = [sbB2("stg0", [128, 4, 64]), sbB2("stg1", [128, 4, 64])]
        mixv = mixd.rearrange("(n p) f -> p n f", p=128)

        def b2_gen():
            for ct in range(12):
                wc, wk = wct[ct % 2], "wct%d" % (ct % 2)
                load(wst2[:], w_in[:, 768 + ct * 128:768 + (ct + 1) * 128].rearrange("(k p) n -> p k n", p=128), "wst2")
                T("pool", "tensor_copy", ["wst2"], [wk], out=wc[:], in_=wst2[:])
                for tcn in range(8):
                    ps, pk = nextps(2, 4)
                    for k in range(8):
                        T("pe", "matmul", [wk] + ["hT:%d" % j for j in range(tcn * 4, tcn * 4 + 4)], [pk], out=ps[:, :], lhsT=wc[:, k, :],
                          rhs=hT[:, k, tcn * 512:(tcn + 1) * 512], start=(k == 0), stop=(k == 7))
                    T("dve", "tensor_copy", [pk], ["U"], out=U[:, 1 + tcn * 512:1 + (tcn + 1) * 512], in_=ps[:, :])
                    yield
                for hh in range(2):
                    o = hh * HL
                    T("dve", "tensor_scalar", ["U", "v128"], ["uc32"], out=uc32[:], in0=U[:, o:o + HL], scalar1=pv("conv_w", ct * 3 + 0), scalar2=pv("conv_b", ct), op0=ALU.mult, op1=ALU.add)
                    T("dve", "scalar_tensor_tensor", ["U", "v128", "uc32"], ["uc32"], out=uc32[:], in0=U[:, o + 1:o + HL + 1], scalar=pv("conv_w", ct * 3 + 1), in1=uc32[:], op0=ALU.mult, op1=ALU.add)
                    T("dve", "scalar_tensor_tensor", ["U", "v128", "uc32"], ["ucb"], out=ucb[:, o:o + HL], in0=U[:, o + 2:o + HL + 2], scalar=pv("conv_w", ct * 3 + 2), in1=uc32[:], op0=ALU.mult, op1=ALU.add)
                    yield
                DMA("sp", ["ucb"], ["ucd"], out=ucd[ct * 128:(ct + 1) * 128, :], in_=ucb[:])

        scale = 64 ** -0.5
        allQT = ["QT:%d" % i for i in range(NO)]
        iters = []
        for hp in range(4):
            for j in range(2):
                for qg in range(4):
                    for kt in range(NT):
                        iters.append((hp, j, qg, kt))

        def emit_st(n):
            hp, j, qg, kt = iters[n]
            kv = hp // 2
            T("pe", "matmul", ["KT2:%d" % kt] + allQT[qg * 4:qg * 4 + 4], ["ps%d" % STB[n % 3]], out=PS[STB[n % 3]][:, :], lhsT=KT2[:, kv, kt * 128:(kt + 1) * 128],
              rhs=QTz[j][:, hp, qg * 512:(qg + 1) * 512], start=True, stop=True)

        def attn_gen():
            emit_st(0)
            emit_st(1)
            ng = 0
            for n in range(len(iters)):
                hp, j, qg, kt = iters[n]
                kv = hp // 2
                h = 2 * hp + j
                pss, pks = PS[STB[n % 3]], "ps%d" % STB[n % 3]
                pt, ptk = PT[n % 3], "PT%d" % (n % 3)
                T("act", "activation", [pks], [ptk], out=pt[:], in_=pss[:, :], func=ACT.Exp, scale=scale)
                if n + 2 < len(iters):
                    emit_st(n + 2)
                gi_ = n // NT
                accb = 4 + (gi_ % 2)
                T("pe", "matmul", [ptk, "Vx"], ["ps%d" % accb], out=PS[accb][0:65, :], lhsT=Vx[:, kt, kv, :], rhs=pt[:, :],
                  start=(kt == 0), stop=(kt == NT - 1))
                if kt == NT - 1:
                    sg_, sgk = stg[ng % 2], "stg%d" % (ng % 2)
                    ac_, ack = accS[ng % 2], "accS%d" % (ng % 2)
                    trb = 6
                    ng += 1
                    T("act", "activation", ["ps%d" % accb], [ack], out=ac_[:, :], in_=PS[accb][0:65, :], func=ACT.Copy)
                    for qi in range(4):
                        T("pe", "transpose", [ack, "ident"], ["ps%d" % trb], out=PS[trb][:, qi * 65:(qi + 1) * 65], in_=ac_[:, qi * 128:(qi + 1) * 128], identity=ident[0:65, 0:65])
                    for qi in range(4):
                        T("dve", "reciprocal", ["ps%d" % trb], ["rden"], out=rden[:, qi:qi + 1], in_=PS[trb][:, qi * 65 + 64:qi * 65 + 65])
                        T("dve", "tensor_scalar", ["ps%d" % trb, "rden"], [sgk], out=sg_[:, qi, :], in0=PS[trb][:, qi * 65:qi * 65 + 64],
                          scalar1=rden[:, qi:qi + 1], scalar2=None, op0=ALU.mult)
                    DMA("sp", [sgk], ["mixd"], out=mixv[:, qg * 4:(qg + 1) * 4, h * 64:(h + 1) * 64], in_=sg_[:])
                yield

        ga, gb = attn_gen(), b2_gen()
        da = db = False
        na_ = 0
        while not (da and db):
            if not da:
                try:
                    next(ga)
                    na_ += 1
                except StopIteration:
                    da = True
            if not db and (da or na_ % 8 == 0):
                try:
                    next(gb)
                except StopIteration:
                    db = True
        S.barrier()
        phB2.close()
        phT.close()
        ph1.close()
        TWO_PI = 2.0 * math.pi
        phD = ExitStack()
        sbD = lambda name, shape, dt=F32: phD.enter_context(nc.sbuf_tensor("s_" + name, list(shape), dt))
        F64 = sbD("F64", [32, 2 * K1], BF16); load(F64[:], F64_d, "F64")
        Gr = sbD("Gr", [128, K1, 128], BF16); load(Gr[:], Gr_d.rearrange("p (k m) -> p k m", m=128), "Gr")
        Gi = sbD("Gi", [128, K1, 128], BF16); load(Gi[:], Gi_d.rearrange("p (k m) -> p k m", m=128), "Gi")
        Wi1 = sbD("Wi1", [128, 256], BF16); load(Wi1[:], Wi1_d, "Wi1")
        GiC = sbD("GiC", [2 * K1, 128, 32], BF16)
        DMA("sp", [], ["GiC"], out=GiC[:, :, :], in_=GiC_d.rearrange("p (b a) -> p b a", a=32))
        h2T = sbD("h2T", [64, L], BF16)
        phM = ExitStack()
        sbM = lambda name, shape, dt=F32: phM.enter_context(nc.sbuf_tensor("s_" + name, list(shape), dt))
        zT = sbM("zT", [33, L]); load(zT[:], zT_d, "zT")
        wf1t = sbM("wf1t", [33, 64]); load(wf1t[:], wf1, "wf1t")
        wf2t = sbM("wf2t", [64, 64]); load(wf2t[:], wf2, "wf2t")
        h1T = sbM("h1T", [64, L])
        mtmp = sbM("mtmp", [64, 512])
        mti = sbM("mti", [64, 512], I32)
        mtf = sbM("mtf", [64, 512])
        for (wt_, wkey, src, skey, dst, dkey, bcol) in ((wf1t, "wf1t", zT, "zT", h1T, "h1T", 0), (wf2t, "wf2t", h1T, "h1T", h2T, "h2T", 2)):
            for tcn in range(8):
                ps, pk = nextps()
                T("pe", "matmul", [wkey, skey], [pk], out=ps[0:64, :], lhsT=wt_[:, :], rhs=src[:, tcn * 512:(tcn + 1) * 512], start=True, stop=True)
                T("dve", "tensor_scalar", [pk, "v64"], ["mtmp"], out=mtmp[:], in0=ps[0:64, :], scalar1=v64t[:, bcol:bcol + 1], scalar2=v64t[:, bcol + 1:bcol + 2], op0=ALU.add, op1=ALU.mult)
                T("dve", "tensor_scalar", ["mtmp"], ["mti"], out=mti[:], in0=mtmp[:], scalar1=1.0 / TWO_PI, scalar2=None, op0=ALU.mult)
                T("dve", "tensor_copy", ["mti"], ["mtf"], out=mtf[:], in_=mti[:])
                T("dve", "scalar_tensor_tensor", ["mtf", "mtmp"], ["mtmp"], out=mtmp[:], in0=mtf[:], scalar=-TWO_PI, in1=mtmp[:], op0=ALU.mult, op1=ALU.add)
                T("dve", "tensor_scalar", ["mtmp"], ["mtf"], out=mtf[:], in0=mtmp[:], scalar1=math.pi, scalar2=-TWO_PI, op0=ALU.is_gt, op1=ALU.mult)
                T("dve", "tensor_tensor", ["mtmp", "mtf"], ["mtmp"], out=mtmp[:], in0=mtmp[:], in1=mtf[:], op=ALU.add)
                T("dve", "tensor_scalar", ["mtmp"], ["mtf"], out=mtf[:], in0=mtmp[:], scalar1=-math.pi, scalar2=TWO_PI, op0=ALU.is_lt, op1=ALU.mult)
                T("dve", "tensor_tensor", ["mtmp", "mtf"], ["mtmp"], out=mtmp[:], in0=mtmp[:], in1=mtf[:], op=ALU.add)
                T("act", "activation", ["mtmp"], [dkey], out=dst[:, tcn * 512:(tcn + 1) * 512], in_=mtmp[:], func=ACT.Sin)
        S.barrier()
        phM.close()
        onesB = sbD("onesB", [32, 128])
        T("pool", "memset", [], ["onesB"], ap=onesB[:], constant=1.0)

        def make_pipe(pid, chunks):
            def K(k):
                return "%s_p%d" % (k, pid)

            w3s = sbD("w3s_%d" % pid, [64, 2, CC])
            w3c = sbD("w3c_%d" % pid, [64, 2, CC], BF16)
            dect = sbD("dect_%d" % pid, [32, CC, 128], BF16)
            hs = sbD("hs_%d" % pid, [32, 2, CC, 128], BF16)
            l1 = sbD("l1_%d" % pid, [32, 2 * CC])
            AplG = sbD("AplG_%d" % pid, [128, K1, 3, CC], BF16)
            AplC = sbD("AplC_%d" % pid, [128, K1, 3, CC], BF16)
            Xt = sbD("Xt_%d" % pid, [128, K1, 2, CC], BF16)
            Yt = sbD("Yt_%d" % pid, [128, 3, K1, CC], BF16)
            rinvB = sbD("rinvB_%d" % pid, [128, 2, CC])
            rbs = sbD("rbs_%d" % pid, [128, 2, CC])
            tmpG = sbD("tmpG_%d" % pid, [128, GK, 2, CC], BF16)
            Hts = [sbD("Ht0_%d" % pid, [128, K1, 2, CC], BF16), sbD("Ht1_%d" % pid, [128, K1, 2, CC], BF16)]
            Dt = sbD("Dt_%d" % pid, [2 * K1, 128, CC], BF16)
            v3 = sbD("v3_%d" % pid, [32, CC, 128], BF16)
            xg = sbD("xg_%d" % pid, [32, CC, 128], BF16)
            z1 = v3
            h2v = h2T[:, :].rearrange("p (a b) -> p a b", b=128)

            def fdft(src3, skey, mode, Apl, akey, Ht=None, hkey=None, mid_hook=None):
                for cg in range(CC // 4):
                    ps, pk = nextps()
                    for cq in range(4):
                        c = cg * 4 + cq
                        T("pe", "matmul", [skey, "F64"], [pk], out=ps[:, cq * 2 * K1:(cq + 1) * 2 * K1], lhsT=src3[:, c, :], rhs=F64[:, :], start=True, stop=True)
                    pv4 = ps[:, 0:8 * K1].rearrange("p (c r k) -> p k r c", c=4, r=2, k=K1)
                    T("act", "activation", [pk], [akey], out=Apl[:, :, 1:3, cg * 4:cg * 4 + 4], in_=pv4, func=ACT.Copy)
                    T("act", "activation", [pk], [akey], out=Apl[:, :, 0, cg * 4:cg * 4 + 4], in_=pv4[:, :, 1, :], func=ACT.Copy, scale=-1.0)
                    if cg % 2 == 1:
                        yield
                hooks = list(mid_hook) if mid_hook is not None else []
                for kg in range((K1 + GK - 1) // GK):
                    ps, pk = nextps()
                    nk = min(GK, K1 - kg * GK)
                    for kq in range(nk):
                        k1 = kg * GK + kq
                        off = kq * 2 * CC
                        T("pe", "matmul", [akey, "Gr"], [pk], out=ps[:, off:off + 2 * CC], lhsT=Gr[:, k1, :], rhs=Apl[:, k1, 1:3, :].rearrange("p r c -> p (r c)"), start=True, stop=False)
                        T("pe", "matmul", [akey, "Gi"], [pk], out=ps[:, off:off + 2 * CC], lhsT=Gi[:, k1, :], rhs=Apl[:, k1, 0:2, :].rearrange("p r c -> p (r c)"), start=False, stop=True)
                    pv3 = ps[:, 0:nk * 2 * CC].rearrange("p (k r c) -> p k r c", k=nk, r=2, c=CC)
                    if mode == "X":
                        T("act", "activation", [pk], [K("Xt")], out=Xt[:, kg * GK:kg * GK + nk, :, :], in_=pv3, func=ACT.Copy)
                    elif mode == "Hset":
                        T("act", "activation", [pk], [hkey], out=Ht[:, kg * GK:kg * GK + nk, :, :], in_=pv3, func=ACT.Copy)
                    else:
                        T("dve", "tensor_tensor", [pk, K("rbs")], [K("tmpG")], out=tmpG[:, 0:nk, :, :], in0=pv3, in1=rbs[:, :, :].unsqueeze(1).broadcast_to([128, nk, 2, CC]), op=ALU.mult)
                        T("dve", "tensor_tensor", [K("tmpG"), hkey], [hkey], out=Ht[:, kg * GK:kg * GK + nk, :, :], in0=Ht[:, kg * GK:kg * GK + nk, :, :], in1=tmpG[:, 0:nk, :, :], op=ALU.add)
                    if hooks:
                        hooks.pop(0)()
                    if kg % 2 == 1:
                        yield
                while hooks:
                    hooks.pop(0)()

            def gen_H(o, c0, Ht, hkey):
                for dr in range(2):
                    col = o * 1024 + dr * 512 + c0
                    DMA("pool", [], [K("w3s")], out=w3s[:, dr, :], in_=wf3[:, col:col + CC])
                DMA("pool", [], [K("dect")], out=dect[:], in_=dec_d[:, c0:c0 + CC, :])
                T("pool", "tensor_copy", [K("w3s")], [K("w3c")], out=w3c[:], in_=w3s[:])
                for bg in range(128 // GK):
                    ps, pk = nextps()
                    for bq in range(GK):
                        b = bg * GK + bq
                        T("pe", "matmul", ["h2T", K("w3c")], [pk], out=ps[0:32, bq * 2 * CC:(bq + 1) * 2 * CC], lhsT=h2v[:, :, b], rhs=w3c[:, :, :].rearrange("p g c -> p (g c)"), start=True, stop=True)
                    T("dve", "tensor_tensor", [pk, K("dect")], [K("hs")], out=hs[:, :, :, bg * GK:(bg + 1) * GK], in0=ps[0:32, :].rearrange("p (b g c) -> p g c b", b=GK, g=2, c=CC),
                      in1=dect[:, :, bg * GK:(bg + 1) * GK].unsqueeze(1).broadcast_to([32, 2, CC, GK]), op=ALU.mult)
                    if bg % 4 == 3:
                        yield
                def l1_piece(g, hf):
                    def f():
                        T("dve", "tensor_reduce", [K("hs")], [K("l1")], out=l1[:, g * CC + hf * (CC // 2):g * CC + (hf + 1) * (CC // 2)], in_=hs[:, g, hf * (CC // 2):(hf + 1) * (CC // 2), :], axis=AX.X, op=ALU.add, apply_absolute_value=True)
                    return f

                def l1_fin():
                    ps, pk = nextps()
                    T("pe", "matmul", [K("l1"), "onesB"], [pk], out=ps[:, 0:2 * CC], lhsT=onesB[:, :], rhs=l1[:, :], start=True, stop=True)
                    T("dve", "tensor_scalar", [pk], [K("rinvB")], out=rinvB[:, :, :].rearrange("p g c -> p (g c)"), in0=ps[:, 0:2 * CC], scalar1=EPS, scalar2=None, op0=ALU.add)
                    T("dve", "reciprocal", [K("rinvB")], [K("rinvB")], out=rinvB[:, :, :], in_=rinvB[:, :, :])
                    T("dve", "tensor_copy", [K("rinvB")], [K("rbs")], out=rbs[:, 0, :], in_=rinvB[:, 1, :])
                    T("dve", "tensor_scalar", [K("rinvB")], [K("rbs")], out=rbs[:, 1, :], in0=rinvB[:, 1, :], scalar1=-1.0, scalar2=None, op0=ALU.mult)

                l1_hooks = [l1_piece(0, 0), l1_piece(0, 1), l1_piece(1, 0), l1_piece(1, 1), l1_fin]
                yield from fdft(hs[:, 0, :, :], K("hs"), "Hset", AplG, K("AplG"), Ht, hkey, mid_hook=l1_hooks)
                T("dve", "tensor_tensor", [hkey, K("rinvB")], [hkey], out=Ht[:, :, :, :], in0=Ht[:, :, :, :],
                  in1=rinvB[:, 0:1, :].unsqueeze(1).broadcast_to([128, K1, 2, CC]), op=ALU.mult)
                yield from fdft(hs[:, 1, :, :], K("hs"), "Hconj", AplG, K("AplG"), Ht, hkey)
                fo = rcol(ROWV["fbias"][0]) + o * 512 + c0
                T("dve", "tensor_tensor", [hkey, "rowt"], [hkey], out=Ht[:, :, 0, :], in0=Ht[:, :, 0, :], in1=rowt[:, fo:fo + CC].unsqueeze(1).broadcast_to([128, K1, CC]), op=ALU.add)
                yield

            def conv(src3, skey, na, dst3, dkey, Ht, hkey):
                yield from fdft(src3, skey, "X", AplC, K("AplC"))
                tA, tB = Yt[:, 0, :, :], AplC[:, :, 0, :]
                T("dve", "tensor_tensor", [K("Xt"), hkey], [K("Yt")], out=tA, in0=Xt[:, :, 0, :], in1=Ht[:, :, 0, :], op=ALU.mult)
                T("dve", "tensor_tensor", [K("Xt"), hkey], [K("AplC")], out=tB, in0=Xt[:, :, 1, :], in1=Ht[:, :, 1, :], op=ALU.mult)
                T("dve", "tensor_tensor", [K("Yt"), K("AplC")], [K("Yt")], out=Yt[:, 1, :, :], in0=tA, in1=tB, op=ALU.subtract)
                yield
                T("dve", "tensor_tensor", [K("Xt"), hkey], [K("Yt")], out=tA, in0=Xt[:, :, 0, :], in1=Ht[:, :, 1, :], op=ALU.mult)
                T("dve", "tensor_tensor", [K("Xt"), hkey], [K("AplC")], out=tB, in0=Xt[:, :, 1, :], in1=Ht[:, :, 0, :], op=ALU.mult)
                T("dve", "tensor_tensor", [K("Yt"), K("AplC")], [K("Yt")], out=Yt[:, 2, :, :], in0=tA, in1=tB, op=ALU.add)
                T("act", "activation", [K("Yt")], [K("Yt")], out=Yt[:, 0, :, :], in_=Yt[:, 2, :, :], func=ACT.Copy, scale=-1.0)
                yield
                for cg in range(CC // 4):
                    ps, pk = nextps()
                    for cq in range(4):
                        c = cg * 4 + cq
                        T("pe", "matmul", [K("Yt"), "Wi1"], [pk], out=ps[0:2 * K1, cq * 128:(cq + 1) * 128], lhsT=Yt[:, 1:3, :, c].rearrange("p r k -> p (r k)"), rhs=Wi1[:, 0:128], start=True, stop=False)
                        T("pe", "matmul", [K("Yt"), "Wi1"], [pk], out=ps[0:2 * K1, cq * 128:(cq + 1) * 128], lhsT=Yt[:, 0:2, :, c].rearrange("p r k -> p (r k)"), rhs=Wi1[:, 128:256], start=False, stop=True)
                    T("act", "activation", [pk], [K("Dt")], out=Dt[:, :, cg * 4:cg * 4 + 4], in_=ps[0:2 * K1, :].rearrange("p (c b) -> p b c", c=4, b=128), func=ACT.Copy)
                    if cg % 2 == 1:
                        yield
                for bg in range(128 // SB):
                    ps, pk = nextps()
                    for bq in range(SB):
                        b = bg * SB + bq
                        T("pe", "matmul", [K("Dt"), "GiC"], [pk], out=ps[0:na, bq * CC:(bq + 1) * CC], lhsT=GiC[:, b, 0:na], rhs=Dt[:, b, :], start=True, stop=True)
                    T("dve", "tensor_tensor", [pk, K("xg")], [dkey], out=dst3[0:na, :, bg * SB:(bg + 1) * SB], in0=ps[0:na, :].rearrange("p (b c) -> p c b", b=SB, c=CC),
                      in1=xg[0:na, :, bg * SB:(bg + 1) * SB], op=ALU.mult)
                    if bg % 2 == 1:
                        yield

            def conv_step2(ch, o, Ht, hkey):
                c0 = ch * CC
                if o == 0:
                    DMA("sp", ["ucd"], [K("v3")], out=v3[:], in_=ucd[c0:c0 + CC, :].rearrange("c (a b) -> a c b", b=128))
                    DMA("sp", ["ucd"], [K("xg")], out=xg[:], in_=ucd[512 + c0:512 + c0 + CC, :].rearrange("c (a b) -> a c b", b=128))
                    yield from conv(v3, K("v3"), 32, v3, K("v3"), Ht, hkey)
                else:
                    DMA("sp", ["ucd"], [K("xg")], out=xg[0:16, :, :], in_=ucd[1024 + c0:1024 + c0 + CC, 0:LO].rearrange("c (a b) -> a c b", b=128))
                    yield from conv(v3, K("v3"), 16, v3, K("v3"), Ht, hkey)
                    DMA("sp", [K("v3")], [K("yhd")], out=yhd[c0:c0 + CC, :].rearrange("c (a b) -> a c b", b=128), in_=v3[0:16, :, :])

            def run():
                steps = [(ch, o) for ch in chunks for o in range(2)]

                def gstep(j):
                    ch, o = steps[j]
                    yield from gen_H(o, ch * CC, Hts[j % 2], K("Ht%d" % (j % 2)))

                def cstep(j):
                    ch, o = steps[j]
                    yield from conv_step2(ch, o, Hts[j % 2], K("Ht%d" % (j % 2)))

                for _ in gstep(0):
                    yield
                for j in range(len(steps)):
                    gc = cstep(j)
                    gg = gstep(j + 1) if j + 1 < len(steps) else iter(())
                    dc = dg = False
                    while not (dc and dg):
                        if not dg:
                            try:
                                next(gg)
                            except StopIteration:
                                dg = True
                        if not dc:
                            try:
                                next(gc)
                            except StopIteration:
                                dc = True
                        yield
            return run()

        pipes = [make_pipe(0, list(range(0, NCH, 2))), make_pipe(1, list(range(1, NCH, 2)))]
        alive = [True, True]
        while any(alive):
            for pi_ in range(2):
                if alive[pi_]:
                    try:
                        next(pipes[pi_])
                    except StopIteration:
                        alive[pi_] = False
        if STAGE == 2:
            dy = nc.dram_tensor("dbg_yh", [512, LO], BF16, kind="ExternalOutput").ap()
            DMA("sp", ["yhd"], ["dbg_yh"], out=dy, in_=yhd)
            S.final_wait("sp", ["dbg_yh"])
            S.emit()
            phD.close()
            return nc
        S.barrier()
        phD.close()

        phX = ExitStack()
        sbX = lambda name, shape, dt=F32: phX.enter_context(nc.sbuf_tensor("s_" + name, list(shape), dt))
        X1 = sbX("X1", [128, NO, D])
        xt = sbX("xt", [128, D])
        rs = sbX("rs", [128, 1])
        xnT = sbX("xnT", [128, 8, LO], BF16)
        phE = ExitStack()
        sbE = lambda name, shape, dt=F32: phE.enter_context(nc.sbuf_tensor("s_" + name, list(shape), dt))
        mixt = sbE("mixt", [128, NO, 512])
        load(mixt[:], mixd.rearrange("(n p) f -> p n f", p=128), "mixt")
        hsq = sbE("hsq", [128, NO, 8])
        T("dve", "tensor_tensor", ["mixt"], ["junkE"], out=junk[:, 0:512], in0=mixt[:, 0, :], in1=mixt[:, 0, :], op=ALU.mult)
        msq = sbE("msq", [128, 512])
        for i in range(NO):
            T("dve", "tensor_tensor", ["mixt"], ["msq"], out=msq[:], in0=mixt[:, i, :], in1=mixt[:, i, :], op=ALU.mult)
            T("dve", "reduce_sum", ["msq"], ["hsq"], out=hsq[:, i, :], in_=msq[:, :].rearrange("p (h d) -> p h d", d=64), axis=AX.X)
        T("act", "activation", ["hsq", "epst"], ["hsq"], out=hsq[:], in_=hsq[:], func=ACT.Sqrt, bias=epst[:], scale=1.0 / 64)
        T("dve", "reciprocal", ["hsq"], ["hsq"], out=hsq[:], in_=hsq[:])
        for i in range(NO):
            T("dve", "tensor_tensor", ["mixt", "hsq"], ["mixt"], out=mixt[:, i, :].rearrange("p (h d) -> p h d", d=64), in0=mixt[:, i, :].rearrange("p (h d) -> p h d", d=64),
              in1=hsq[:, i, :].unsqueeze(2).broadcast_to([128, 8, 64]), op=ALU.mult)
            T("dve", "tensor_tensor", ["mixt", "rowt"], ["mixt"], out=mixt[:, i, :], in0=mixt[:, i, :], in1=rv("g_attn"), op=ALU.mult)
            ps, pk = nextps()
            for blk in range(4):
                T("pe", "transpose", ["mixt", "ident"], [pk], out=ps[:, blk * 128:(blk + 1) * 128], in_=mixt[:, i, blk * 128:(blk + 1) * 128], identity=ident[:])
            T("act", "activation", [pk], ["xnT"], out=xnT[:, 0:4, i * 128:(i + 1) * 128], in_=ps[:, :].rearrange("p (k t) -> p k t", t=128), func=ACT.Copy)
        bones = sbE("bones", [128, 128]); load(bones[:], bones_d, "bones")
        yhT = sbE("yhT", [128, 4, LO], BF16)
        load(yhT[:], yhd.rearrange("(k p) t -> p k t", p=128), "yhT", reads=["mixt"])
        ysq = sbE("ysq", [128, 512])
        yr = sbE("yr", [128, 512])
        for ct in range(4):
            for tcn in range(4):
                sl = slice(tcn * 512, (tcn + 1) * 512)
                T("dve", "tensor_tensor", ["yhT"], ["ysq"], out=ysq[:], in0=yhT[:, ct, sl], in1=yhT[:, ct, sl], op=ALU.mult)
                ps, pk = nextps()
                T("pe", "matmul", ["ysq", "bones"], [pk], out=ps[:, :], lhsT=bones[:, :], rhs=ysq[:, :], start=True, stop=True)
                T("act", "activation", [pk, "epst"], ["yr"], out=yr[:], in_=ps[:, :], func=ACT.Sqrt, bias=epst[:], scale=1.0 / 64)
                T("dve", "reciprocal", ["yr"], ["yr"], out=yr[:], in_=yr[:])
                T("dve", "scalar_tensor_tensor", ["yhT", "yr", "v128"], ["xnT"], out=xnT[:, 4 + ct, sl], in0=yhT[:, ct, sl], scalar=pv("g_hy", ct), in1=yr[:], op0=ALU.mult, op1=ALU.mult)
        wstE = sbE("wstE", [128, 8, 256])
        wbig = sbE("wbig", [128, 8, D], BF16)

        def load_w8(src2d):
            for j in range(4):
                load(wstE[:], src2d[:, j * 256:(j + 1) * 256].rearrange("(k p) n -> p k n", p=128), "wstE")
                T("act", "activation", ["wstE"], ["wbig"], out=wbig[:, :, j * 256:(j + 1) * 256], in_=wstE[:], func=ACT.Copy)

        load_w8(w_out)
        load(X1[:], xb[0:LO, :].rearrange("(n p) f -> p n f", p=128), "X1", reads=["wstE"])
        for i in range(NO):
            for nh in range(2):
                ps, pk = nextps()
                for k in range(8):
                    T("pe", "matmul", ["xnT", "wbig"], [pk], out=ps[:, :], lhsT=xnT[:, k, i * 128:(i + 1) * 128], rhs=wbig[:, k, nh * 512:(nh + 1) * 512], start=(k == 0), stop=(k == 7))
                T("dve", "tensor_tensor", [pk, "X1"], ["X1"], out=X1[:, i, nh * 512:(nh + 1) * 512], in0=X1[:, i, nh * 512:(nh + 1) * 512], in1=ps[:, :], op=ALU.add)
        if STAGE == 3:
            dx = nc.dram_tensor("dbg_x1", [128, NO, D], F32, kind="ExternalOutput").ap()
            DMA("sp", ["X1"], ["dbg_x1"], out=dx, in_=X1[:])
            S.final_wait("sp", ["dbg_x1"])
            S.emit()
            phE.close()
            phX.close()
            return nc
        S.barrier()
        phE.close()

        phF = ExitStack()
        sbF = lambda name, shape, dt=F32: phF.enter_context(nc.sbuf_tensor("s_" + name, list(shape), dt))
        Wt = sbF("Wt", [128, NO, 32])
        wrt = sbF("wrt", [128, 8, 36]); load(wrt[:], wr.rearrange("(k p) n -> p k n", p=128), "wrt")
        xnF = sbF("xnF", [128, 8, 128])
        lg = sbF("lg", [128, 36])
        sm = sbF("sm", [128, 16])
        gm = sbF("gm", [128, 4])
        e1 = sbF("e1", [128, 32])
        e2 = sbF("e2", [128, 32])
        s1 = sbF("s1", [128, 32])
        s2 = sbF("s2", [128, 32])
        BIG = 1.0e9

        def norm_T(i, gname, f32dst=None, xkey="X1", tkey="xnT"):
            T("act", "activation", [xkey], ["xt"], out=xt[:], in_=X1[:, i, :], func=ACT.Copy)
            rstd_of(xt[:], D, "xt", rs[:], "rs")
            T("act", "activation", ["xt", "rs"], ["xt"], out=xt[:], in_=xt[:], func=ACT.Copy, scale=rs[:, 0:1])
            for g in range(2):
                ps, pk = nextps()
                for kq in range(4):
                    k = g * 4 + kq
                    T("pe", "transpose", ["xt", "ident"], [pk], out=ps[:, kq * 128:(kq + 1) * 128], in_=xt[:, k * 128:(k + 1) * 128], identity=ident[:])
                for kq in range(4):
                    k = g * 4 + kq
                    T("dve", "tensor_scalar", [pk, "v128"], [tkey], out=xnT[:, k, i * 128:(i + 1) * 128], in0=ps[:, kq * 128:(kq + 1) * 128], scalar1=pv(gname, k), scalar2=None, op0=ALU.mult)
                    if f32dst is not None:
                        T("dve", "tensor_scalar", [pk, "v128"], ["xnF"], out=f32dst[:, k, :], in0=ps[:, kq * 128:(kq + 1) * 128], scalar1=pv(gname, k), scalar2=None, op0=ALU.mult)

        def route_tile(i):
            norm_T(i, "g_moe", xnF, "X1:%d" % i, "xnT:%d" % i)
            ps, pk = nextps()
            for k in range(8):
                T("pe", "matmul", ["xnF", "wrt"], [pk], out=ps[:, 0:36], lhsT=xnF[:, k, :], rhs=wrt[:, k, :], start=(k == 0), stop=(k == 7))
            T("dve", "tensor_tensor", [pk, "rowt"], ["lg"], out=lg[:], in0=ps[:, 0:36], in1=rv("br"), op=ALU.add)
            T("dve", "reduce_max", ["lg"], ["sm"], out=sm[:, 0:1], in_=lg[:, 0:4], axis=AX.X)
            T("dve", "tensor_scalar", ["lg", "sm"], ["gm"], out=gm[:], in0=lg[:, 0:4], scalar1=sm[:, 0:1], scalar2=None, op0=ALU.subtract)
            T("act", "activation", ["gm"], ["e1"], out=e1[:, 0:4], in_=gm[:], func=ACT.Exp)
            T("dve", "reduce_sum", ["e1"], ["sm"], out=sm[:, 1:2], in_=e1[:, 0:4], axis=AX.X)
            T("dve", "reciprocal", ["sm"], ["sm"], out=sm[:, 2:3], in_=sm[:, 1:2])
            T("dve", "tensor_scalar", ["gm"], ["gm"], out=gm[:], in0=gm[:], scalar1=0.0, scalar2=BIG, op0=ALU.is_lt, op1=ALU.mult)
            T("dve", "tensor_tensor", ["lg", "gm"], ["e1"], out=e1[:, :].rearrange("p (g e) -> p g e", e=8), in0=lg[:, 4:36].rearrange("p (g e) -> p g e", e=8),
              in1=gm[:, :].unsqueeze(2).broadcast_to([128, 4, 8]), op=ALU.subtract)
            T("dve", "reduce_max", ["e1"], ["sm"], out=sm[:, 3:4], in_=e1[:], axis=AX.X)
            T("dve", "tensor_scalar", ["e1", "sm"], ["s1"], out=s1[:], in0=e1[:], scalar1=sm[:, 3:4], scalar2=None, op0=ALU.is_ge)
            T("dve", "scalar_tensor_tensor", ["s1", "e1"], ["e2"], out=e2[:], in0=s1[:], scalar=-BIG, in1=e1[:], op0=ALU.mult, op1=ALU.add)
            T("dve", "reduce_max", ["e2"], ["sm"], out=sm[:, 4:5], in_=e2[:], axis=AX.X)
            T("dve", "tensor_scalar", ["e2", "sm"], ["s2"], out=s2[:], in0=e2[:], scalar1=sm[:, 4:5], scalar2=None, op0=ALU.is_ge)
            T("dve", "tensor_tensor", ["sm"], ["sm"], out=sm[:, 5:6], in0=sm[:, 4:5], in1=sm[:, 3:4], op=ALU.subtract)
            T("act", "activation", ["sm"], ["sm"], out=sm[:, 6:7], in_=sm[:, 5:6], func=ACT.Exp)
            T("dve", "tensor_scalar", ["sm"], ["sm"], out=sm[:, 6:7], in0=sm[:, 6:7], scalar1=1.0, scalar2=None, op0=ALU.add)
            T("dve", "reciprocal", ["sm"], ["sm"], out=sm[:, 7:8], in_=sm[:, 6:7])
            T("dve", "tensor_tensor", ["sm"], ["sm"], out=sm[:, 8:9], in0=sm[:, 7:8], in1=sm[:, 2:3], op=ALU.mult)
            T("dve", "tensor_tensor", ["sm"], ["sm"], out=sm[:, 9:10], in0=sm[:, 2:3], in1=sm[:, 8:9], op=ALU.subtract)
            T("dve", "tensor_scalar", ["s1", "sm"], ["s1"], out=s1[:], in0=s1[:], scalar1=sm[:, 8:9], scalar2=None, op0=ALU.mult)
            T("dve", "scalar_tensor_tensor", ["s2", "sm", "s1"], ["Wt:%d" % i], out=Wt[:, i, :], in0=s2[:], scalar=sm[:, 9:10], in1=s1[:], op0=ALU.mult, op1=ALU.add)

        wfl = sbF("wfl", [128, 4096])
        wgs = wfl[:, :].rearrange("p (k n) -> p k n", n=512)
        wds = wfl[:, :].rearrange("p (k n) -> p k n", n=D)
        wgb = sbF("wgb", [128, 8, 512], BF16)
        wub = sbF("wub", [128, 8, 512], BF16)
        wdb = sbF("wdb", [128, 4, D], BF16)
        hmTs = [sbF("hmT0", [128, 4, LO], BF16), sbF("hmT1", [128, 4, LO], BF16)]
        sg = sbF("sg", [128, 512])

        def experts_gen():
          for ex in range(32):
            hmT, hmk = hmTs[ex % 2], "hmT%d" % (ex % 2)
            load(wgs, wgate[ex].rearrange("(k p) n -> p k n", p=128), "wgs")
            T("act", "activation", ["wgs"], ["wgb"], out=wgb[:], in_=wgs, func=ACT.Copy)
            load(wgs, wup[ex].rearrange("(k p) n -> p k n", p=128), "wgs")
            T("act", "activation", ["wgs"], ["wub"], out=wub[:], in_=wgs, func=ACT.Copy)
            load(wds, wdown[ex].rearrange("(k p) n -> p k n", p=128), "wgs")
            T("act", "activation", ["wgs"], ["wdb"], out=wdb[:], in_=wds, func=ACT.Copy)
            for tcn in range(4):
                xk4 = ["xnT:%d" % j for j in range(tcn * 4, tcn * 4 + 4)]
                for ft in range(4):
                    yield (4 * tcn + 4) if ex == 0 else NO
                    sl = slice(tcn * 512, (tcn + 1) * 512)
                    psg, pkg = nextps()
                    psu, pku = nextps()
                    for k in range(8):
                        T("pe", "matmul", xk4 + ["wgb"], [pkg], out=psg[:, :], lhsT=wgb[:, k, ft * 128:(ft + 1) * 128], rhs=xnT[:, k, sl], start=(k == 0), stop=(k == 7))
                    for k in range(8):
                        T("pe", "matmul", xk4 + ["wub"], [pku], out=psu[:, :], lhsT=wub[:, k, ft * 128:(ft + 1) * 128], rhs=xnT[:, k, sl], start=(k == 0), stop=(k == 7))
                    T("act", "activation", [pkg], ["sg"], out=sg[:], in_=psg[:, :], func=ACT.Silu)
                    T("dve", "tensor_tensor", ["sg", pku], [hmk], out=hmT[:, ft, sl], in0=sg[:], in1=psu[:, :], op=ALU.mult)
            for i in range(NO):
                yield NO
                for nh in range(2):
                    ps, pk = nextps()
                    for ft in range(4):
                        T("pe", "matmul", [hmk, "wdb"], [pk], out=ps[:, :], lhsT=hmT[:, ft, i * 128:(i + 1) * 128], rhs=wdb[:, ft, nh * 512:(nh + 1) * 512], start=(ft == 0), stop=(ft == 3))
                    T("dve", "scalar_tensor_tensor", [pk, "Wt:%d" % i, "X1:%d" % i], ["X1:%d" % i], out=X1[:, i, nh * 512:(nh + 1) * 512], in0=ps[:, :], scalar=Wt[:, i, ex:ex + 1],
                      in1=X1[:, i, nh * 512:(nh + 1) * 512], op0=ALU.mult, op1=ALU.add)

        routed = 0
        cnt_y = 0
        for need in experts_gen():
            while routed < need:
                route_tile(routed)
                routed += 1
            cnt_y += 1
            if routed < NO and cnt_y % 2 == 0:
                route_tile(routed)
                routed += 1
        S.barrier()
        phF.close()

        phG = ExitStack()
        sbG = lambda name, shape, dt=F32: phG.enter_context(nc.sbuf_tensor("s_" + name, list(shape), dt))
        wstE = sbG("wstG", [128, 8, 256])
        wbig = sbG("wbigG", [128, 8, D], BF16)
        load_w8(wpg)
        wps = sbG("wps", [128, 2, D])
        wpb = sbG("wpb", [128, 2, D], BF16)
        load(wps[:], wple.rearrange("(k p) n -> p k n", p=128), "wps")
        T("act", "activation", ["wps"], ["wpb"], out=wpb[:], in_=wps[:], func=ACT.Copy)
        pt_ = sbG("pt", [128, NO, 256])
        load(pt_[:], pb.rearrange("(n p) f -> p n f", p=128), "pt")
        pT = sbG("pT", [128, 2, LO], BF16)
        gt = sbG("gt", [128, 512])
        ot = sbG("ot", [128, D])
        for i in range(NO):
            norm_T(i, "g_ple", None, "X1", "xnT:%d" % i)
            ps, pk = nextps()
            for kq in range(2):
                T("pe", "transpose", ["pt", "ident"], [pk], out=ps[:, kq * 128:(kq + 1) * 128], in_=pt_[:, i, kq * 128:(kq + 1) * 128], identity=ident[:])
            T("act", "activation", [pk], ["pT"], out=pT[:, :, i * 128:(i + 1) * 128], in_=ps[:, 0:256].rearrange("p (k t) -> p k t", t=128), func=ACT.Copy)
        rowG = sbG("rowG", [128, 2048])
        load(rowG[:], rowv[:, ROWV["g_final"][0]:ROWV["g_final"][0] + 2048].partition_broadcast(128), "rowG")
        o_bpg = 1024
        for i in range(NO):
            for nh in range(2):
                sl = slice(nh * 512, (nh + 1) * 512)
                ps, pk = nextps()
                for k in range(8):
                    T("pe", "matmul", ["xnT:%d" % i, "wbig"], [pk], out=ps[:, :], lhsT=xnT[:, k, i * 128:(i + 1) * 128], rhs=wbig[:, k, sl], start=(k == 0), stop=(k == 7))
                T("dve", "tensor_tensor", [pk, "rowG"], ["gt"], out=gt[:], in0=ps[:, :], in1=rowG[:, o_bpg + nh * 512:o_bpg + (nh + 1) * 512], op=ALU.add)
                T("act", "activation", ["gt"], ["gt"], out=gt[:], in_=gt[:], func=ACT.Sigmoid)
                ps2, pk2 = nextps()
                for k in range(2):
                    T("pe", "matmul", ["pT", "wpb"], [pk2], out=ps2[:, :], lhsT=pT[:, k, i * 128:(i + 1) * 128], rhs=wpb[:, k, sl], start=(k == 0), stop=(k == 1))
                T("dve", "tensor_tensor", [pk2, "gt"], ["gt"], out=gt[:], in0=gt[:], in1=ps2[:, :], op=ALU.mult)
                T("dve", "tensor_tensor", ["gt", "X1"], ["X1"], out=X1[:, i, sl], in0=X1[:, i, sl], in1=gt[:], op=ALU.add)
            rstd_of(X1[:, i, :], D, "X1", rs[:], "rs")
            T("act", "activation", ["X1", "rs"], ["ot"], out=ot[:], in_=X1[:, i, :], func=ACT.Copy, scale=rs[:, 0:1])
            T("dve", "tensor_tensor", ["ot", "rowG"], ["ot"], out=ot[:], in0=ot[:], in1=rowG[:, 0:1024], op=ALU.mult)
            DMA("sp", ["ot"], ["out"], out=out_d[i * 128:(i + 1) * 128, :], in_=ot[:])
        S.final_wait("sp", ["out"])
        S.barrier()
        S.emit()
        phG.close()
        phX.close()
    return nc


def prep_inputs(inputs):
    c = host_consts()
    g = lambda k: np.asarray(inputs[k], dtype=np.float32)
    x = g("x"); p = g("p")[0]
    w_in = g("w_in")[0]
    conv_w = g("conv_w")[0]; conv_b = g("conv_b")[0]
    wf3 = g("w_f3")[0]
    in_maps = []
    for core in range(8):
        b, half = core // 2, core % 2
        rev = (half == 1)
        xb = x[b][::-1] if rev else x[b]
        pb = p[b][::-1][:LO] if rev else p[b][:LO]
        cw = conv_w[::-1] if rev else conv_w
        if rev:
            w3 = wf3.reshape(64, 2, 2, 512)[:, :, ::-1, :].reshape(64, 2048)
        else:
            w3 = wf3
        v128 = np.zeros((128, V128_N), np.float32)
        v128[:, 0:8] = g("g_mix")[0].reshape(8, 128).T
        v128[:, 8:16] = g("g_moe")[0].reshape(8, 128).T
        v128[:, 16:24] = g("g_ple")[0].reshape(8, 128).T
        v128[:, 24:60] = cw.reshape(3, 12, 128).transpose(2, 1, 0).reshape(128, 36)
        v128[:, 60:72] = conv_b.reshape(12, 128).T
        v128[:, 72:76] = g("g_hyena_out")[0].reshape(4, 128).T
        v64 = np.stack([g("b_f1")[0], g("freq1")[0], g("b_f2")[0], g("freq2")[0]], axis=1)
        rowv = np.concatenate([g("q_gain")[0], g("k_gain")[0], g("g_attn_out")[0], g("g_final"), g("b_ple_gate")[0],
                               g("b_group")[0], g("b_router")[0], g("filt_bias")[0].reshape(-1)])[None, :]
        m = {
            "xb": xb, "pb": pb, "w_in": w_in, "w_out": g("w_out")[0], "wgate": g("w_gate")[0], "wup": g("w_up")[0],
            "wdown": g("w_down")[0], "wpg": g("w_ple_gate")[0], "wple": g("w_ple")[0], "wf1": g("w_f1")[0], "wf2": g("w_f2")[0],
            "wf3": w3, "wr": np.concatenate([g("w_group")[0], g("w_router")[0]], axis=1), "v128": v128, "v64": v64, "rowv": rowv,
            "ropeC": c["ropeC"][::-1] if rev else c["ropeC"], "ropeS": c["ropeS"][::-1] if rev else c["ropeS"],
        }
        for k in ("ident", "bones", "ones32", "zT", "dec", "F64", "Gr", "Gi", "Wi1", "Wi2", "GiC"):
            m[k] = c[k]
        in_maps.append({k: np.ascontiguousarray(v) for k, v in m.items()})
    return in_maps


_NC = None


def kernel(**inputs):
    global _NC
    in_maps = prep_inputs(inputs)
    if _NC is None:
        _NC = build_nc()
    res = run_bass_kernel_spmd(_NC, in_maps, core_ids=list(range(8)))
    out = np.zeros((4, L, D), np.float32)
    for core in range(8):
        b, half = core // 2, core % 2
        o = np.asarray(res.results[core]["out"], dtype=np.float32)
        if half == 0:
            out[b, :LO] = o
        else:
            out[b, LO:] = o[::-1]
    return out
```

```python
import math
import numpy as np
import ml_dtypes
from contextlib import ExitStack
import concourse.bass as bass
import concourse.mybir as mybir
from concourse.bass_utils import run_bass_kernel_spmd

F32 = mybir.dt.float32
BF16 = mybir.dt.bfloat16
I32 = mybir.dt.int32
ACT = mybir.ActivationFunctionType
ALU = mybir.AluOpType
AX = mybir.AxisListType

ENGS = ("pe", "act", "dve", "pool", "sp")
L = 4096
D = 1024
NT = 32
NO = 16
LO = 2048
EPS = 1e-6
CC = 32
GK = 512 // (2 * CC)
SB = 512 // CC
NCH = 512 // CC
K1 = 33
STAGE = 99
SAME_ENG_FIFO = False


class Sched:
    SEM_LIMIT = 20000
    NDMA = 24

    def __init__(self, nc, stack):
        self.nc = nc
        self.stack = stack
        self.q = {e: [] for e in ENGS}
        self.cur_sem = {}
        self.cur_cnt = {}
        self.nsem = 0
        for e in ENGS:
            self._new_eng_sem(e)
        self.dma_sems = [self._sem("dma%d" % i) for i in range(self.NDMA)]
        self.dma_tgt = [0] * self.NDMA
        self.dma_k = 0
        self.last_w = {}
        self.readers = {}
        self.seen = {e: {} for e in ENGS}

    def _sem(self, name):
        self.nsem += 1
        return self.stack.enter_context(self.nc.semaphore(name))

    def _new_eng_sem(self, e):
        self.cur_sem[e] = self._sem("c_%s_%d" % (e, self.nsem))
        self.cur_cnt[e] = 0

    def _waits_for(self, eng, toks):
        out = []
        seen = self.seen[eng]
        for t in toks:
            if t is None:
                continue
            sem, val, teng = t
            if teng == eng and (eng == "pe" or SAME_ENG_FIFO):
                continue
            k = id(sem)
            if seen.get(k, 0) >= val:
                continue
            seen[k] = val
            out.append((sem, val))
        return out

    @staticmethod
    def _excl(reads, writes):
        r2 = [b for b in reads if not b.startswith("ps")]
        w2 = list(writes) + [b for b in reads if b.startswith("ps")]
        return r2, w2

    def _deps(self, reads, writes):
        toks = []
        for b in reads:
            toks.append(self.last_w.get(b))
        for b in writes:
            toks.append(self.last_w.get(b))
            toks.extend(self.readers.get(b, ()))
        return toks

    def _commit(self, tok, reads, writes):
        for b in reads:
            self.readers.setdefault(b, []).append(tok)
        for b in writes:
            self.last_w[b] = tok
            self.readers[b] = []

    def op(self, eng, fn, reads=(), writes=()):
        reads, writes = self._excl(reads, writes)
        toks = self._deps(reads, writes)
        waits = self._waits_for(eng, toks)
        if self.cur_cnt[eng] >= self.SEM_LIMIT:
            self._new_eng_sem(eng)
        self.cur_cnt[eng] += 1
        sem = self.cur_sem[eng]
        tok = (sem, self.cur_cnt[eng], eng)
        self.q[eng].append((waits, fn, sem, 1))
        self._commit(tok, reads, writes)
        return tok

    def dma(self, eng, fn, reads=(), writes=()):
        reads, writes = self._excl(reads, writes)
        toks = self._deps(reads, writes)
        i = self.dma_k % self.NDMA
        self.dma_k += 1
        sem = self.dma_sems[i]
        if self.dma_tgt[i] > 0:
            toks.append((sem, self.dma_tgt[i], "dma"))
        waits = self._waits_for(eng, toks)
        self.dma_tgt[i] += 16
        tok = (sem, self.dma_tgt[i], "dma")
        self.q[eng].append((waits, fn, sem, 16))
        self._commit(tok, reads, writes)
        return tok

    def barrier(self):
        toks = [(self.cur_sem[e], self.cur_cnt[e], e) for e in ENGS if self.cur_cnt[e] > 0]
        toks += [(self.dma_sems[i], self.dma_tgt[i], "dma") for i in range(self.NDMA) if self.dma_tgt[i] > 0]
        for e in ENGS:
            tk = toks
            waits = self._waits_for(e, tk)
            self.q[e].append((waits, None, None, 0))
        self.last_w = {}
        self.readers = {}

    def final_wait(self, eng, bufs):
        toks = [self.last_w.get(b) for b in bufs]
        waits = self._waits_for(eng, toks)
        self.q[eng].append((waits, None, None, 0))

    def emit(self):
        nc = self.nc
        q = self.q

        def run(engobj, lst):
            for waits, fn, sem, inc in lst:
                for (s, v) in waits:
                    engobj.wait_ge(s, v)
                if fn is not None:
                    meth, kw = fn
                    ins = getattr(engobj, meth)(**kw)
                    ins.then_inc(sem, inc)

        with nc.Block() as block:
            @block.tensor
            def _(e):
                run(e, q["pe"])

            @block.scalar
            def _(e):
                run(e, q["act"])

            @block.vector
            def _(e):
                run(e, q["dve"])

            @block.gpsimd
            def _(e):
                run(e, q["pool"])

            @block.sync
            def _(e):
                run(e, q["sp"])


def _bf(a):
    return np.ascontiguousarray(a.astype(np.float32)).astype(ml_dtypes.bfloat16)


_CONST = None


def host_consts():
    global _CONST
    if _CONST is not None:
        return _CONST
    c = {}
    c["ident"] = np.eye(128, dtype=np.float32)
    bo = np.zeros((128, 128), np.float32)
    bo[:64, :64] = 1.0
    bo[64:, 64:] = 1.0
    c["bones"] = bo
    c["ones32"] = np.ones((32, 32), np.float32)
    S = L
    rows = S // 64
    r_idx, c_idx = np.meshgrid(np.arange(rows, dtype=np.float32), np.arange(64, dtype=np.float32), indexing="ij")
    r_idx, c_idx = r_idx.reshape(S), c_idx.reshape(S)
    half = 32
    inv = (10000.0 ** (-np.arange(0, half, 2, dtype=np.float32) / half)).astype(np.float32)
    ang_r = (r_idx[:, None] * inv[None]).astype(np.float32)
    ang_c = (c_idx[:, None] * inv[None]).astype(np.float32)
    cr, sr, cc_, sc = np.cos(ang_r), np.sin(ang_r), np.cos(ang_c), np.sin(ang_c)
    c["ropeC"] = np.concatenate([cr, cr, cc_, cc_], axis=1).astype(np.float32)
    c["ropeS"] = np.concatenate([-sr, sr, -sc, sc], axis=1).astype(np.float32)
    bands = 16
    t = np.linspace(0.0, 1.0, L, dtype=np.float32)[:, None]
    w = ((2.0 * math.pi / L) * np.arange(L, dtype=np.float32))[:, None]
    f = np.linspace(1e-4, bands - 1, bands, dtype=np.float32)[None]
    z = (f * w).astype(np.float32)
    zz = np.concatenate([t, np.cos(z), -np.sin(z)], axis=-1).astype(np.float32)
    c["zT"] = np.ascontiguousarray(zz.T)
    max_decay = math.log(1e-2) / 0.3
    min_decay = math.log(1e-2) / 1.5
    deltas = np.linspace(min_decay, max_decay, 512, dtype=np.float32)
    dec = np.exp(-t * np.abs(deltas)[None]).astype(np.float32)
    c["dec"] = _bf(dec.reshape(32, 128, 512).transpose(0, 2, 1))
    a = np.arange(32)[:, None].astype(np.float64)
    k1 = np.arange(K1)[None, :].astype(np.float64)
    th = 2 * np.pi * a * k1 / 64.0
    c["F64"] = _bf(np.concatenate([np.cos(th), -np.sin(th)], axis=1))
    b = np.arange(128).astype(np.float64)
    kk = (np.arange(K1)[:, None] + 64 * np.arange(128)[None, :]).astype(np.float64)
    th = 2 * np.pi * b[:, None, None] * kk[None] / 8192.0
    c["Gr"] = _bf(np.cos(th).reshape(128, K1 * 128))
    c["Gi"] = _bf((-np.sin(th)).reshape(128, K1 * 128))
    k2 = np.arange(128).astype(np.float64)
    th = 2 * np.pi * k2[:, None] * b[None, :] / 128.0
    wr_, wi_ = np.cos(th), np.sin(th)
    c["Wi1"] = _bf(np.concatenate([wr_, wi_], axis=1))
    c["Wi2"] = _bf(np.concatenate([-wi_, wr_], axis=1))
    k1v = np.arange(K1).astype(np.float64)
    wgt = np.full(K1, 2.0); wgt[0] = 1.0; wgt[32] = 1.0
    av = np.arange(32).astype(np.float64)
    th = 2 * np.pi * (k1v[:, None, None] * av[None, None, :] / 64.0 + k1v[:, None, None] * b[None, :, None] / 8192.0)
    gr_ = (np.cos(th) * wgt[:, None, None] / 8192.0).reshape(K1, 128 * 32)
    gi_ = (-np.sin(th) * wgt[:, None, None] / 8192.0).reshape(K1, 128 * 32)
    c["GiC"] = _bf(np.concatenate([gr_, gi_], axis=0))
    _CONST = c
    return c


ROWV = {}
_off = 0
for _n, _l in (("q_gain", 64), ("k_gain", 64), ("g_attn", 512), ("g_final", 1024), ("b_pg", 1024), ("br", 36), ("fbias", 1024)):
    ROWV[_n] = (_off, _l)
    _off += _l
ROWV_N = _off
V128 = {"g_mix": (0, 8), "g_moe": (8, 8), "g_ple": (16, 8), "conv_w": (24, 36), "conv_b": (60, 12), "g_hy": (72, 4)}
V128_N = 76


def build_nc():
    nc = bass.Bass("TRN2", target_bir_lowering=False)

    def din(name, shape, dt=F32):
        return nc.dram_tensor(name, list(shape), dt, kind="ExternalInput").ap()

    xb = din("xb", [L, D])
    pb = din("pb", [LO, 256])
    w_in = din("w_in", [D, 2304])
    w_out = din("w_out", [D, D])
    if STAGE > 3:
        wgate = din("wgate", [32, D, 512])
        wup = din("wup", [32, D, 512])
        wdown = din("wdown", [32, 512, D])
    wpg = din("wpg", [D, D])
    wple = din("wple", [256, D])
    wf1 = din("wf1", [33, 64])
    wf2 = din("wf2", [64, 64])
    wf3 = din("wf3", [64, 2048])
    wr = din("wr", [D, 36])
    v128 = din("v128", [128, V128_N])
    v64 = din("v64", [64, 4])
    rowv = din("rowv", [1, ROWV_N])
    ident_d = din("ident", [128, 128])
    bones_d = din("bones", [128, 128])
    ones32_d = din("ones32", [32, 32])
    ropeC_d = din("ropeC", [L, 64])
    ropeS_d = din("ropeS", [L, 64])
    zT_d = din("zT", [33, L])
    dec_d = din("dec", [32, 512, 128], BF16)
    F64_d = din("F64", [32, 2 * K1], BF16)
    Gr_d = din("Gr", [128, K1 * 128], BF16)
    Gi_d = din("Gi", [128, K1 * 128], BF16)
    Wi1_d = din("Wi1", [128, 256], BF16)
    Wi2_d = din("Wi2", [128, 256], BF16)
    GiC_d = din("GiC", [2 * K1, 128 * 32], BF16)
    out_d = nc.dram_tensor("out", [LO, D], F32, kind="ExternalOutput").ap()
    ucd = nc.dram_tensor("ucd", [1536, L], BF16, kind="Internal").ap()
    yhd = nc.dram_tensor("yhd", [512, LO], BF16, kind="Internal").ap()

    with ExitStack() as st:
        S = Sched(nc, st)

        def sb(name, shape, dt=F32):
            return st.enter_context(nc.sbuf_tensor("s_" + name, list(shape), dt))

        PS = [st.enter_context(nc.psum_tensor("ps%d" % i, [128, 512], F32)) for i in range(8)]
        psk = [0]

        def nextps(lo=0, hi=8):
            i = lo + psk[0] % (hi - lo)
            psk[0] += 1
            return PS[i], "ps%d" % i

        def T(eng, meth, reads, writes, **kw):
            return S.op(eng, (meth, kw), list(reads), list(writes))

        def DMA(eng, reads, writes, **kw):
            return S.dma(eng, ("dma_start", kw), list(reads), list(writes))

        def load(dst, src, key, reads=()):
            DMA("sp", reads, [key], out=dst, in_=src)


        ident = sb("ident", [128, 128])
        load(ident[:], ident_d, "ident")
        v128t = sb("v128t", [128, V128_N])
        load(v128t[:], v128, "v128")
        v64t = sb("v64t", [64, 4])
        load(v64t[:], v64, "v64")
        RA = ROWV["g_final"][0]
        RB0 = ROWV["br"][0]
        RBN = ROWV_N - RB0
        rowt = sb("rowt", [128, RA + RBN])
        load(rowt[:, 0:RA], rowv[:, 0:RA].partition_broadcast(128), "rowt")
        load(rowt[:, RA:RA + RBN], rowv[:, RB0:ROWV_N].partition_broadcast(128), "rowt")

        def rcol(off):
            return off if off < RA else off - RB0 + RA
        epst = sb("epst", [128, 1])
        T("dve", "memset", [], ["epst"], ap=epst[:], constant=EPS)
        negpi = sb("negpi", [128, 1])
        T("dve", "memset", [], ["negpi"], ap=negpi[:], constant=-math.pi)
        junk = sb("junk", [128, 1024])
        ssq = sb("ssq", [128, 16])

        def rv(name):
            o, l = ROWV[name]
            return rowt[:, rcol(o):rcol(o) + l]

        def pv(name, j=0, n=1):
            o, l = V128[name]
            return v128t[:, o + j:o + j + n]

        def rstd_of(src_ap, width, key_src, dst_ap, key_dst):
            T("dve", "tensor_tensor", [key_src], ["junk"], out=junk[:, 0:width], in0=src_ap, in1=src_ap, op=ALU.mult)
            T("dve", "reduce_sum", ["junk"], ["ssq"], out=ssq[:, 0:1], in_=junk[:, 0:width], axis=AX.X)
            T("act", "activation", ["ssq", "epst"], ["ssq"], out=ssq[:, 1:2], in_=ssq[:, 0:1], func=ACT.Sqrt, bias=epst[:], scale=1.0 / width)
            T("dve", "reciprocal", ["ssq"], [key_dst], out=dst_ap, in_=ssq[:, 1:2])

        def norm_transpose(xt, xk, rs, rk, gname, dstT, tcol, tkey):
            rstd_of(xt[:], D, xk, rs[:], rk)
            T("act", "activation", [xk, rk], [xk], out=xt[:], in_=xt[:], func=ACT.Copy, scale=rs[:, 0:1])
            for g in range(2):
                ps, pk = nextps(0, 4)
                for kq in range(4):
                    k = g * 4 + kq
                    T("pe", "transpose", [xk, "ident"], [pk], out=ps[:, kq * 128:(kq + 1) * 128], in_=xt[:, k * 128:(k + 1) * 128], identity=ident[:])
                for kq in range(4):
                    k = g * 4 + kq
                    if kq % 2 == 0:
                        T("dve", "tensor_scalar", [pk, "v128"], [tkey], out=dstT[:, k, tcol:tcol + 128], in0=ps[:, kq * 128:(kq + 1) * 128],
                          scalar1=pv(gname, k), scalar2=None, op0=ALU.mult)
                    else:
                        T("act", "activation", [pk, "v128"], [tkey], out=dstT[:, k, tcol:tcol + 128], in_=ps[:, kq * 128:(kq + 1) * 128],
                          func=ACT.Copy, scale=pv(gname, k))

        mixd = nc.dram_tensor("mixd", [LO, 512], F32, kind="Internal").ap()
        ph1 = ExitStack()
        sb1 = lambda name, shape, dt=F32: ph1.enter_context(nc.sbuf_tensor("s_" + name, list(shape), dt))
        QTz = [sb1("QT0", [128, 4, LO], BF16), sb1("QT1", [128, 4, LO], BF16)]
        T("pool", "memset", [], ["QTz0"], ap=QTz[0][64:128, :, :], constant=0.0)
        T("pool", "memset", [], ["QTz1"], ap=QTz[1][0:64, :, :], constant=0.0)
        KT2 = sb1("KT2", [128, 2, L], BF16)
        Vx = sb1("Vx", [128, NT, 2, 65], BF16)
        T("pool", "memset", [], ["Vx"], ap=Vx[:], constant=1.0)
        phT = ExitStack()
        hT = phT.enter_context(nc.sbuf_tensor("s_hT", [128, 8, L], BF16))
        phA = ExitStack()
        NXB = 4
        xt2 = [phA.enter_context(nc.sbuf_tensor("s_xt%d" % i, [128, D], F32)) for i in range(NXB)]
        rs2 = [phA.enter_context(nc.sbuf_tensor("s_rs%d" % i, [128, 1], F32)) for i in range(NXB)]

        def A_tile(i):
            xt, xk = xt2[i % NXB], "xt%d" % (i % NXB)
            load(xt[:], xb[i * 128:(i + 1) * 128, :], xk)
            norm_transpose(xt, xk, rs2[i % NXB], "rs%d" % (i % NXB), "g_mix", hT, i * 128, "hT:%d" % i)

        phB = ExitStack()
        sbB = lambda name, shape, dt=F32: phB.enter_context(nc.sbuf_tensor("s_" + name, list(shape), dt))
        wst = sbB("wst", [128, 8, 256])
        wqkv = sbB("wqkv", [128, 8, 768], BF16)
        for j in range(3):
            load(wst[:], w_in[:, j * 256:(j + 1) * 256].rearrange("(k p) n -> p k n", p=128), "wst")
            T("pool", "tensor_copy", ["wst"], ["wqkv"], out=wqkv[:, :, j * 256:(j + 1) * 256], in_=wst[:])
        ropeC = sbB("ropeC", [128, NT, 64])
        ropeS = sbB("ropeS", [128, NT, 64])
        load(ropeC[:], ropeC_d.rearrange("(n p) f -> p n f", p=128), "ropeC")
        load(ropeS[:], ropeS_d.rearrange("(n p) f -> p n f", p=128), "ropeS")
        qk = sbB("qk", [128, 10, 64])
        qk2 = sbB("qk2", [128, 10, 64])
        qk3 = sbB("qk3", [128, 12, 64])
        hs10 = sbB("hs10", [128, 10])

        def B1_tile(i):
            own = i < NO
            nh = 10 if own else 2
            h0 = 0 if own else 8
            hk = ["hT:%d" % i, "wqkv"]
            ps2, pk2 = nextps(0, 4)
            if own:
                ps, pk = nextps(0, 4)
                for k in range(8):
                    T("pe", "matmul", hk, [pk], out=ps[:, 0:512], lhsT=hT[:, k, i * 128:(i + 1) * 128], rhs=wqkv[:, k, 0:512], start=(k == 0), stop=(k == 7))
            for k in range(8):
                T("pe", "matmul", hk, [pk2], out=ps2[:, 0:256], lhsT=hT[:, k, i * 128:(i + 1) * 128], rhs=wqkv[:, k, 512:768], start=(k == 0), stop=(k == 7))
            if own:
                T("act", "activation", [pk], ["qk"], out=qk[:, 0:8, :], in_=ps[:, 0:512].rearrange("p (h d) -> p h d", d=64), func=ACT.Copy)
            T("act", "activation", [pk2], ["qk"], out=qk[:, 8:10, :], in_=ps2[:, 0:128].rearrange("p (h d) -> p h d", d=64), func=ACT.Copy)
            T("dve", "tensor_copy", [pk2], ["Vx"], out=Vx[:, i, :, 0:64], in_=ps2[:, 128:256].rearrange("p (h d) -> p h d", d=64))
            T("dve", "tensor_tensor", ["qk"], ["qk2"], out=qk2[:, h0:10, :], in0=qk[:, h0:10, :], in1=qk[:, h0:10, :], op=ALU.mult)
            T("dve", "reduce_sum", ["qk2"], ["hs10"], out=hs10[:, h0:10], in_=qk2[:, h0:10, :], axis=AX.X)
            T("act", "activation", ["hs10", "epst"], ["hs10"], out=hs10[:, h0:10], in_=hs10[:, h0:10], func=ACT.Sqrt, bias=epst[:], scale=1.0 / 64)
            T("dve", "reciprocal", ["hs10"], ["hs10"], out=hs10[:, h0:10], in_=hs10[:, h0:10])
            T("dve", "tensor_tensor", ["qk", "hs10"], ["qk"], out=qk[:, h0:10, :], in0=qk[:, h0:10, :],
              in1=hs10[:, h0:10].unsqueeze(2).broadcast_to([128, nh, 64]), op=ALU.mult)
            if own:
                T("dve", "tensor_tensor", ["qk", "rowt"], ["qk"], out=qk[:, 0:8, :], in0=qk[:, 0:8, :],
                  in1=rv("q_gain").unsqueeze(1).broadcast_to([128, 8, 64]), op=ALU.mult)
            T("dve", "tensor_tensor", ["qk", "rowt"], ["qk"], out=qk[:, 8:10, :], in0=qk[:, 8:10, :],
              in1=rv("k_gain").unsqueeze(1).broadcast_to([128, 2, 64]), op=ALU.mult)
            v5 = qk[:, h0:10, :].rearrange("p h (g t n) -> p h g t n", g=2, t=2, n=16)
            w5 = qk2[:, h0:10, :].rearrange("p h (g t n) -> p h g t n", g=2, t=2, n=16)
            T("pool", "tensor_copy", ["qk"], ["qk2"], out=w5[:, :, :, 0, :], in_=v5[:, :, :, 1, :])
            T("pool", "tensor_copy", ["qk"], ["qk2"], out=w5[:, :, :, 1, :], in_=v5[:, :, :, 0, :])
            T("dve", "tensor_tensor", ["qk", "ropeC"], ["qk"], out=qk[:, h0:10, :], in0=qk[:, h0:10, :],
              in1=ropeC[:, i, :].unsqueeze(1).broadcast_to([128, nh, 64]), op=ALU.mult)
            T("dve", "tensor_tensor", ["qk2", "ropeS"], ["qk2"], out=qk2[:, h0:10, :], in0=qk2[:, h0:10, :],
              in1=ropeS[:, i, :].unsqueeze(1).broadcast_to([128, nh, 64]), op=ALU.mult)
            if own:
                T("dve", "tensor_tensor", ["qk", "qk2"], ["qk3"], out=qk3[:, 0:8, :], in0=qk[:, 0:8, :], in1=qk2[:, 0:8, :], op=ALU.add)
            for kv in range(2):
                for dup in range(2):
                    T("dve", "tensor_tensor", ["qk", "qk2"], ["qk3"], out=qk3[:, 8 + 2 * kv + dup, :], in0=qk[:, 8 + kv, :], in1=qk2[:, 8 + kv, :], op=ALU.add)
            if own:
                ps, pk = nextps(0, 4)
                for blk in range(4):
                    T("pe", "transpose", ["qk3", "ident"], [pk], out=ps[:, blk * 128:(blk + 1) * 128],
                      in_=qk3[:, 2 * blk:2 * blk + 2, :].rearrange("p h d -> p (h d)"), identity=ident[:])
                T("act", "activation", [pk, "QTz0"], ["QT:%d" % i], out=QTz[0][0:64, :, i * 128:(i + 1) * 128], in_=ps[0:64, 0:512].rearrange("p (h t) -> p h t", t=128), func=ACT.Copy)
                T("act", "activation", [pk, "QTz1"], ["QT:%d" % i], out=QTz[1][64:128, :, i * 128:(i + 1) * 128], in_=ps[64:128, 0:512].rearrange("p (h t) -> p h t", t=128), func=ACT.Copy)
            ps3, pk3 = nextps(0, 4)
            for kv in range(2):
                T("pe", "transpose", ["qk3", "ident"], [pk3], out=ps3[:, kv * 128:(kv + 1) * 128],
                  in_=qk3[:, 8 + 2 * kv:10 + 2 * kv, :].rearrange("p h d -> p (h d)"), identity=ident[:])
            T("act", "activation", [pk3], ["KT2:%d" % i], out=KT2[:, :, i * 128:(i + 1) * 128], in_=ps3[:, 0:256].rearrange("p (h t) -> p h t", t=128), func=ACT.Copy)
        A_tile(0)
        A_tile(1)
        for i in range(NT):
            if i + 2 < NT:
                A_tile(i + 2)
            B1_tile(i)
        S.barrier()
        phB.close()
        phA.close()
        phB2 = ExitStack()
        sbB2 = lambda name, shape, dt=F32: phB2.enter_context(nc.sbuf_tensor("s_" + name, list(shape), dt))
        wst2 = sbB2("wst2", [128, 8, 128])
        wct = [sbB2("wct0", [128, 8, 128], BF16), sbB2("wct1", [128, 8, 128], BF16)]
        U = sbB2("U", [128, L + 2])
        T("pool", "memset", [], ["U"], ap=U[:], constant=0.0)
        HL = L // 2
        uc32 = sbB2("uc32", [128, HL])
        ucb = sbB2("ucb", [128, L], BF16)
        PT = [sbB2("PT0", [128, 512], BF16), sbB2("PT1", [128, 512], BF16), sbB2("PT2", [128, 512], BF16)]
        STB = [0, 1, 7]
        rden = sbB2("rden", [128, 4])
        accS = [sbB2("accS0", [65, 512]), sbB2("accS1", [65, 512])]
        stg = [sbB2("stg0", [128, 4, 64]), sbB2("stg1", [128, 4, 64])]
        mixv = mixd.rearrange("(n p) f -> p n f", p=128)

        def b2_gen():
            for ct in range(12):
                wc, wk = wct[ct % 2], "wct%d" % (ct % 2)
                load(wst2[:], w_in[:, 768 + ct * 128:768 + (ct + 1) * 128].rearrange("(k p) n -> p k n", p=128), "wst2")
                T("pool", "tensor_copy", ["wst2"], [wk], out=wc[:], in_=wst2[:])
                for tcn in range(8):
                    ps, pk = nextps(2, 4)
                    for k in range(8):
                        T("pe", "matmul", [wk] + ["hT:%d" % j for j in range(tcn * 4, tcn * 4 + 4)], [pk], out=ps[:, :], lhsT=wc[:, k, :],
                          rhs=hT[:, k, tcn * 512:(tcn + 1) * 512], start=(k == 0), stop=(k == 7))
                    T("dve", "tensor_copy", [pk], ["U"], out=U[:, 1 + tcn * 512:1 + (tcn + 1) * 512], in_=ps[:, :])
                    yield
                for hh in range(2):
                    o = hh * HL
                    T("dve", "tensor_scalar", ["U", "v128"], ["uc32"], out=uc32[:], in0=U[:, o:o + HL], scalar1=pv("conv_w", ct * 3 + 0), scalar2=pv("conv_b", ct), op0=ALU.mult, op1=ALU.add)
                    T("dve", "scalar_tensor_tensor", ["U", "v128", "uc32"], ["uc32"], out=uc32[:], in0=U[:, o + 1:o + HL + 1], scalar=pv("conv_w", ct * 3 + 1), in1=uc32[:], op0=ALU.mult, op1=ALU.add)
                    T("dve", "scalar_tensor_tensor", ["U", "v128", "uc32"], ["ucb"], out=ucb[:, o:o + HL], in0=U[:, o + 2:o + HL + 2], scalar=pv("conv_w", ct * 3 + 2), in1=uc32[:], op0=ALU.mult, op1=ALU.add)
                    yield
                DMA("sp", ["ucb"], ["ucd"], out=ucd[ct * 128:(ct + 1) * 128, :], in_=ucb[:])

        scale = 64 ** -0.5
        allQT = ["QT:%d" % i for i in range(NO)]
        iters = []
        for hp in range(4):
            for j in range(2):
                for qg in range(4):
                    for kt in range(NT):
                        iters.append((hp, j, qg, kt))

        def emit_st(n):
            hp, j, qg, kt = iters[n]
            kv = hp // 2
            T("pe", "matmul", ["KT2:%d" % kt] + allQT[qg * 4:qg * 4 + 4], ["ps%d" % STB[n % 3]], out=PS[STB[n % 3]][:, :], lhsT=KT2[:, kv, kt * 128:(kt + 1) * 128],
              rhs=QTz[j][:, hp, qg * 512:(qg + 1) * 512], start=True, stop=True)

        def attn_gen():
            emit_st(0)
            emit_st(1)
            ng = 0
            for n in range(len(iters)):
                hp, j, qg, kt = iters[n]
                kv = hp // 2
                h = 2 * hp + j
                pss, pks = PS[STB[n % 3]], "ps%d" % STB[n % 3]
                pt, ptk = PT[n % 3], "PT%d" % (n % 3)
                T("act", "activation", [pks], [ptk], out=pt[:], in_=pss[:, :], func=ACT.Exp, scale=scale)
                if n + 2 < len(iters):
                    emit_st(n + 2)
                gi_ = n // NT
                accb = 4 + (gi_ % 2)
                T("pe", "matmul", [ptk, "Vx"], ["ps%d" % accb], out=PS[accb][0:65, :], lhsT=Vx[:, kt, kv, :], rhs=pt[:, :],
                  start=(kt == 0), stop=(kt == NT - 1))
                if kt == NT - 1:
                    sg_, sgk = stg[ng % 2], "stg%d" % (ng % 2)
                    ac_, ack = accS[ng % 2], "accS%d" % (ng % 2)
                    trb = 6
                    ng += 1
                    T("act", "activation", ["ps%d" % accb], [ack], out=ac_[:, :], in_=PS[accb][0:65, :], func=ACT.Copy)
                    for qi in range(4):
                        T("pe", "transpose", [ack, "ident"], ["ps%d" % trb], out=PS[trb][:, qi * 65:(qi + 1) * 65], in_=ac_[:, qi * 128:(qi + 1) * 128], identity=ident[0:65, 0:65])
                    for qi in range(4):
                        T("dve", "reciprocal", ["ps%d" % trb], ["rden"], out=rden[:, qi:qi + 1], in_=PS[trb][:, qi * 65 + 64:qi * 65 + 65])
                        T("dve", "tensor_scalar", ["ps%d" % trb, "rden"], [sgk], out=sg_[:, qi, :], in0=PS[trb][:, qi * 65:qi * 65 + 64],
                          scalar1=rden[:, qi:qi + 1], scalar2=None, op0=ALU.mult)
                    DMA("sp", [sgk], ["mixd"], out=mixv[:, qg * 4:(qg + 1) * 4, h * 64:(h + 1) * 64], in_=sg_[:])
                yield

        ga, gb = attn_gen(), b2_gen()
        da = db = False
        na_ = 0
        while not (da and db):
            if not da:
                try:
                    next(ga)
                    na_ += 1
                except StopIteration:
                    da = True
            if not db and (da or na_ % 8 == 0):
                try:
                    next(gb)
                except StopIteration:
                    db = True
        S.barrier()
        phB2.close()
        phT.close()
        ph1.close()
        TWO_PI = 2.0 * math.pi
        phD = ExitStack()
        sbD = lambda name, shape, dt=F32: phD.enter_context(nc.sbuf_tensor("s_" + name, list(shape), dt))
        F64 = sbD("F64", [32, 2 * K1], BF16); load(F64[:], F64_d, "F64")
        Gr = sbD("Gr", [128, K1, 128], BF16); load(Gr[:], Gr_d.rearrange("p (k m) -> p k m", m=128), "Gr")
        Gi = sbD("Gi", [128, K1, 128], BF16); load(Gi[:], Gi_d.rearrange("p (k m) -> p k m", m=128), "Gi")
        Wi1 = sbD("Wi1", [128, 256], BF16); load(Wi1[:], Wi1_d, "Wi1")
        GiC = sbD("GiC", [2 * K1, 128, 32], BF16)
        DMA("sp", [], ["GiC"], out=GiC[:, :, :], in_=GiC_d.rearrange("p (b a) -> p b a", a=32))
        h2T = sbD("h2T", [64, L], BF16)
        phM = ExitStack()
        sbM = lambda name, shape, dt=F32: phM.enter_context(nc.sbuf_tensor("s_" + name, list(shape), dt))
        zT = sbM("zT", [33, L]); load(zT[:], zT_d, "zT")
        wf1t = sbM("wf1t", [33, 64]); load(wf1t[:], wf1, "wf1t")
        wf2t = sbM("wf2t", [64, 64]); load(wf2t[:], wf2, "wf2t")
        h1T = sbM("h1T", [64, L])
        mtmp = sbM("mtmp", [64, 512])
        mti = sbM("mti", [64, 512], I32)
        mtf = sbM("mtf", [64, 512])
        for (wt_, wkey, src, skey, dst, dkey, bcol) in ((wf1t, "wf1t", zT, "zT", h1T, "h1T", 0), (wf2t, "wf2t", h1T, "h1T", h2T, "h2T", 2)):
            for tcn in range(8):
                ps, pk = nextps()
                T("pe", "matmul", [wkey, skey], [pk], out=ps[0:64, :], lhsT=wt_[:, :], rhs=src[:, tcn * 512:(tcn + 1) * 512], start=True, stop=True)
                T("dve", "tensor_scalar", [pk, "v64"], ["mtmp"], out=mtmp[:], in0=ps[0:64, :], scalar1=v64t[:, bcol:bcol + 1], scalar2=v64t[:, bcol + 1:bcol + 2], op0=ALU.add, op1=ALU.mult)
                T("dve", "tensor_scalar", ["mtmp"], ["mti"], out=mti[:], in0=mtmp[:], scalar1=1.0 / TWO_PI, scalar2=None, op0=ALU.mult)
                T("dve", "tensor_copy", ["mti"], ["mtf"], out=mtf[:], in_=mti[:])
                T("dve", "scalar_tensor_tensor", ["mtf", "mtmp"], ["mtmp"], out=mtmp[:], in0=mtf[:], scalar=-TWO_PI, in1=mtmp[:], op0=ALU.mult, op1=ALU.add)
                T("dve", "tensor_scalar", ["mtmp"], ["mtf"], out=mtf[:], in0=mtmp[:], scalar1=math.pi, scalar2=-TWO_PI, op0=ALU.is_gt, op1=ALU.mult)
                T("dve", "tensor_tensor", ["mtmp", "mtf"], ["mtmp"], out=mtmp[:], in0=mtmp[:], in1=mtf[:], op=ALU.add)
                T("dve", "tensor_scalar", ["mtmp"], ["mtf"], out=mtf[:], in0=mtmp[:], scalar1=-math.pi, scalar2=TWO_PI, op0=ALU.is_lt, op1=ALU.mult)
                T("dve", "tensor_tensor", ["mtmp", "mtf"], ["mtmp"], out=mtmp[:], in0=mtmp[:], in1=mtf[:], op=ALU.add)
                T("act", "activation", ["mtmp"], [dkey], out=dst[:, tcn * 512:(tcn + 1) * 512], in_=mtmp[:], func=ACT.Sin)
        S.barrier()
        phM.close()
        onesB = sbD("onesB", [32, 128])
        T("pool", "memset", [], ["onesB"], ap=onesB[:], constant=1.0)

        def make_pipe(pid, chunks):
            def K(k):
                return "%s_p%d" % (k, pid)

            w3s = sbD("w3s_%d" % pid, [64, 2, CC])
            w3c = sbD("w3c_%d" % pid, [64, 2, CC], BF16)
            dect = sbD("dect_%d" % pid, [32, CC, 128], BF16)
            hs = sbD("hs_%d" % pid, [32, 2, CC, 128], BF16)
            l1 = sbD("l1_%d" % pid, [32, 2 * CC])
            AplG = sbD("AplG_%d" % pid, [128, K1, 3, CC], BF16)
            AplC = sbD("AplC_%d" % pid, [128, K1, 3, CC], BF16)
            Xt = sbD("Xt_%d" % pid, [128, K1, 2, CC], BF16)
            Yt = sbD("Yt_%d" % pid, [128, 3, K1, CC], BF16)
            rinvB = sbD("rinvB_%d" % pid, [128, 2, CC])
            rbs = sbD("rbs_%d" % pid, [128, 2, CC])
            tmpG = sbD("tmpG_%d" % pid, [128, GK, 2, CC], BF16)
            Hts = [sbD("Ht0_%d" % pid, [128, K1, 2, CC], BF16), sbD("Ht1_%d" % pid, [128, K1, 2, CC], BF16)]
            Dt = sbD("Dt_%d" % pid, [2 * K1, 128, CC], BF16)
            v3 = sbD("v3_%d" % pid, [32, CC, 128], BF16)
            xg = sbD("xg_%d" % pid, [32, CC, 128], BF16)
            z1 = v3
            h2v = h2T[:, :].rearrange("p (a b) -> p a b", b=128)

            def fdft(src3, skey, mode, Apl, akey, Ht=None, hkey=None, mid_hook=None):
                for cg in range(CC // 4):
                    ps, pk = nextps()
                    for cq in range(4):
                        c = cg * 4 + cq
                        T("pe", "matmul", [skey, "F64"], [pk], out=ps[:, cq * 2 * K1:(cq + 1) * 2 * K1], lhsT=src3[:, c, :], rhs=F64[:, :], start=True, stop=True)
                    pv4 = ps[:, 0:8 * K1].rearrange("p (c r k) -> p k r c", c=4, r=2, k=K1)
                    T("act", "activation", [pk], [akey], out=Apl[:, :, 1:3, cg * 4:cg * 4 + 4], in_=pv4, func=ACT.Copy)
                    T("act", "activation", [pk], [akey], out=Apl[:, :, 0, cg * 4:cg * 4 + 4], in_=pv4[:, :, 1, :], func=ACT.Copy, scale=-1.0)
                    if cg % 2 == 1:
                        yield
                hooks = list(mid_hook) if mid_hook is not None else []
                for kg in range((K1 + GK - 1) // GK):
                    ps, pk = nextps()
                    nk = min(GK, K1 - kg * GK)
                    for kq in range(nk):
                        k1 = kg * GK + kq
                        off = kq * 2 * CC
                        T("pe", "matmul", [akey, "Gr"], [pk], out=ps[:, off:off + 2 * CC], lhsT=Gr[:, k1, :], rhs=Apl[:, k1, 1:3, :].rearrange("p r c -> p (r c)"), start=True, stop=False)
                        T("pe", "matmul", [akey, "Gi"], [pk], out=ps[:, off:off + 2 * CC], lhsT=Gi[:, k1, :], rhs=Apl[:, k1, 0:2, :].rearrange("p r c -> p (r c)"), start=False, stop=True)
                    pv3 = ps[:, 0:nk * 2 * CC].rearrange("p (k r c) -> p k r c", k=nk, r=2, c=CC)
                    if mode == "X":
                        T("act", "activation", [pk], [K("Xt")], out=Xt[:, kg * GK:kg * GK + nk, :, :], in_=pv3, func=ACT.Copy)
                    elif mode == "Hset":
                        T("act", "activation", [pk], [hkey], out=Ht[:, kg * GK:kg * GK + nk, :, :], in_=pv3, func=ACT.Copy)
                    else:
                        T("dve", "tensor_tensor", [pk, K("rbs")], [K("tmpG")], out=tmpG[:, 0:nk, :, :], in0=pv3, in1=rbs[:, :, :].unsqueeze(1).broadcast_to([128, nk, 2, CC]), op=ALU.mult)
                        T("dve", "tensor_tensor", [K("tmpG"), hkey], [hkey], out=Ht[:, kg * GK:kg * GK + nk, :, :], in0=Ht[:, kg * GK:kg * GK + nk, :, :], in1=tmpG[:, 0:nk, :, :], op=ALU.add)
                    if hooks:
                        hooks.pop(0)()
                    if kg % 2 == 1:
                        yield
                while hooks:
                    hooks.pop(0)()

            def gen_H(o, c0, Ht, hkey):
                for dr in range(2):
                    col = o * 1024 + dr * 512 + c0
                    DMA("pool", [], [K("w3s")], out=w3s[:, dr, :], in_=wf3[:, col:col + CC])
                DMA("pool", [], [K("dect")], out=dect[:], in_=dec_d[:, c0:c0 + CC, :])
                T("pool", "tensor_copy", [K("w3s")], [K("w3c")], out=w3c[:], in_=w3s[:])
                for bg in range(128 // GK):
                    ps, pk = nextps()
                    for bq in range(GK):
                        b = bg * GK + bq
                        T("pe", "matmul", ["h2T", K("w3c")], [pk], out=ps[0:32, bq * 2 * CC:(bq + 1) * 2 * CC], lhsT=h2v[:, :, b], rhs=w3c[:, :, :].rearrange("p g c -> p (g c)"), start=True, stop=True)
                    T("dve", "tensor_tensor", [pk, K("dect")], [K("hs")], out=hs[:, :, :, bg * GK:(bg + 1) * GK], in0=ps[0:32, :].rearrange("p (b g c) -> p g c b", b=GK, g=2, c=CC),
                      in1=dect[:, :, bg * GK:(bg + 1) * GK].unsqueeze(1).broadcast_to([32, 2, CC, GK]), op=ALU.mult)
                    if bg % 4 == 3:
                        yield
                def l1_piece(g, hf):
                    def f():
                        T("dve", "tensor_reduce", [K("hs")], [K("l1")], out=l1[:, g * CC + hf * (CC // 2):g * CC + (hf + 1) * (CC // 2)], in_=hs[:, g, hf * (CC // 2):(hf + 1) * (CC // 2), :], axis=AX.X, op=ALU.add, apply_absolute_value=True)
                    return f

                def l1_fin():
                    ps, pk = nextps()
                    T("pe", "matmul", [K("l1"), "onesB"], [pk], out=ps[:, 0:2 * CC], lhsT=onesB[:, :], rhs=l1[:, :], start=True, stop=True)
                    T("dve", "tensor_scalar", [pk], [K("rinvB")], out=rinvB[:, :, :].rearrange("p g c -> p (g c)"), in0=ps[:, 0:2 * CC], scalar1=EPS, scalar2=None, op0=ALU.add)
                    T("dve", "reciprocal", [K("rinvB")], [K("rinvB")], out=rinvB[:, :, :], in_=rinvB[:, :, :])
                    T("dve", "tensor_copy", [K("rinvB")], [K("rbs")], out=rbs[:, 0, :], in_=rinvB[:, 1, :])
                    T("dve", "tensor_scalar", [K("rinvB")], [K("rbs")], out=rbs[:, 1, :], in0=rinvB[:, 1, :], scalar1=-1.0, scalar2=None, op0=ALU.mult)

                l1_hooks = [l1_piece(0, 0), l1_piece(0, 1), l1_piece(1, 0), l1_piece(1, 1), l1_fin]
                yield from fdft(hs[:, 0, :, :], K("hs"), "Hset", AplG, K("AplG"), Ht, hkey, mid_hook=l1_hooks)
                T("dve", "tensor_tensor", [hkey, K("rinvB")], [hkey], out=Ht[:, :, :, :], in0=Ht[:, :, :, :],
                  in1=rinvB[:, 0:1, :].unsqueeze(1).broadcast_to([128, K1, 2, CC]), op=ALU.mult)
                yield from fdft(hs[:, 1, :, :], K("hs"), "Hconj", AplG, K("AplG"), Ht, hkey)
                fo = rcol(ROWV["fbias"][0]) + o * 512 + c0
                T("dve", "tensor_tensor", [hkey, "rowt"], [hkey], out=Ht[:, :, 0, :], in0=Ht[:, :, 0, :], in1=rowt[:, fo:fo + CC].unsqueeze(1).broadcast_to([128, K1, CC]), op=ALU.add)
                yield

            def conv(src3, skey, na, dst3, dkey, Ht, hkey):
                yield from fdft(src3, skey, "X", AplC, K("AplC"))
                tA, tB = Yt[:, 0, :, :], AplC[:, :, 0, :]
                T("dve", "tensor_tensor", [K("Xt"), hkey], [K("Yt")], out=tA, in0=Xt[:, :, 0, :], in1=Ht[:, :, 0, :], op=ALU.mult)
                T("dve", "tensor_tensor", [K("Xt"), hkey], [K("AplC")], out=tB, in0=Xt[:, :, 1, :], in1=Ht[:, :, 1, :], op=ALU.mult)
                T("dve", "tensor_tensor", [K("Yt"), K("AplC")], [K("Yt")], out=Yt[:, 1, :, :], in0=tA, in1=tB, op=ALU.subtract)
                yield
                T("dve", "tensor_tensor", [K("Xt"), hkey], [K("Yt")], out=tA, in0=Xt[:, :, 0, :], in1=Ht[:, :, 1, :], op=ALU.mult)
                T("dve", "tensor_tensor", [K("Xt"), hkey], [K("AplC")], out=tB, in0=Xt[:, :, 1, :], in1=Ht[:, :, 0, :], op=ALU.mult)
                T("dve", "tensor_tensor", [K("Yt"), K("AplC")], [K("Yt")], out=Yt[:, 2, :, :], in0=tA, in1=tB, op=ALU.add)
                T("act", "activation", [K("Yt")], [K("Yt")], out=Yt[:, 0, :, :], in_=Yt[:, 2, :, :], func=ACT.Copy, scale=-1.0)
                yield
                for cg in range(CC // 4):
                    ps, pk = nextps()
                    for cq in range(4):
                        c = cg * 4 + cq
                        T("pe", "matmul", [K("Yt"), "Wi1"], [pk], out=ps[0:2 * K1, cq * 128:(cq + 1) * 128], lhsT=Yt[:, 1:3, :, c].rearrange("p r k -> p (r k)"), rhs=Wi1[:, 0:128], start=True, stop=False)
                        T("pe", "matmul", [K("Yt"), "Wi1"], [pk], out=ps[0:2 * K1, cq * 128:(cq + 1) * 128], lhsT=Yt[:, 0:2, :, c].rearrange("p r k -> p (r k)"), rhs=Wi1[:, 128:256], start=False, stop=True)
                    T("act", "activation", [pk], [K("Dt")], out=Dt[:, :, cg * 4:cg * 4 + 4], in_=ps[0:2 * K1, :].rearrange("p (c b) -> p b c", c=4, b=128), func=ACT.Copy)
                    if cg % 2 == 1:
                        yield
                for bg in range(128 // SB):
                    ps, pk = nextps()
                    for bq in range(SB):
                        b = bg * SB + bq
                        T("pe", "matmul", [K("Dt"), "GiC"], [pk], out=ps[0:na, bq * CC:(bq + 1) * CC], lhsT=GiC[:, b, 0:na], rhs=Dt[:, b, :], start=True, stop=True)
                    T("dve", "tensor_tensor", [pk, K("xg")], [dkey], out=dst3[0:na, :, bg * SB:(bg + 1) * SB], in0=ps[0:na, :].rearrange("p (b c) -> p c b", b=SB, c=CC),
                      in1=xg[0:na, :, bg * SB:(bg + 1) * SB], op=ALU.mult)
                    if bg % 2 == 1:
                        yield

            def conv_step2(ch, o, Ht, hkey):
                c0 = ch * CC
                if o == 0:
                    DMA("sp", ["ucd"], [K("v3")], out=v3[:], in_=ucd[c0:c0 + CC, :].rearrange("c (a b) -> a c b", b=128))
                    DMA("sp", ["ucd"], [K("xg")], out=xg[:], in_=ucd[512 + c0:512 + c0 + CC, :].rearrange("c (a b) -> a c b", b=128))
                    yield from conv(v3, K("v3"), 32, v3, K("v3"), Ht, hkey)
                else:
                    DMA("sp", ["ucd"], [K("xg")], out=xg[0:16, :, :], in_=ucd[1024 + c0:1024 + c0 + CC, 0:LO].rearrange("c (a b) -> a c b", b=128))
                    yield from conv(v3, K("v3"), 16, v3, K("v3"), Ht, hkey)
                    DMA("sp", [K("v3")], [K("yhd")], out=yhd[c0:c0 + CC, :].rearrange("c (a b) -> a c b", b=128), in_=v3[0:16, :, :])

            def run():
                steps = [(ch, o) for ch in chunks for o in range(2)]

                def gstep(j):
                    ch, o = steps[j]
                    yield from gen_H(o, ch * CC, Hts[j % 2], K("Ht%d" % (j % 2)))

                def cstep(j):
                    ch, o = steps[j]
                    yield from conv_step2(ch, o, Hts[j % 2], K("Ht%d" % (j % 2)))

                for _ in gstep(0):
                    yield
                for j in range(len(steps)):
                    gc = cstep(j)
                    gg = gstep(j + 1) if j + 1 < len(steps) else iter(())
                    dc = dg = False
                    while not (dc and dg):
                        if not dg:
                            try:
                                next(gg)
                            except StopIteration:
                                dg = True
                        if not dc:
                            try:
                                next(gc)
                            except StopIteration:
                                dc = True
                        yield
            return run()

        pipes = [make_pipe(0, list(range(0, NCH, 2))), make_pipe(1, list(range(1, NCH, 2)))]
        alive = [True, True]
        while any(alive):
            for pi_ in range(2):
                if alive[pi_]:
                    try:
                        next(pipes[pi_])
                    except StopIteration:
                        alive[pi_] = False
        if STAGE == 2:
            dy = nc.dram_tensor("dbg_yh", [512, LO], BF16, kind="ExternalOutput").ap()
            DMA("sp", ["yhd"], ["dbg_yh"], out=dy, in_=yhd)
            S.final_wait("sp", ["dbg_yh"])
            S.emit()
            phD.close()
            return nc
        S.barrier()
        phD.close()

        phX = ExitStack()
        sbX = lambda name, shape, dt=F32: phX.enter_context(nc.sbuf_tensor("s_" + name, list(shape), dt))
        X1 = sbX("X1", [128, NO, D])
        xt = sbX("xt", [128, D])
        rs = sbX("rs", [128, 1])
        xt_a, rs_a = xt, rs
        xtB = sbX("xtB", [128, D])
        rsB = sbX("rsB", [128, 1])
        xnT = sbX("xnT", [128, 8, LO], BF16)
        phE = ExitStack()
        sbE = lambda name, shape, dt=F32: phE.enter_context(nc.sbuf_tensor("s_" + name, list(shape), dt))
        mixt = sbE("mixt", [128, NO, 512])
        load(mixt[:], mixd.rearrange("(n p) f -> p n f", p=128), "mixt")
        hsq = sbE("hsq", [128, NO, 8])
        T("dve", "tensor_tensor", ["mixt"], ["junkE"], out=junk[:, 0:512], in0=mixt[:, 0, :], in1=mixt[:, 0, :], op=ALU.mult)
        msq = sbE("msq", [128, 512])
        for i in range(NO):
            T("dve", "tensor_tensor", ["mixt"], ["msq"], out=msq[:], in0=mixt[:, i, :], in1=mixt[:, i, :], op=ALU.mult)
            T("dve", "reduce_sum", ["msq"], ["hsq"], out=hsq[:, i, :], in_=msq[:, :].rearrange("p (h d) -> p h d", d=64), axis=AX.X)
        T("act", "activation", ["hsq", "epst"], ["hsq"], out=hsq[:], in_=hsq[:], func=ACT.Sqrt, bias=epst[:], scale=1.0 / 64)
        T("dve", "reciprocal", ["hsq"], ["hsq"], out=hsq[:], in_=hsq[:])
        for i in range(NO):
            T("dve", "tensor_tensor", ["mixt", "hsq"], ["mixt"], out=mixt[:, i, :].rearrange("p (h d) -> p h d", d=64), in0=mixt[:, i, :].rearrange("p (h d) -> p h d", d=64),
              in1=hsq[:, i, :].unsqueeze(2).broadcast_to([128, 8, 64]), op=ALU.mult)
            T("dve", "tensor_tensor", ["mixt", "rowt"], ["mixt"], out=mixt[:, i, :], in0=mixt[:, i, :], in1=rv("g_attn"), op=ALU.mult)
            ps, pk = nextps()
            for blk in range(4):
                T("pe", "transpose", ["mixt", "ident"], [pk], out=ps[:, blk * 128:(blk + 1) * 128], in_=mixt[:, i, blk * 128:(blk + 1) * 128], identity=ident[:])
            T("act", "activation", [pk], ["xnT"], out=xnT[:, 0:4, i * 128:(i + 1) * 128], in_=ps[:, :].rearrange("p (k t) -> p k t", t=128), func=ACT.Copy)
        bones = sbE("bones", [128, 128]); load(bones[:], bones_d, "bones")
        yhT = sbE("yhT", [128, 4, LO], BF16)
        load(yhT[:], yhd.rearrange("(k p) t -> p k t", p=128), "yhT", reads=["mixt"])
        ysq = sbE("ysq", [128, 512])
        yr = sbE("yr", [128, 512])
        for ct in range(4):
            for tcn in range(4):
                sl = slice(tcn * 512, (tcn + 1) * 512)
                T("dve", "tensor_tensor", ["yhT"], ["ysq"], out=ysq[:], in0=yhT[:, ct, sl], in1=yhT[:, ct, sl], op=ALU.mult)
                ps, pk = nextps()
                T("pe", "matmul", ["ysq", "bones"], [pk], out=ps[:, :], lhsT=bones[:, :], rhs=ysq[:, :], start=True, stop=True)
                T("act", "activation", [pk, "epst"], ["yr"], out=yr[:], in_=ps[:, :], func=ACT.Sqrt, bias=epst[:], scale=1.0 / 64)
                T("dve", "reciprocal", ["yr"], ["yr"], out=yr[:], in_=yr[:])
                T("dve", "scalar_tensor_tensor", ["yhT", "yr", "v128"], ["xnT"], out=xnT[:, 4 + ct, sl], in0=yhT[:, ct, sl], scalar=pv("g_hy", ct), in1=yr[:], op0=ALU.mult, op1=ALU.mult)
        wstE = sbE("wstE", [128, 8, 256])
        wbig = sbE("wbig", [128, 8, D], BF16)

        def load_w8(src2d):
            for j in range(4):
                load(wstE[:], src2d[:, j * 256:(j + 1) * 256].rearrange("(k p) n -> p k n", p=128), "wstE")
                T("act", "activation", ["wstE"], ["wbig"], out=wbig[:, :, j * 256:(j + 1) * 256], in_=wstE[:], func=ACT.Copy)

        load_w8(w_out)
        load(X1[:], xb[0:LO, :].rearrange("(n p) f -> p n f", p=128), "X1", reads=["wstE"])
        for i in range(NO):
            for nh in range(2):
                ps, pk = nextps()
                for k in range(8):
                    T("pe", "matmul", ["xnT", "wbig"], [pk], out=ps[:, :], lhsT=xnT[:, k, i * 128:(i + 1) * 128], rhs=wbig[:, k, nh * 512:(nh + 1) * 512], start=(k == 0), stop=(k == 7))
                T("dve", "tensor_tensor", [pk, "X1"], ["X1"], out=X1[:, i, nh * 512:(nh + 1) * 512], in0=X1[:, i, nh * 512:(nh + 1) * 512], in1=ps[:, :], op=ALU.add)
        if STAGE == 3:
            dx = nc.dram_tensor("dbg_x1", [128, NO, D], F32, kind="ExternalOutput").ap()
            DMA("sp", ["X1"], ["dbg_x1"], out=dx, in_=X1[:])
            S.final_wait("sp", ["dbg_x1"])
            S.emit()
            phE.close()
            phX.close()
            return nc
        S.barrier()
        phE.close()

        phF = ExitStack()
        sbF = lambda name, shape, dt=F32: phF.enter_context(nc.sbuf_tensor("s_" + name, list(shape), dt))
        Wt = sbF("Wt", [128, NO, 32])
        wrt = sbF("wrt", [128, 8, 36]); load(wrt[:], wr.rearrange("(k p) n -> p k n", p=128), "wrt")
        xnF = sbF("xnF", [128, 8, 128])
        lg = sbF("lg", [128, 36])
        sm = sbF("sm", [128, 16])
        gm = sbF("gm", [128, 4])
        e1 = sbF("e1", [128, 32])
        e2 = sbF("e2", [128, 32])
        s1 = sbF("s1", [128, 32])
        s2 = sbF("s2", [128, 32])
        BIG = 1.0e9

        def norm_T(i, gname, f32dst=None, xkey="X1", tkey="xnT"):
            xt, rs = (xt_a, rs_a) if i % 2 == 0 else (xtB, rsB)
            xk_, rk_ = "xt%d" % (i % 2), "rs%d" % (i % 2)
            T("act", "activation", [xkey], [xk_], out=xt[:], in_=X1[:, i, :], func=ACT.Copy)
            rstd_of(xt[:], D, xk_, rs[:], rk_)
            T("act", "activation", [xk_, rk_], [xk_], out=xt[:], in_=xt[:], func=ACT.Copy, scale=rs[:, 0:1])
            for g in range(2):
                ps, pk = nextps()
                for kq in range(4):
                    k = g * 4 + kq
                    T("pe", "transpose", [xk_, "ident"], [pk], out=ps[:, kq * 128:(kq + 1) * 128], in_=xt[:, k * 128:(k + 1) * 128], identity=ident[:])
                for kq in range(4):
                    k = g * 4 + kq
                    T("dve", "tensor_scalar", [pk, "v128"], [tkey], out=xnT[:, k, i * 128:(i + 1) * 128], in0=ps[:, kq * 128:(kq + 1) * 128], scalar1=pv(gname, k), scalar2=None, op0=ALU.mult)
                    if f32dst is not None:
                        T("dve", "tensor_scalar", [pk, "v128"], ["xnF"], out=f32dst[:, k, :], in0=ps[:, kq * 128:(kq + 1) * 128], scalar1=pv(gname, k), scalar2=None, op0=ALU.mult)

        def route_tile(i):
            norm_T(i, "g_moe", xnF, "X1:%d" % i, "xnT:%d" % i)
            ps, pk = nextps()
            for k in range(8):
                T("pe", "matmul", ["xnF", "wrt"], [pk], out=ps[:, 0:36], lhsT=xnF[:, k, :], rhs=wrt[:, k, :], start=(k == 0), stop=(k == 7))
            T("dve", "tensor_tensor", [pk, "rowt"], ["lg"], out=lg[:], in0=ps[:, 0:36], in1=rv("br"), op=ALU.add)
            T("dve", "reduce_max", ["lg"], ["sm"], out=sm[:, 0:1], in_=lg[:, 0:4], axis=AX.X)
            T("dve", "tensor_scalar", ["lg", "sm"], ["gm"], out=gm[:], in0=lg[:, 0:4], scalar1=sm[:, 0:1], scalar2=None, op0=ALU.subtract)
            T("act", "activation", ["gm"], ["e1"], out=e1[:, 0:4], in_=gm[:], func=ACT.Exp)
            T("dve", "reduce_sum", ["e1"], ["sm"], out=sm[:, 1:2], in_=e1[:, 0:4], axis=AX.X)
            T("dve", "reciprocal", ["sm"], ["sm"], out=sm[:, 2:3], in_=sm[:, 1:2])
            T("dve", "tensor_scalar", ["gm"], ["gm"], out=gm[:], in0=gm[:], scalar1=0.0, scalar2=BIG, op0=ALU.is_lt, op1=ALU.mult)
            T("dve", "tensor_tensor", ["lg", "gm"], ["e1"], out=e1[:, :].rearrange("p (g e) -> p g e", e=8), in0=lg[:, 4:36].rearrange("p (g e) -> p g e", e=8),
              in1=gm[:, :].unsqueeze(2).broadcast_to([128, 4, 8]), op=ALU.subtract)
            T("dve", "reduce_max", ["e1"], ["sm"], out=sm[:, 3:4], in_=e1[:], axis=AX.X)
            T("dve", "tensor_scalar", ["e1", "sm"], ["s1"], out=s1[:], in0=e1[:], scalar1=sm[:, 3:4], scalar2=None, op0=ALU.is_ge)
            T("dve", "scalar_tensor_tensor", ["s1", "e1"], ["e2"], out=e2[:], in0=s1[:], scalar=-BIG, in1=e1[:], op0=ALU.mult, op1=ALU.add)
            T("dve", "reduce_max", ["e2"], ["sm"], out=sm[:, 4:5], in_=e2[:], axis=AX.X)
            T("dve", "tensor_scalar", ["e2", "sm"], ["s2"], out=s2[:], in0=e2[:], scalar1=sm[:, 4:5], scalar2=None, op0=ALU.is_ge)
            T("dve", "tensor_tensor", ["sm"], ["sm"], out=sm[:, 5:6], in0=sm[:, 4:5], in1=sm[:, 3:4], op=ALU.subtract)
            T("act", "activation", ["sm"], ["sm"], out=sm[:, 6:7], in_=sm[:, 5:6], func=ACT.Exp)
            T("dve", "tensor_scalar", ["sm"], ["sm"], out=sm[:, 6:7], in0=sm[:, 6:7], scalar1=1.0, scalar2=None, op0=ALU.add)
            T("dve", "reciprocal", ["sm"], ["sm"], out=sm[:, 7:8], in_=sm[:, 6:7])
            T("dve", "tensor_tensor", ["sm"], ["sm"], out=sm[:, 8:9], in0=sm[:, 7:8], in1=sm[:, 2:3], op=ALU.mult)
            T("dve", "tensor_tensor", ["sm"], ["sm"], out=sm[:, 9:10], in0=sm[:, 2:3], in1=sm[:, 8:9], op=ALU.subtract)
            T("dve", "tensor_scalar", ["s1", "sm"], ["s1"], out=s1[:], in0=s1[:], scalar1=sm[:, 8:9], scalar2=None, op0=ALU.mult)
            T("dve", "scalar_tensor_tensor", ["s2", "sm", "s1"], ["Wt:%d" % i], out=Wt[:, i, :], in0=s2[:], scalar=sm[:, 9:10], in1=s1[:], op0=ALU.mult, op1=ALU.add)

        wfl = sbF("wfl", [128, 4096])
        wgs = wfl[:, :].rearrange("p (k n) -> p k n", n=512)
        wds = wfl[:, :].rearrange("p (k n) -> p k n", n=D)
        wgb = sbF("wgb", [128, 8, 512], BF16)
        wub = sbF("wub", [128, 8, 512], BF16)
        wdb = sbF("wdb", [128, 4, D], BF16)
        hmTs = [sbF("hmT0", [128, 4, LO], BF16), sbF("hmT1", [128, 4, LO], BF16)]
        sg = sbF("sg", [128, 512])

        def experts_gen():
          for ex in range(32):
            hmT, hmk = hmTs[ex % 2], "hmT%d" % (ex % 2)
            load(wgs, wgate[ex].rearrange("(k p) n -> p k n", p=128), "wgs")
            T("act", "activation", ["wgs"], ["wgb"], out=wgb[:], in_=wgs, func=ACT.Copy)
            load(wgs, wup[ex].rearrange("(k p) n -> p k n", p=128), "wgs")
            T("act", "activation", ["wgs"], ["wub"], out=wub[:], in_=wgs, func=ACT.Copy)
            load(wds, wdown[ex].rearrange("(k p) n -> p k n", p=128), "wgs")
            T("act", "activation", ["wgs"], ["wdb"], out=wdb[:], in_=wds, func=ACT.Copy)
            for tcn in range(4):
                xk4 = ["xnT:%d" % j for j in range(tcn * 4, tcn * 4 + 4)]
                for ft in range(4):
                    yield (4 * tcn + 4) if ex == 0 else NO
                    sl = slice(tcn * 512, (tcn + 1) * 512)
                    psg, pkg = nextps()
                    psu, pku = nextps()
                    for k in range(8):
                        T("pe", "matmul", xk4 + ["wgb"], [pkg], out=psg[:, :], lhsT=wgb[:, k, ft * 128:(ft + 1) * 128], rhs=xnT[:, k, sl], start=(k == 0), stop=(k == 7))
                    for k in range(8):
                        T("pe", "matmul", xk4 + ["wub"], [pku], out=psu[:, :], lhsT=wub[:, k, ft * 128:(ft + 1) * 128], rhs=xnT[:, k, sl], start=(k == 0), stop=(k == 7))
                    T("act", "activation", [pkg], ["sg"], out=sg[:], in_=psg[:, :], func=ACT.Silu)
                    T("dve", "tensor_tensor", ["sg", pku], [hmk], out=hmT[:, ft, sl], in0=sg[:], in1=psu[:, :], op=ALU.mult)
            for i in range(NO):
                yield NO
                for nh in range(2):
                    ps, pk = nextps()
                    for ft in range(4):
                        T("pe", "matmul", [hmk, "wdb"], [pk], out=ps[:, :], lhsT=hmT[:, ft, i * 128:(i + 1) * 128], rhs=wdb[:, ft, nh * 512:(nh + 1) * 512], start=(ft == 0), stop=(ft == 3))
                    T("dve", "scalar_tensor_tensor", [pk, "Wt:%d" % i, "X1:%d" % i], ["X1:%d" % i], out=X1[:, i, nh * 512:(nh + 1) * 512], in0=ps[:, :], scalar=Wt[:, i, ex:ex + 1],
                      in1=X1[:, i, nh * 512:(nh + 1) * 512], op0=ALU.mult, op1=ALU.add)

        routed = 0
        cnt_y = 0
        for need in experts_gen():
            while routed < need:
                route_tile(routed)
                routed += 1
            cnt_y += 1
            if routed < NO and cnt_y % 2 == 0:
                route_tile(routed)
                routed += 1
        S.barrier()
        phF.close()

        phG = ExitStack()
        sbG = lambda name, shape, dt=F32: phG.enter_context(nc.sbuf_tensor("s_" + name, list(shape), dt))
        wstE = sbG("wstG", [128, 8, 256])
        wbig = sbG("wbigG", [128, 8, D], BF16)
        load_w8(wpg)
        wps = sbG("wps", [128, 2, D])
        wpb = sbG("wpb", [128, 2, D], BF16)
        load(wps[:], wple.rearrange("(k p) n -> p k n", p=128), "wps")
        T("act", "activation", ["wps"], ["wpb"], out=wpb[:], in_=wps[:], func=ACT.Copy)
        pt_ = sbG("pt", [128, NO, 256])
        load(pt_[:], pb.rearrange("(n p) f -> p n f", p=128), "pt")
        pT = sbG("pT", [128, 2, LO], BF16)
        gt = sbG("gt", [128, 512])
        ot = sbG("ot", [128, D])
        for i in range(NO):
            norm_T(i, "g_ple", None, "X1", "xnT:%d" % i)
            ps, pk = nextps()
            for kq in range(2):
                T("pe", "transpose", ["pt", "ident"], [pk], out=ps[:, kq * 128:(kq + 1) * 128], in_=pt_[:, i, kq * 128:(kq + 1) * 128], identity=ident[:])
            T("act", "activation", [pk], ["pT"], out=pT[:, :, i * 128:(i + 1) * 128], in_=ps[:, 0:256].rearrange("p (k t) -> p k t", t=128), func=ACT.Copy)
        rowG = sbG("rowG", [128, 2048])
        load(rowG[:], rowv[:, ROWV["g_final"][0]:ROWV["g_final"][0] + 2048].partition_broadcast(128), "rowG")
        o_bpg = 1024
        for i in range(NO):
            for nh in range(2):
                sl = slice(nh * 512, (nh + 1) * 512)
                ps, pk = nextps()
                for k in range(8):
                    T("pe", "matmul", ["xnT:%d" % i, "wbig"], [pk], out=ps[:, :], lhsT=xnT[:, k, i * 128:(i + 1) * 128], rhs=wbig[:, k, sl], start=(k == 0), stop=(k == 7))
                T("dve", "tensor_tensor", [pk, "rowG"], ["gt"], out=gt[:], in0=ps[:, :], in1=rowG[:, o_bpg + nh * 512:o_bpg + (nh + 1) * 512], op=ALU.add)
                T("act", "activation", ["gt"], ["gt"], out=gt[:], in_=gt[:], func=ACT.Sigmoid)
                ps2, pk2 = nextps()
                for k in range(2):
                    T("pe", "matmul", ["pT", "wpb"], [pk2], out=ps2[:, :], lhsT=pT[:, k, i * 128:(i + 1) * 128], rhs=wpb[:, k, sl], start=(k == 0), stop=(k == 1))
                T("dve", "tensor_tensor", [pk2, "gt"], ["gt"], out=gt[:], in0=gt[:], in1=ps2[:, :], op=ALU.mult)
                T("dve", "tensor_tensor", ["gt", "X1"], ["X1"], out=X1[:, i, sl], in0=X1[:, i, sl], in1=gt[:], op=ALU.add)
            rstd_of(X1[:, i, :], D, "X1", rs[:], "rs0")
            T("act", "activation", ["X1", "rs0"], ["ot"], out=ot[:], in_=X1[:, i, :], func=ACT.Copy, scale=rs[:, 0:1])
            T("dve", "tensor_tensor", ["ot", "rowG"], ["ot"], out=ot[:], in0=ot[:], in1=rowG[:, 0:1024], op=ALU.mult)
            DMA("sp", ["ot"], ["out"], out=out_d[i * 128:(i + 1) * 128, :], in_=ot[:])
        S.final_wait("sp", ["out"])
        S.barrier()
        S.emit()
        phG.close()
        phX.close()
    return nc


def prep_inputs(inputs):
    c = host_consts()
    g = lambda k: np.asarray(inputs[k], dtype=np.float32)
    x = g("x"); p = g("p")[0]
    w_in = g("w_in")[0]
    conv_w = g("conv_w")[0]; conv_b = g("conv_b")[0]
    wf3 = g("w_f3")[0]
    in_maps = []
    for core in range(8):
        b, half = core // 2, core % 2
        rev = (half == 1)
        xb = x[b][::-1] if rev else x[b]
        pb = p[b][::-1][:LO] if rev else p[b][:LO]
        cw = conv_w[::-1] if rev else conv_w
        if rev:
            w3 = wf3.reshape(64, 2, 2, 512)[:, :, ::-1, :].reshape(64, 2048)
        else:
            w3 = wf3
        v128 = np.zeros((128, V128_N), np.float32)
        v128[:, 0:8] = g("g_mix")[0].reshape(8, 128).T
        v128[:, 8:16] = g("g_moe")[0].reshape(8, 128).T
        v128[:, 16:24] = g("g_ple")[0].reshape(8, 128).T
        v128[:, 24:60] = cw.reshape(3, 12, 128).transpose(2, 1, 0).reshape(128, 36)
        v128[:, 60:72] = conv_b.reshape(12, 128).T
        v128[:, 72:76] = g("g_hyena_out")[0].reshape(4, 128).T
        v64 = np.stack([g("b_f1")[0], g("freq1")[0], g("b_f2")[0], g("freq2")[0]], axis=1)
        rowv = np.concatenate([g("q_gain")[0], g("k_gain")[0], g("g_attn_out")[0], g("g_final"), g("b_ple_gate")[0],
                               g("b_group")[0], g("b_router")[0], g("filt_bias")[0].reshape(-1)])[None, :]
        m = {
            "xb": xb, "pb": pb, "w_in": w_in, "w_out": g("w_out")[0], "wgate": g("w_gate")[0], "wup": g("w_up")[0],
            "wdown": g("w_down")[0], "wpg": g("w_ple_gate")[0], "wple": g("w_ple")[0], "wf1": g("w_f1")[0], "wf2": g("w_f2")[0],
            "wf3": w3, "wr": np.concatenate([g("w_group")[0], g("w_router")[0]], axis=1), "v128": v128, "v64": v64, "rowv": rowv,
            "ropeC": c["ropeC"][::-1] if rev else c["ropeC"], "ropeS": c["ropeS"][::-1] if rev else c["ropeS"],
        }
        for k in ("ident", "bones", "ones32", "zT", "dec", "F64", "Gr", "Gi", "Wi1", "Wi2", "GiC"):
            m[k] = c[k]
        in_maps.append({k: np.ascontiguousarray(v) for k, v in m.items()})
    return in_maps


_NC = None


def kernel(**inputs):
    global _NC
    in_maps = prep_inputs(inputs)
    if _NC is None:
        _NC = build_nc()
    res = run_bass_kernel_spmd(_NC, in_maps, core_ids=list(range(8)))
    out = np.zeros((4, L, D), np.float32)
    for core in range(8):
        b, half = core // 2, core % 2
        o = np.asarray(res.results[core]["out"], dtype=np.float32)
        if half == 0:
            out[b, :LO] = o
        else:
            out[b, LO:] = o[::-1]
    return out
```

```python
import math
import numpy as np
import ml_dtypes
from contextlib import ExitStack
import concourse.bass as bass
import concourse.mybir as mybir
from concourse.bass_utils import run_bass_kernel_spmd

F32 = mybir.dt.float32
BF16 = mybir.dt.bfloat16
I32 = mybir.dt.int32
ACT = mybir.ActivationFunctionType
ALU = mybir.AluOpType
AX = mybir.AxisListType

ENGS = ("pe", "act", "dve", "pool", "sp")
L = 4096
D = 1024
NT = 32
NO = 16
LO = 2048
EPS = 1e-6
CC = 32
GK = 512 // (2 * CC)
SB = 512 // CC
NCH = 512 // CC
K1 = 33
STAGE = 99
SAME_ENG_FIFO = False


class Sched:
    SEM_LIMIT = 20000
    NDMA = 24

    def __init__(self, nc, stack):
        self.nc = nc
        self.stack = stack
        self.q = {e: [] for e in ENGS}
        self.cur_sem = {}
        self.cur_cnt = {}
        self.nsem = 0
        for e in ENGS:
            self._new_eng_sem(e)
        self.dma_sems = [self._sem("dma%d" % i) for i in range(self.NDMA)]
        self.dma_tgt = [0] * self.NDMA
        self.dma_k = 0
        self.last_w = {}
        self.readers = {}
        self.seen = {e: {} for e in ENGS}

    def _sem(self, name):
        self.nsem += 1
        return self.stack.enter_context(self.nc.semaphore(name))

    def _new_eng_sem(self, e):
        self.cur_sem[e] = self._sem("c_%s_%d" % (e, self.nsem))
        self.cur_cnt[e] = 0

    def _waits_for(self, eng, toks):
        out = []
        seen = self.seen[eng]
        for t in toks:
            if t is None:
                continue
            sem, val, teng = t
            if teng == eng and (eng == "pe" or SAME_ENG_FIFO):
                continue
            k = id(sem)
            if seen.get(k, 0) >= val:
                continue
            seen[k] = val
            out.append((sem, val))
        return out

    @staticmethod
    def _excl(reads, writes):
        r2 = [b for b in reads if not b.startswith("ps")]
        w2 = list(writes) + [b for b in reads if b.startswith("ps")]
        return r2, w2

    def _deps(self, reads, writes):
        toks = []
        for b in reads:
            toks.append(self.last_w.get(b))
        for b in writes:
            toks.append(self.last_w.get(b))
            toks.extend(self.readers.get(b, ()))
        return toks

    def _commit(self, tok, reads, writes):
        for b in reads:
            self.readers.setdefault(b, []).append(tok)
        for b in writes:
            self.last_w[b] = tok
            self.readers[b] = []

    def op(self, eng, fn, reads=(), writes=()):
        reads, writes = self._excl(reads, writes)
        toks = self._deps(reads, writes)
        waits = self._waits_for(eng, toks)
        if self.cur_cnt[eng] >= self.SEM_LIMIT:
            self._new_eng_sem(eng)
        self.cur_cnt[eng] += 1
        sem = self.cur_sem[eng]
        tok = (sem, self.cur_cnt[eng], eng)
        self.q[eng].append((waits, fn, sem, 1))
        self._commit(tok, reads, writes)
        return tok

    def dma(self, eng, fn, reads=(), writes=()):
        reads, writes = self._excl(reads, writes)
        toks = self._deps(reads, writes)
        i = self.dma_k % self.NDMA
        self.dma_k += 1
        sem = self.dma_sems[i]
        if self.dma_tgt[i] > 0:
            toks.append((sem, self.dma_tgt[i], "dma"))
        waits = self._waits_for(eng, toks)
        self.dma_tgt[i] += 16
        tok = (sem, self.dma_tgt[i], "dma")
        self.q[eng].append((waits, fn, sem, 16))
        self._commit(tok, reads, writes)
        return tok

    def barrier(self):
        toks = [(self.cur_sem[e], self.cur_cnt[e], e) for e in ENGS if self.cur_cnt[e] > 0]
        toks += [(self.dma_sems[i], self.dma_tgt[i], "dma") for i in range(self.NDMA) if self.dma_tgt[i] > 0]
        for e in ENGS:
            tk = toks
            waits = self._waits_for(e, tk)
            self.q[e].append((waits, None, None, 0))
        self.last_w = {}
        self.readers = {}

    def final_wait(self, eng, bufs):
        toks = [self.last_w.get(b) for b in bufs]
        waits = self._waits_for(eng, toks)
        self.q[eng].append((waits, None, None, 0))

    def emit(self):
        nc = self.nc
        q = self.q

        def run(engobj, lst):
            for waits, fn, sem, inc in lst:
                for (s, v) in waits:
                    engobj.wait_ge(s, v)
                if fn is not None:
                    meth, kw = fn
                    ins = getattr(engobj, meth)(**kw)
                    ins.then_inc(sem, inc)

        with nc.Block() as block:
            @block.tensor
            def _(e):
                run(e, q["pe"])

            @block.scalar
            def _(e):
                run(e, q["act"])

            @block.vector
            def _(e):
                run(e, q["dve"])

            @block.gpsimd
            def _(e):
                run(e, q["pool"])

            @block.sync
            def _(e):
                run(e, q["sp"])


def _bf(a):
    return np.ascontiguousarray(a.astype(np.float32)).astype(ml_dtypes.bfloat16)


_CONST = None


def host_consts():
    global _CONST
    if _CONST is not None:
        return _CONST
    c = {}
    c["ident"] = np.eye(128, dtype=np.float32)
    bo = np.zeros((128, 128), np.float32)
    bo[:64, :64] = 1.0
    bo[64:, 64:] = 1.0
    c["bones"] = bo
    c["ones32"] = np.ones((32, 32), np.float32)
    S = L
    rows = S // 64
    r_idx, c_idx = np.meshgrid(np.arange(rows, dtype=np.float32), np.arange(64, dtype=np.float32), indexing="ij")
    r_idx, c_idx = r_idx.reshape(S), c_idx.reshape(S)
    half = 32
    inv = (10000.0 ** (-np.arange(0, half, 2, dtype=np.float32) / half)).astype(np.float32)
    ang_r = (r_idx[:, None] * inv[None]).astype(np.float32)
    ang_c = (c_idx[:, None] * inv[None]).astype(np.float32)
    cr, sr, cc_, sc = np.cos(ang_r), np.sin(ang_r), np.cos(ang_c), np.sin(ang_c)
    c["ropeC"] = np.concatenate([cr, cr, cc_, cc_], axis=1).astype(np.float32)
    c["ropeS"] = np.concatenate([-sr, sr, -sc, sc], axis=1).astype(np.float32)
    bands = 16
    t = np.linspace(0.0, 1.0, L, dtype=np.float32)[:, None]
    w = ((2.0 * math.pi / L) * np.arange(L, dtype=np.float32))[:, None]
    f = np.linspace(1e-4, bands - 1, bands, dtype=np.float32)[None]
    z = (f * w).astype(np.float32)
    zz = np.concatenate([t, np.cos(z), -np.sin(z)], axis=-1).astype(np.float32)
    c["zT"] = np.ascontiguousarray(zz.T)
    max_decay = math.log(1e-2) / 0.3
    min_decay = math.log(1e-2) / 1.5
    deltas = np.linspace(min_decay, max_decay, 512, dtype=np.float32)
    dec = np.exp(-t * np.abs(deltas)[None]).astype(np.float32)
    c["dec"] = _bf(dec.reshape(32, 128, 512).transpose(0, 2, 1))
    a = np.arange(32)[:, None].astype(np.float64)
    k1 = np.arange(K1)[None, :].astype(np.float64)
    th = 2 * np.pi * a * k1 / 64.0
    c["F64"] = _bf(np.concatenate([np.cos(th), -np.sin(th)], axis=1))
    b = np.arange(128).astype(np.float64)
    kk = (np.arange(K1)[:, None] + 64 * np.arange(128)[None, :]).astype(np.float64)
    th = 2 * np.pi * b[:, None, None] * kk[None] / 8192.0
    c["Gr"] = _bf(np.cos(th).reshape(128, K1 * 128))
    c["Gi"] = _bf((-np.sin(th)).reshape(128, K1 * 128))
    k2 = np.arange(128).astype(np.float64)
    th = 2 * np.pi * k2[:, None] * b[None, :] / 128.0
    wr_, wi_ = np.cos(th), np.sin(th)
    c["Wi1"] = _bf(np.concatenate([wr_, wi_], axis=1))
    c["Wi2"] = _bf(np.concatenate([-wi_, wr_], axis=1))
    k1v = np.arange(K1).astype(np.float64)
    wgt = np.full(K1, 2.0); wgt[0] = 1.0; wgt[32] = 1.0
    av = np.arange(32).astype(np.float64)
    th = 2 * np.pi * (k1v[:, None, None] * av[None, None, :] / 64.0 + k1v[:, None, None] * b[None, :, None] / 8192.0)
    gr_ = (np.cos(th) * wgt[:, None, None] / 8192.0).reshape(K1, 128 * 32)
    gi_ = (-np.sin(th) * wgt[:, None, None] / 8192.0).reshape(K1, 128 * 32)
    c["GiC"] = _bf(np.concatenate([gr_, gi_], axis=0))
    _CONST = c
    return c


ROWV = {}
_off = 0
for _n, _l in (("q_gain", 64), ("k_gain", 64), ("g_attn", 512), ("g_final", 1024), ("b_pg", 1024), ("br", 36), ("fbias", 1024)):
    ROWV[_n] = (_off, _l)
    _off += _l
ROWV_N = _off
V128 = {"g_mix": (0, 8), "g_moe": (8, 8), "g_ple": (16, 8), "conv_w": (24, 36), "conv_b": (60, 12), "g_hy": (72, 4)}
V128_N = 76


def build_nc():
    nc = bass.Bass("TRN2", target_bir_lowering=False)

    def din(name, shape, dt=F32):
        return nc.dram_tensor(name, list(shape), dt, kind="ExternalInput").ap()

    xb = din("xb", [L, D])
    pb = din("pb", [LO, 256])
    w_in = din("w_in", [D, 2304])
    w_out = din("w_out", [D, D])
    if STAGE > 3:
        wgate = din("wgate", [32, D, 512])
        wup = din("wup", [32, D, 512])
        wdown = din("wdown", [32, 512, D])
    wpg = din("wpg", [D, D])
    wple = din("wple", [256, D])
    wf1 = din("wf1", [33, 64])
    wf2 = din("wf2", [64, 64])
    wf3 = din("wf3", [64, 2048])
    wr = din("wr", [D, 36])
    v128 = din("v128", [128, V128_N])
    v64 = din("v64", [64, 4])
    rowv = din("rowv", [1, ROWV_N])
    ident_d = din("ident", [128, 128])
    bones_d = din("bones", [128, 128])
    ones32_d = din("ones32", [32, 32])
    ropeC_d = din("ropeC", [L, 64])
    ropeS_d = din("ropeS", [L, 64])
    zT_d = din("zT", [33, L])
    dec_d = din("dec", [32, 512, 128], BF16)
    F64_d = din("F64", [32, 2 * K1], BF16)
    Gr_d = din("Gr", [128, K1 * 128], BF16)
    Gi_d = din("Gi", [128, K1 * 128], BF16)
    Wi1_d = din("Wi1", [128, 256], BF16)
    Wi2_d = din("Wi2", [128, 256], BF16)
    GiC_d = din("GiC", [2 * K1, 128 * 32], BF16)
    out_d = nc.dram_tensor("out", [LO, D], F32, kind="ExternalOutput").ap()
    ucd = nc.dram_tensor("ucd", [1536, L], BF16, kind="Internal").ap()
    yhd = nc.dram_tensor("yhd", [512, LO], BF16, kind="Internal").ap()

    with ExitStack() as st:
        S = Sched(nc, st)

        def sb(name, shape, dt=F32):
            return st.enter_context(nc.sbuf_tensor("s_" + name, list(shape), dt))

        PS = [st.enter_context(nc.psum_tensor("ps%d" % i, [128, 512], F32)) for i in range(8)]
        psk = [0]

        def nextps(lo=0, hi=8):
            i = lo + psk[0] % (hi - lo)
            psk[0] += 1
            return PS[i], "ps%d" % i

        def T(eng, meth, reads, writes, **kw):
            return S.op(eng, (meth, kw), list(reads), list(writes))

        def DMA(eng, reads, writes, **kw):
            return S.dma(eng, ("dma_start", kw), list(reads), list(writes))

        def load(dst, src, key, reads=()):
            DMA("sp", reads, [key], out=dst, in_=src)


        ident = sb("ident", [128, 128])
        load(ident[:], ident_d, "ident")
        v128t = sb("v128t", [128, V128_N])
        load(v128t[:], v128, "v128")
        v64t = sb("v64t", [64, 4])
        load(v64t[:], v64, "v64")
        RA = ROWV["g_final"][0]
        RB0 = ROWV["br"][0]
        RBN = ROWV_N - RB0
        rowt = sb("rowt", [128, RA + RBN])
        load(rowt[:, 0:RA], rowv[:, 0:RA].partition_broadcast(128), "rowt")
        load(rowt[:, RA:RA + RBN], rowv[:, RB0:ROWV_N].partition_broadcast(128), "rowt")

        def rcol(off):
            return off if off < RA else off - RB0 + RA
        epst = sb("epst", [128, 1])
        T("dve", "memset", [], ["epst"], ap=epst[:], constant=EPS)
        negpi = sb("negpi", [128, 1])
        T("dve", "memset", [], ["negpi"], ap=negpi[:], constant=-math.pi)
        junk = sb("junk", [128, 1024])
        ssq = sb("ssq", [128, 16])

        def rv(name):
            o, l = ROWV[name]
            return rowt[:, rcol(o):rcol(o) + l]

        def pv(name, j=0, n=1):
            o, l = V128[name]
            return v128t[:, o + j:o + j + n]

        def rstd_of(src_ap, width, key_src, dst_ap, key_dst):
            T("dve", "tensor_tensor", [key_src], ["junk"], out=junk[:, 0:width], in0=src_ap, in1=src_ap, op=ALU.mult)
            T("dve", "reduce_sum", ["junk"], ["ssq"], out=ssq[:, 0:1], in_=junk[:, 0:width], axis=AX.X)
            T("act", "activation", ["ssq", "epst"], ["ssq"], out=ssq[:, 1:2], in_=ssq[:, 0:1], func=ACT.Sqrt, bias=epst[:], scale=1.0 / width)
            T("dve", "reciprocal", ["ssq"], [key_dst], out=dst_ap, in_=ssq[:, 1:2])

        def norm_transpose(xt, xk, rs, rk, gname, dstT, tcol, tkey):
            rstd_of(xt[:], D, xk, rs[:], rk)
            T("act", "activation", [xk, rk], [xk], out=xt[:], in_=xt[:], func=ACT.Copy, scale=rs[:, 0:1])
            for g in range(2):
                ps, pk = nextps(0, 4)
                for kq in range(4):
                    k = g * 4 + kq
                    T("pe", "transpose", [xk, "ident"], [pk], out=ps[:, kq * 128:(kq + 1) * 128], in_=xt[:, k * 128:(k + 1) * 128], identity=ident[:])
                for kq in range(4):
                    k = g * 4 + kq
                    if kq % 2 == 0:
                        T("dve", "tensor_scalar", [pk, "v128"], [tkey], out=dstT[:, k, tcol:tcol + 128], in0=ps[:, kq * 128:(kq + 1) * 128],
                          scalar1=pv(gname, k), scalar2=None, op0=ALU.mult)
                    else:
                        T("act", "activation", [pk, "v128"], [tkey], out=dstT[:, k, tcol:tcol + 128], in_=ps[:, kq * 128:(kq + 1) * 128],
                          func=ACT.Copy, scale=pv(gname, k))

        mixd = nc.dram_tensor("mixd", [LO, 512], F32, kind="Internal").ap()
        ph1 = ExitStack()
        sb1 = lambda name, shape, dt=F32: ph1.enter_context(nc.sbuf_tensor("s_" + name, list(shape), dt))
        QTz = [sb1("QT0", [128, 4, LO], BF16), sb1("QT1", [128, 4, LO], BF16)]
        T("pool", "memset", [], ["QTz0"], ap=QTz[0][64:128, :, :], constant=0.0)
        T("pool", "memset", [], ["QTz1"], ap=QTz[1][0:64, :, :], constant=0.0)
        KT2 = sb1("KT2", [128, 2, L], BF16)
        Vx = sb1("Vx", [128, NT, 2, 65], BF16)
        T("pool", "memset", [], ["Vx"], ap=Vx[:], constant=1.0)
        phT = ExitStack()
        hT = phT.enter_context(nc.sbuf_tensor("s_hT", [128, 8, L], BF16))
        phA = ExitStack()
        NXB = 4
        xt2 = [phA.enter_context(nc.sbuf_tensor("s_xt%d" % i, [128, D], F32)) for i in range(NXB)]
        rs2 = [phA.enter_context(nc.sbuf_tensor("s_rs%d" % i, [128, 1], F32)) for i in range(NXB)]

        def A_tile(i):
            xt, xk = xt2[i % NXB], "xt%d" % (i % NXB)
            load(xt[:], xb[i * 128:(i + 1) * 128, :], xk)
            norm_transpose(xt, xk, rs2[i % NXB], "rs%d" % (i % NXB), "g_mix", hT, i * 128, "hT:%d" % i)

        phB = ExitStack()
        sbB = lambda name, shape, dt=F32: phB.enter_context(nc.sbuf_tensor("s_" + name, list(shape), dt))
        wst = sbB("wst", [128, 8, 256])
        wqkv = sbB("wqkv", [128, 8, 768], BF16)
        for j in range(3):
            load(wst[:], w_in[:, j * 256:(j + 1) * 256].rearrange("(k p) n -> p k n", p=128), "wst")
            T("act", "activation", ["wst"], ["wqkv"], out=wqkv[:, :, j * 256:(j + 1) * 256], in_=wst[:], func=ACT.Copy)
        ropeC = sbB("ropeC", [128, NT, 64])
        ropeS = sbB("ropeS", [128, NT, 64])
        load(ropeC[:], ropeC_d.rearrange("(n p) f -> p n f", p=128), "ropeC")
        load(ropeS[:], ropeS_d.rearrange("(n p) f -> p n f", p=128), "ropeS")
        qk = sbB("qk", [128, 10, 64])
        qk2 = sbB("qk2", [128, 10, 64])
        qk3 = sbB("qk3", [128, 12, 64])
        hs10 = sbB("hs10", [128, 10])

        def B1_tile(i):
            own = i < NO
            nh = 10 if own else 2
            h0 = 0 if own else 8
            hk = ["hT:%d" % i, "wqkv"]
            ps2, pk2 = nextps(0, 4)
            if own:
                ps, pk = nextps(0, 4)
                for k in range(8):
                    T("pe", "matmul", hk, [pk], out=ps[:, 0:512], lhsT=hT[:, k, i * 128:(i + 1) * 128], rhs=wqkv[:, k, 0:512], start=(k == 0), stop=(k == 7))
            for k in range(8):
                T("pe", "matmul", hk, [pk2], out=ps2[:, 0:256], lhsT=hT[:, k, i * 128:(i + 1) * 128], rhs=wqkv[:, k, 512:768], start=(k == 0), stop=(k == 7))
            if own:
                T("act", "activation", [pk], ["qk"], out=qk[:, 0:8, :], in_=ps[:, 0:512].rearrange("p (h d) -> p h d", d=64), func=ACT.Copy)
            T("act", "activation", [pk2], ["qk"], out=qk[:, 8:10, :], in_=ps2[:, 0:128].rearrange("p (h d) -> p h d", d=64), func=ACT.Copy)
            T("dve", "tensor_copy", [pk2], ["Vx"], out=Vx[:, i, :, 0:64], in_=ps2[:, 128:256].rearrange("p (h d) -> p h d", d=64))
            T("dve", "tensor_tensor", ["qk"], ["qk2"], out=qk2[:, h0:10, :], in0=qk[:, h0:10, :], in1=qk[:, h0:10, :], op=ALU.mult)
            T("dve", "reduce_sum", ["qk2"], ["hs10"], out=hs10[:, h0:10], in_=qk2[:, h0:10, :], axis=AX.X)
            T("act", "activation", ["hs10", "epst"], ["hs10"], out=hs10[:, h0:10], in_=hs10[:, h0:10], func=ACT.Sqrt, bias=epst[:], scale=1.0 / 64)
            T("dve", "reciprocal", ["hs10"], ["hs10"], out=hs10[:, h0:10], in_=hs10[:, h0:10])
            T("dve", "tensor_tensor", ["qk", "hs10"], ["qk"], out=qk[:, h0:10, :], in0=qk[:, h0:10, :],
              in1=hs10[:, h0:10].unsqueeze(2).broadcast_to([128, nh, 64]), op=ALU.mult)
            if own:
                T("dve", "tensor_tensor", ["qk", "rowt"], ["qk"], out=qk[:, 0:8, :], in0=qk[:, 0:8, :],
                  in1=rv("q_gain").unsqueeze(1).broadcast_to([128, 8, 64]), op=ALU.mult)
            T("dve", "tensor_tensor", ["qk", "rowt"], ["qk"], out=qk[:, 8:10, :], in0=qk[:, 8:10, :],
              in1=rv("k_gain").unsqueeze(1).broadcast_to([128, 2, 64]), op=ALU.mult)
            v5 = qk[:, h0:10, :].rearrange("p h (g t n) -> p h g t n", g=2, t=2, n=16)
            w5 = qk2[:, h0:10, :].rearrange("p h (g t n) -> p h g t n", g=2, t=2, n=16)
            T("act", "activation", ["qk"], ["qk2"], out=w5[:, :, :, 0, :], in_=v5[:, :, :, 1, :], func=ACT.Copy)
            T("act", "activation", ["qk"], ["qk2"], out=w5[:, :, :, 1, :], in_=v5[:, :, :, 0, :], func=ACT.Copy)
            T("dve", "tensor_tensor", ["qk", "ropeC"], ["qk"], out=qk[:, h0:10, :], in0=qk[:, h0:10, :],
              in1=ropeC[:, i, :].unsqueeze(1).broadcast_to([128, nh, 64]), op=ALU.mult)
            T("dve", "tensor_tensor", ["qk2", "ropeS"], ["qk2"], out=qk2[:, h0:10, :], in0=qk2[:, h0:10, :],
              in1=ropeS[:, i, :].unsqueeze(1).broadcast_to([128, nh, 64]), op=ALU.mult)
            if own:
                T("dve", "tensor_tensor", ["qk", "qk2"], ["qk3"], out=qk3[:, 0:8, :], in0=qk[:, 0:8, :], in1=qk2[:, 0:8, :], op=ALU.add)
            for kv in range(2):
                for dup in range(2):
                    T("dve", "tensor_tensor", ["qk", "qk2"], ["qk3"], out=qk3[:, 8 + 2 * kv + dup, :], in0=qk[:, 8 + kv, :], in1=qk2[:, 8 + kv, :], op=ALU.add)
            if own:
                ps, pk = nextps(0, 4)
                for blk in range(4):
                    T("pe", "transpose", ["qk3", "ident"], [pk], out=ps[:, blk * 128:(blk + 1) * 128],
                      in_=qk3[:, 2 * blk:2 * blk + 2, :].rearrange("p h d -> p (h d)"), identity=ident[:])
                T("act", "activation", [pk, "QTz0"], ["QT:%d" % i], out=QTz[0][0:64, :, i * 128:(i + 1) * 128], in_=ps[0:64, 0:512].rearrange("p (h t) -> p h t", t=128), func=ACT.Copy)
                T("act", "activation", [pk, "QTz1"], ["QT:%d" % i], out=QTz[1][64:128, :, i * 128:(i + 1) * 128], in_=ps[64:128, 0:512].rearrange("p (h t) -> p h t", t=128), func=ACT.Copy)
            ps3, pk3 = nextps(0, 4)
            for kv in range(2):
                T("pe", "transpose", ["qk3", "ident"], [pk3], out=ps3[:, kv * 128:(kv + 1) * 128],
                  in_=qk3[:, 8 + 2 * kv:10 + 2 * kv, :].rearrange("p h d -> p (h d)"), identity=ident[:])
            T("act", "activation", [pk3], ["KT2:%d" % i], out=KT2[:, :, i * 128:(i + 1) * 128], in_=ps3[:, 0:256].rearrange("p (h t) -> p h t", t=128), func=ACT.Copy)
        A_tile(0)
        A_tile(1)
        for i in range(NT):
            if i + 2 < NT:
                A_tile(i + 2)
            B1_tile(i)
        S.barrier()
        phB.close()
        phA.close()
        phB2 = ExitStack()
        sbB2 = lambda name, shape, dt=F32: phB2.enter_context(nc.sbuf_tensor("s_" + name, list(shape), dt))
        wst2 = sbB2("wst2", [128, 8, 128])
        wct = [sbB2("wct0", [128, 8, 128], BF16), sbB2("wct1", [128, 8, 128], BF16)]
        U = sbB2("U", [128, L + 2])
        T("pool", "memset", [], ["U"], ap=U[:], constant=0.0)
        HL = L // 2
        uc32 = sbB2("uc32", [128, HL])
        ucb = sbB2("ucb", [128, L], BF16)
        PT = [sbB2("PT0", [128, 512], BF16), sbB2("PT1", [128, 512], BF16), sbB2("PT2", [128, 512], BF16)]
        STB = [0, 1, 7]
        rden = sbB2("rden", [128, 4])
        accS = [sbB2("accS0", [65, 512]), sbB2("accS1", [65, 512])]
        stg = [sbB2("stg0", [128, 4, 64]), sbB2("stg1", [128, 4, 64])]
        mixv = mixd.rearrange("(n p) f -> p n f", p=128)

        def b2_gen():
            for ct in range(12):
                wc, wk = wct[ct % 2], "wct%d" % (ct % 2)
                load(wst2[:], w_in[:, 768 + ct * 128:768 + (ct + 1) * 128].rearrange("(k p) n -> p k n", p=128), "wst2")
                T("pool", "tensor_copy", ["wst2"], [wk], out=wc[:], in_=wst2[:])
                for tcn in range(8):
                    ps, pk = nextps(2, 4)
                    for k in range(8):
                        T("pe", "matmul", [wk] + ["hT:%d" % j for j in range(tcn * 4, tcn * 4 + 4)], [pk], out=ps[:, :], lhsT=wc[:, k, :],
                          rhs=hT[:, k, tcn * 512:(tcn + 1) * 512], start=(k == 0), stop=(k == 7))
                    T("dve", "tensor_copy", [pk], ["U"], out=U[:, 1 + tcn * 512:1 + (tcn + 1) * 512], in_=ps[:, :])
                    yield
                for hh in range(2):
                    o = hh * HL
                    T("dve", "tensor_scalar", ["U", "v128"], ["uc32"], out=uc32[:], in0=U[:, o:o + HL], scalar1=pv("conv_w", ct * 3 + 0), scalar2=pv("conv_b", ct), op0=ALU.mult, op1=ALU.add)
                    T("dve", "scalar_tensor_tensor", ["U", "v128", "uc32"], ["uc32"], out=uc32[:], in0=U[:, o + 1:o + HL + 1], scalar=pv("conv_w", ct * 3 + 1), in1=uc32[:], op0=ALU.mult, op1=ALU.add)
                    T("dve", "scalar_tensor_tensor", ["U", "v128", "uc32"], ["ucb"], out=ucb[:, o:o + HL], in0=U[:, o + 2:o + HL + 2], scalar=pv("conv_w", ct * 3 + 2), in1=uc32[:], op0=ALU.mult, op1=ALU.add)
                    yield
                DMA("sp", ["ucb"], ["ucd"], out=ucd[ct * 128:(ct + 1) * 128, :], in_=ucb[:])

        scale = 64 ** -0.5
        allQT = ["QT:%d" % i for i in range(NO)]
        iters = []
        for hp in range(4):
            for j in range(2):
                for qg in range(4):
                    for kt in range(NT):
                        iters.append((hp, j, qg, kt))

        def emit_st(n):
            hp, j, qg, kt = iters[n]
            kv = hp // 2
            T("pe", "matmul", ["KT2:%d" % kt] + allQT[qg * 4:qg * 4 + 4], ["ps%d" % STB[n % 3]], out=PS[STB[n % 3]][:, :], lhsT=KT2[:, kv, kt * 128:(kt + 1) * 128],
              rhs=QTz[j][:, hp, qg * 512:(qg + 1) * 512], start=True, stop=True)

        def attn_gen():
            emit_st(0)
            emit_st(1)
            ng = 0
            for n in range(len(iters)):
                hp, j, qg, kt = iters[n]
                kv = hp // 2
                h = 2 * hp + j
                pss, pks = PS[STB[n % 3]], "ps%d" % STB[n % 3]
                pt, ptk = PT[n % 3], "PT%d" % (n % 3)
                T("act", "activation", [pks], [ptk], out=pt[:], in_=pss[:, :], func=ACT.Exp, scale=scale)
                if n + 2 < len(iters):
                    emit_st(n + 2)
                gi_ = n // NT
                accb = 4 + (gi_ % 2)
                T("pe", "matmul", [ptk, "Vx"], ["ps%d" % accb], out=PS[accb][0:65, :], lhsT=Vx[:, kt, kv, :], rhs=pt[:, :],
                  start=(kt == 0), stop=(kt == NT - 1))
                if kt == NT - 1:
                    sg_, sgk = stg[ng % 2], "stg%d" % (ng % 2)
                    ac_, ack = accS[ng % 2], "accS%d" % (ng % 2)
                    trb = 6
                    ng += 1
                    T("act", "activation", ["ps%d" % accb], [ack], out=ac_[:, :], in_=PS[accb][0:65, :], func=ACT.Copy)
                    for qi in range(4):
                        T("pe", "transpose", [ack, "ident"], ["ps%d" % trb], out=PS[trb][:, qi * 65:(qi + 1) * 65], in_=ac_[:, qi * 128:(qi + 1) * 128], identity=ident[0:65, 0:65])
                    for qi in range(4):
                        T("dve", "reciprocal", ["ps%d" % trb], ["rden"], out=rden[:, qi:qi + 1], in_=PS[trb][:, qi * 65 + 64:qi * 65 + 65])
                        T("dve", "tensor_scalar", ["ps%d" % trb, "rden"], [sgk], out=sg_[:, qi, :], in0=PS[trb][:, qi * 65:qi * 65 + 64],
                          scalar1=rden[:, qi:qi + 1], scalar2=None, op0=ALU.mult)
                    DMA("sp", [sgk], ["mixd"], out=mixv[:, qg * 4:(qg + 1) * 4, h * 64:(h + 1) * 64], in_=sg_[:])
                yield

        ga, gb = attn_gen(), b2_gen()
        da = db = False
        na_ = 0
        while not (da and db):
            if not da:
                try:
                    next(ga)
                    na_ += 1
                except StopIteration:
                    da = True
            if not db and (da or na_ % 8 == 0):
                try:
                    next(gb)
                except StopIteration:
                    db = True
        S.barrier()
        phB2.close()
        phT.close()
        ph1.close()
        TWO_PI = 2.0 * math.pi
        phD = ExitStack()
        sbD = lambda name, shape, dt=F32: phD.enter_context(nc.sbuf_tensor("s_" + name, list(shape), dt))
        F64 = sbD("F64", [32, 2 * K1], BF16); load(F64[:], F64_d, "F64")
        Gr = sbD("Gr", [128, K1, 128], BF16); load(Gr[:], Gr_d.rearrange("p (k m) -> p k m", m=128), "Gr")
        Gi = sbD("Gi", [128, K1, 128], BF16); load(Gi[:], Gi_d.rearrange("p (k m) -> p k m", m=128), "Gi")
        Wi1 = sbD("Wi1", [128, 256], BF16); load(Wi1[:], Wi1_d, "Wi1")
        GiC = sbD("GiC", [2 * K1, 128, 32], BF16)
        DMA("sp", [], ["GiC"], out=GiC[:, :, :], in_=GiC_d.rearrange("p (b a) -> p b a", a=32))
        h2T = sbD("h2T", [64, L], BF16)
        phM = ExitStack()
        sbM = lambda name, shape, dt=F32: phM.enter_context(nc.sbuf_tensor("s_" + name, list(shape), dt))
        zT = sbM("zT", [33, L]); load(zT[:], zT_d, "zT")
        wf1t = sbM("wf1t", [33, 64]); load(wf1t[:], wf1, "wf1t")
        wf2t = sbM("wf2t", [64, 64]); load(wf2t[:], wf2, "wf2t")
        h1T = sbM("h1T", [64, L])
        mtmp = sbM("mtmp", [64, 512])
        mti = sbM("mti", [64, 512], I32)
        mtf = sbM("mtf", [64, 512])
        for (wt_, wkey, src, skey, dst, dkey, bcol) in ((wf1t, "wf1t", zT, "zT", h1T, "h1T", 0), (wf2t, "wf2t", h1T, "h1T", h2T, "h2T", 2)):
            for tcn in range(8):
                ps, pk = nextps()
                T("pe", "matmul", [wkey, skey], [pk], out=ps[0:64, :], lhsT=wt_[:, :], rhs=src[:, tcn * 512:(tcn + 1) * 512], start=True, stop=True)
                T("dve", "tensor_scalar", [pk, "v64"], ["mtmp"], out=mtmp[:], in0=ps[0:64, :], scalar1=v64t[:, bcol:bcol + 1], scalar2=v64t[:, bcol + 1:bcol + 2], op0=ALU.add, op1=ALU.mult)
                T("dve", "tensor_scalar", ["mtmp"], ["mti"], out=mti[:], in0=mtmp[:], scalar1=1.0 / TWO_PI, scalar2=None, op0=ALU.mult)
                T("dve", "tensor_copy", ["mti"], ["mtf"], out=mtf[:], in_=mti[:])
                T("dve", "scalar_tensor_tensor", ["mtf", "mtmp"], ["mtmp"], out=mtmp[:], in0=mtf[:], scalar=-TWO_PI, in1=mtmp[:], op0=ALU.mult, op1=ALU.add)
                T("dve", "tensor_scalar", ["mtmp"], ["mtf"], out=mtf[:], in0=mtmp[:], scalar1=math.pi, scalar2=-TWO_PI, op0=ALU.is_gt, op1=ALU.mult)
                T("dve", "tensor_tensor", ["mtmp", "mtf"], ["mtmp"], out=mtmp[:], in0=mtmp[:], in1=mtf[:], op=ALU.add)
                T("dve", "tensor_scalar", ["mtmp"], ["mtf"], out=mtf[:], in0=mtmp[:], scalar1=-math.pi, scalar2=TWO_PI, op0=ALU.is_lt, op1=ALU.mult)
                T("dve", "tensor_tensor", ["mtmp", "mtf"], ["mtmp"], out=mtmp[:], in0=mtmp[:], in1=mtf[:], op=ALU.add)
                T("act", "activation", ["mtmp"], [dkey], out=dst[:, tcn * 512:(tcn + 1) * 512], in_=mtmp[:], func=ACT.Sin)
        S.barrier()
        phM.close()
        onesB = sbD("onesB", [32, 128])
        T("pool", "memset", [], ["onesB"], ap=onesB[:], constant=1.0)

        def make_pipe(pid, chunks):
            def K(k):
                return "%s_p%d" % (k, pid)

            w3s = sbD("w3s_%d" % pid, [64, 2, CC])
            w3c = sbD("w3c_%d" % pid, [64, 2, CC], BF16)
            dect = sbD("dect_%d" % pid, [32, CC, 128], BF16)
            hs = sbD("hs_%d" % pid, [32, 2, CC, 128], BF16)
            l1 = sbD("l1_%d" % pid, [32, 2 * CC])
            AplG = sbD("AplG_%d" % pid, [128, K1, 3, CC], BF16)
            AplC = sbD("AplC_%d" % pid, [128, K1, 3, CC], BF16)
            Xt = sbD("Xt_%d" % pid, [128, K1, 2, CC], BF16)
            Yt = sbD("Yt_%d" % pid, [128, 3, K1, CC], BF16)
            rinvB = sbD("rinvB_%d" % pid, [128, 2, CC])
            rbs = sbD("rbs_%d" % pid, [128, 2, CC])
            tmpG = sbD("tmpG_%d" % pid, [128, GK, 2, CC], BF16)
            Hts = [sbD("Ht0_%d" % pid, [128, K1, 2, CC], BF16), sbD("Ht1_%d" % pid, [128, K1, 2, CC], BF16)]
            Dt = sbD("Dt_%d" % pid, [2 * K1, 128, CC], BF16)
            v3 = sbD("v3_%d" % pid, [32, CC, 128], BF16)
            xg = sbD("xg_%d" % pid, [32, CC, 128], BF16)
            z1 = v3
            h2v = h2T[:, :].rearrange("p (a b) -> p a b", b=128)

            def fdft(src3, skey, mode, Apl, akey, Ht=None, hkey=None, mid_hook=None):
                for cg in range(CC // 4):
                    ps, pk = nextps()
                    for cq in range(4):
                        c = cg * 4 + cq
                        T("pe", "matmul", [skey, "F64"], [pk], out=ps[:, cq * 2 * K1:(cq + 1) * 2 * K1], lhsT=src3[:, c, :], rhs=F64[:, :], start=True, stop=True)
                    pv4 = ps[:, 0:8 * K1].rearrange("p (c r k) -> p k r c", c=4, r=2, k=K1)
                    T("act", "activation", [pk], [akey], out=Apl[:, :, 1:3, cg * 4:cg * 4 + 4], in_=pv4, func=ACT.Copy)
                    T("act", "activation", [pk], [akey], out=Apl[:, :, 0, cg * 4:cg * 4 + 4], in_=pv4[:, :, 1, :], func=ACT.Copy, scale=-1.0)
                    if cg % 2 == 1:
                        yield
                hooks = list(mid_hook) if mid_hook is not None else []
                for kg in range((K1 + GK - 1) // GK):
                    ps, pk = nextps()
                    nk = min(GK, K1 - kg * GK)
                    for kq in range(nk):
                        k1 = kg * GK + kq
                        off = kq * 2 * CC
                        T("pe", "matmul", [akey, "Gr"], [pk], out=ps[:, off:off + 2 * CC], lhsT=Gr[:, k1, :], rhs=Apl[:, k1, 1:3, :].rearrange("p r c -> p (r c)"), start=True, stop=False)
                        T("pe", "matmul", [akey, "Gi"], [pk], out=ps[:, off:off + 2 * CC], lhsT=Gi[:, k1, :], rhs=Apl[:, k1, 0:2, :].rearrange("p r c -> p (r c)"), start=False, stop=True)
                    pv3 = ps[:, 0:nk * 2 * CC].rearrange("p (k r c) -> p k r c", k=nk, r=2, c=CC)
                    if mode == "X":
                        T("act", "activation", [pk], [K("Xt")], out=Xt[:, kg * GK:kg * GK + nk, :, :], in_=pv3, func=ACT.Copy)
                    elif mode == "Hset":
                        T("act", "activation", [pk], [hkey], out=Ht[:, kg * GK:kg * GK + nk, :, :], in_=pv3, func=ACT.Copy)
                    else:
                        T("dve", "tensor_tensor", [pk, K("rbs")], [K("tmpG")], out=tmpG[:, 0:nk, :, :], in0=pv3, in1=rbs[:, :, :].unsqueeze(1).broadcast_to([128, nk, 2, CC]), op=ALU.mult)
                        T("dve", "tensor_tensor", [K("tmpG"), hkey], [hkey], out=Ht[:, kg * GK:kg * GK + nk, :, :], in0=Ht[:, kg * GK:kg * GK + nk, :, :], in1=tmpG[:, 0:nk, :, :], op=ALU.add)
                    if hooks:
                        hooks.pop(0)()
                    if kg % 2 == 1:
                        yield
                while hooks:
                    hooks.pop(0)()

            def gen_H(o, c0, Ht, hkey):
                for dr in range(2):
                    col = o * 1024 + dr * 512 + c0
                    DMA("pool", [], [K("w3s")], out=w3s[:, dr, :], in_=wf3[:, col:col + CC])
                DMA("pool", [], [K("dect")], out=dect[:], in_=dec_d[:, c0:c0 + CC, :])
                T("pool", "tensor_copy", [K("w3s")], [K("w3c")], out=w3c[:], in_=w3s[:])
                for bg in range(128 // GK):
                    ps, pk = nextps()
                    for bq in range(GK):
                        b = bg * GK + bq
                        T("pe", "matmul", ["h2T", K("w3c")], [pk], out=ps[0:32, bq * 2 * CC:(bq + 1) * 2 * CC], lhsT=h2v[:, :, b], rhs=w3c[:, :, :].rearrange("p g c -> p (g c)"), start=True, stop=True)
                    T("dve", "tensor_tensor", [pk, K("dect")], [K("hs")], out=hs[:, :, :, bg * GK:(bg + 1) * GK], in0=ps[0:32, :].rearrange("p (b g c) -> p g c b", b=GK, g=2, c=CC),
                      in1=dect[:, :, bg * GK:(bg + 1) * GK].unsqueeze(1).broadcast_to([32, 2, CC, GK]), op=ALU.mult)
                    if bg % 4 == 3:
                        yield
                def l1_piece(g, hf):
                    def f():
                        T("dve", "tensor_reduce", [K("hs")], [K("l1")], out=l1[:, g * CC + hf * (CC // 2):g * CC + (hf + 1) * (CC // 2)], in_=hs[:, g, hf * (CC // 2):(hf + 1) * (CC // 2), :], axis=AX.X, op=ALU.add, apply_absolute_value=True)
                    return f

                def l1_fin():
                    ps, pk = nextps()
                    T("pe", "matmul", [K("l1"), "onesB"], [pk], out=ps[:, 0:2 * CC], lhsT=onesB[:, :], rhs=l1[:, :], start=True, stop=True)
                    T("dve", "tensor_scalar", [pk], [K("rinvB")], out=rinvB[:, :, :].rearrange("p g c -> p (g c)"), in0=ps[:, 0:2 * CC], scalar1=EPS, scalar2=None, op0=ALU.add)
                    T("dve", "reciprocal", [K("rinvB")], [K("rinvB")], out=rinvB[:, :, :], in_=rinvB[:, :, :])
                    T("dve", "tensor_copy", [K("rinvB")], [K("rbs")], out=rbs[:, 0, :], in_=rinvB[:, 1, :])
                    T("dve", "tensor_scalar", [K("rinvB")], [K("rbs")], out=rbs[:, 1, :], in0=rinvB[:, 1, :], scalar1=-1.0, scalar2=None, op0=ALU.mult)

                l1_hooks = [l1_piece(0, 0), l1_piece(0, 1), l1_piece(1, 0), l1_piece(1, 1), l1_fin]
                yield from fdft(hs[:, 0, :, :], K("hs"), "Hset", AplG, K("AplG"), Ht, hkey, mid_hook=l1_hooks)
                T("dve", "tensor_tensor", [hkey, K("rinvB")], [hkey], out=Ht[:, :, :, :], in0=Ht[:, :, :, :],
                  in1=rinvB[:, 0:1, :].unsqueeze(1).broadcast_to([128, K1, 2, CC]), op=ALU.mult)
                yield from fdft(hs[:, 1, :, :], K("hs"), "Hconj", AplG, K("AplG"), Ht, hkey)
                fo = rcol(ROWV["fbias"][0]) + o * 512 + c0
                T("dve", "tensor_tensor", [hkey, "rowt"], [hkey], out=Ht[:, :, 0, :], in0=Ht[:, :, 0, :], in1=rowt[:, fo:fo + CC].unsqueeze(1).broadcast_to([128, K1, CC]), op=ALU.add)
                yield

            def conv(src3, skey, na, dst3, dkey, Ht, hkey):
                yield from fdft(src3, skey, "X", AplC, K("AplC"))
                tA, tB = Yt[:, 0, :, :], AplC[:, :, 0, :]
                T("dve", "tensor_tensor", [K("Xt"), hkey], [K("Yt")], out=tA, in0=Xt[:, :, 0, :], in1=Ht[:, :, 0, :], op=ALU.mult)
                T("dve", "tensor_tensor", [K("Xt"), hkey], [K("AplC")], out=tB, in0=Xt[:, :, 1, :], in1=Ht[:, :, 1, :], op=ALU.mult)
                T("dve", "tensor_tensor", [K("Yt"), K("AplC")], [K("Yt")], out=Yt[:, 1, :, :], in0=tA, in1=tB, op=ALU.subtract)
                yield
                T("dve", "tensor_tensor", [K("Xt"), hkey], [K("Yt")], out=tA, in0=Xt[:, :, 0, :], in1=Ht[:, :, 1, :], op=ALU.mult)
                T("dve", "tensor_tensor", [K("Xt"), hkey], [K("AplC")], out=tB, in0=Xt[:, :, 1, :], in1=Ht[:, :, 0, :], op=ALU.mult)
                T("dve", "tensor_tensor", [K("Yt"), K("AplC")], [K("Yt")], out=Yt[:, 2, :, :], in0=tA, in1=tB, op=ALU.add)
                T("act", "activation", [K("Yt")], [K("Yt")], out=Yt[:, 0, :, :], in_=Yt[:, 2, :, :], func=ACT.Copy, scale=-1.0)
                yield
                for cg in range(CC // 4):
                    ps, pk = nextps()
                    for cq in range(4):
                        c = cg * 4 + cq
                        T("pe", "matmul", [K("Yt"), "Wi1"], [pk], out=ps[0:2 * K1, cq * 128:(cq + 1) * 128], lhsT=Yt[:, 1:3, :, c].rearrange("p r k -> p (r k)"), rhs=Wi1[:, 0:128], start=True, stop=False)
                        T("pe", "matmul", [K("Yt"), "Wi1"], [pk], out=ps[0:2 * K1, cq * 128:(cq + 1) * 128], lhsT=Yt[:, 0:2, :, c].rearrange("p r k -> p (r k)"), rhs=Wi1[:, 128:256], start=False, stop=True)
                    T("act", "activation", [pk], [K("Dt")], out=Dt[:, :, cg * 4:cg * 4 + 4], in_=ps[0:2 * K1, :].rearrange("p (c b) -> p b c", c=4, b=128), func=ACT.Copy)
                    if cg % 2 == 1:
                        yield
                for bg in range(128 // SB):
                    ps, pk = nextps()
                    for bq in range(SB):
                        b = bg * SB + bq
                        T("pe", "matmul", [K("Dt"), "GiC"], [pk], out=ps[0:na, bq * CC:(bq + 1) * CC], lhsT=GiC[:, b, 0:na], rhs=Dt[:, b, :], start=True, stop=True)
                    T("dve", "tensor_tensor", [pk, K("xg")], [dkey], out=dst3[0:na, :, bg * SB:(bg + 1) * SB], in0=ps[0:na, :].rearrange("p (b c) -> p c b", b=SB, c=CC),
                      in1=xg[0:na, :, bg * SB:(bg + 1) * SB], op=ALU.mult)
                    if bg % 2 == 1:
                        yield

            def conv_step2(ch, o, Ht, hkey):
                c0 = ch * CC
                if o == 0:
                    DMA("sp", ["ucd"], [K("v3")], out=v3[:], in_=ucd[c0:c0 + CC, :].rearrange("c (a b) -> a c b", b=128))
                    DMA("sp", ["ucd"], [K("xg")], out=xg[:], in_=ucd[512 + c0:512 + c0 + CC, :].rearrange("c (a b) -> a c b", b=128))
                    yield from conv(v3, K("v3"), 32, v3, K("v3"), Ht, hkey)
                else:
                    DMA("sp", ["ucd"], [K("xg")], out=xg[0:16, :, :], in_=ucd[1024 + c0:1024 + c0 + CC, 0:LO].rearrange("c (a b) -> a c b", b=128))
                    yield from conv(v3, K("v3"), 16, v3, K("v3"), Ht, hkey)
                    DMA("sp", [K("v3")], [K("yhd")], out=yhd[c0:c0 + CC, :].rearrange("c (a b) -> a c b", b=128), in_=v3[0:16, :, :])

            def run():
                steps = [(ch, o) for ch in chunks for o in range(2)]

                def gstep(j):
                    ch, o = steps[j]
                    yield from gen_H(o, ch * CC, Hts[j % 2], K("Ht%d" % (j % 2)))

                def cstep(j):
                    ch, o = steps[j]
                    yield from conv_step2(ch, o, Hts[j % 2], K("Ht%d" % (j % 2)))

                for _ in gstep(0):
                    yield
                for j in range(len(steps)):
                    gc = cstep(j)
                    gg = gstep(j + 1) if j + 1 < len(steps) else iter(())
                    dc = dg = False
                    while not (dc and dg):
                        if not dg:
                            try:
                                next(gg)
                            except StopIteration:
                                dg = True
                        if not dc:
                            try:
                                next(gc)
                            except StopIteration:
                                dc = True
                        yield
            return run()

        pipes = [make_pipe(0, list(range(0, NCH, 2))), make_pipe(1, list(range(1, NCH, 2)))]
        alive = [True, True]
        while any(alive):
            for pi_ in range(2):
                if alive[pi_]:
                    try:
                        next(pipes[pi_])
                    except StopIteration:
                        alive[pi_] = False
        if STAGE == 2:
            dy = nc.dram_tensor("dbg_yh", [512, LO], BF16, kind="ExternalOutput").ap()
            DMA("sp", ["yhd"], ["dbg_yh"], out=dy, in_=yhd)
            S.final_wait("sp", ["dbg_yh"])
            S.emit()
            phD.close()
            return nc
        S.barrier()
        phD.close()

        phX = ExitStack()
        sbX = lambda name, shape, dt=F32: phX.enter_context(nc.sbuf_tensor("s_" + name, list(shape), dt))
        X1 = sbX("X1", [128, NO, D])
        xt = sbX("xt", [128, D])
        rs = sbX("rs", [128, 1])
        xt_a, rs_a = xt, rs
        xtB = sbX("xtB", [128, D])
        rsB = sbX("rsB", [128, 1])
        xnT = sbX("xnT", [128, 8, LO], BF16)
        phE = ExitStack()
        sbE = lambda name, shape, dt=F32: phE.enter_context(nc.sbuf_tensor("s_" + name, list(shape), dt))
        mixt = sbE("mixt", [128, NO, 512])
        load(mixt[:], mixd.rearrange("(n p) f -> p n f", p=128), "mixt")
        hsq = sbE("hsq", [128, NO, 8])
        T("dve", "tensor_tensor", ["mixt"], ["junkE"], out=junk[:, 0:512], in0=mixt[:, 0, :], in1=mixt[:, 0, :], op=ALU.mult)
        msq = sbE("msq", [128, 512])
        for i in range(NO):
            T("dve", "tensor_tensor", ["mixt"], ["msq"], out=msq[:], in0=mixt[:, i, :], in1=mixt[:, i, :], op=ALU.mult)
            T("dve", "reduce_sum", ["msq"], ["hsq"], out=hsq[:, i, :], in_=msq[:, :].rearrange("p (h d) -> p h d", d=64), axis=AX.X)
        T("act", "activation", ["hsq", "epst"], ["hsq"], out=hsq[:], in_=hsq[:], func=ACT.Sqrt, bias=epst[:], scale=1.0 / 64)
        T("dve", "reciprocal", ["hsq"], ["hsq"], out=hsq[:], in_=hsq[:])
        for i in range(NO):
            T("dve", "tensor_tensor", ["mixt", "hsq"], ["mixt"], out=mixt[:, i, :].rearrange("p (h d) -> p h d", d=64), in0=mixt[:, i, :].rearrange("p (h d) -> p h d", d=64),
              in1=hsq[:, i, :].unsqueeze(2).broadcast_to([128, 8, 64]), op=ALU.mult)
            T("dve", "tensor_tensor", ["mixt", "rowt"], ["mixt"], out=mixt[:, i, :], in0=mixt[:, i, :], in1=rv("g_attn"), op=ALU.mult)
            ps, pk = nextps()
            for blk in range(4):
                T("pe", "transpose", ["mixt", "ident"], [pk], out=ps[:, blk * 128:(blk + 1) * 128], in_=mixt[:, i, blk * 128:(blk + 1) * 128], identity=ident[:])
            T("act", "activation", [pk], ["xnT"], out=xnT[:, 0:4, i * 128:(i + 1) * 128], in_=ps[:, :].rearrange("p (k t) -> p k t", t=128), func=ACT.Copy)
        bones = sbE("bones", [128, 128]); load(bones[:], bones_d, "bones")
        yhT = sbE("yhT", [128, 4, LO], BF16)
        load(yhT[:], yhd.rearrange("(k p) t -> p k t", p=128), "yhT", reads=["mixt"])
        ysqs = [sbE("ysq0", [128, 512]), sbE("ysq1", [128, 512])]
        yrs = [sbE("yr0", [128, 512]), sbE("yr1", [128, 512])]
        for ct in range(4):
            for tcn in range(4):
                it_ = ct * 4 + tcn
                ysq, yqk = ysqs[it_ % 2], "ysq%d" % (it_ % 2)
                yr, yrk = yrs[it_ % 2], "yr%d" % (it_ % 2)
                sl = slice(tcn * 512, (tcn + 1) * 512)
                T("dve", "tensor_tensor", ["yhT"], [yqk], out=ysq[:], in0=yhT[:, ct, sl], in1=yhT[:, ct, sl], op=ALU.mult)
                ps, pk = nextps()
                T("pe", "matmul", [yqk, "bones"], [pk], out=ps[:, :], lhsT=bones[:, :], rhs=ysq[:, :], start=True, stop=True)
                T("act", "activation", [pk, "epst"], [yrk], out=yr[:], in_=ps[:, :], func=ACT.Sqrt, bias=epst[:], scale=1.0 / 64)
                T("dve", "reciprocal", [yrk], [yrk], out=yr[:], in_=yr[:])
                T("dve", "scalar_tensor_tensor", ["yhT", yrk, "v128"], ["xnT"], out=xnT[:, 4 + ct, sl], in0=yhT[:, ct, sl], scalar=pv("g_hy", ct), in1=yr[:], op0=ALU.mult, op1=ALU.mult)
        wstE = sbE("wstE", [128, 8, 256])
        wbig = sbE("wbig", [128, 8, D], BF16)

        def load_w8(src2d):
            for j in range(4):
                load(wstE[:], src2d[:, j * 256:(j + 1) * 256].rearrange("(k p) n -> p k n", p=128), "wstE")
                T("act", "activation", ["wstE"], ["wbig"], out=wbig[:, :, j * 256:(j + 1) * 256], in_=wstE[:], func=ACT.Copy)

        load_w8(w_out)
        load(X1[:], xb[0:LO, :].rearrange("(n p) f -> p n f", p=128), "X1", reads=["wstE"])
        for i in range(NO):
            for nh in range(2):
                ps, pk = nextps()
                for k in range(8):
                    T("pe", "matmul", ["xnT", "wbig"], [pk], out=ps[:, :], lhsT=xnT[:, k, i * 128:(i + 1) * 128], rhs=wbig[:, k, nh * 512:(nh + 1) * 512], start=(k == 0), stop=(k == 7))
                T("dve", "tensor_tensor", [pk, "X1"], ["X1"], out=X1[:, i, nh * 512:(nh + 1) * 512], in0=X1[:, i, nh * 512:(nh + 1) * 512], in1=ps[:, :], op=ALU.add)
        if STAGE == 3:
            dx = nc.dram_tensor("dbg_x1", [128, NO, D], F32, kind="ExternalOutput").ap()
            DMA("sp", ["X1"], ["dbg_x1"], out=dx, in_=X1[:])
            S.final_wait("sp", ["dbg_x1"])
            S.emit()
            phE.close()
            phX.close()
            return nc
        S.barrier()
        phE.close()

        phF = ExitStack()
        sbF = lambda name, shape, dt=F32: phF.enter_context(nc.sbuf_tensor("s_" + name, list(shape), dt))
        Wt = sbF("Wt", [128, NO, 32])
        wrt = sbF("wrt", [128, 8, 36]); load(wrt[:], wr.rearrange("(k p) n -> p k n", p=128), "wrt")
        xnF = sbF("xnF", [128, 8, 128])
        lg = sbF("lg", [128, 36])
        sm = sbF("sm", [128, 16])
        gm = sbF("gm", [128, 4])
        e1 = sbF("e1", [128, 32])
        e2 = sbF("e2", [128, 32])
        s1 = sbF("s1", [128, 32])
        s2 = sbF("s2", [128, 32])
        BIG = 1.0e9

        def norm_T(i, gname, f32dst=None, xkey="X1", tkey="xnT"):
            xt, rs = (xt_a, rs_a) if i % 2 == 0 else (xtB, rsB)
            xk_, rk_ = "xt%d" % (i % 2), "rs%d" % (i % 2)
            T("act", "activation", [xkey], [xk_], out=xt[:], in_=X1[:, i, :], func=ACT.Copy)
            rstd_of(xt[:], D, xk_, rs[:], rk_)
            T("act", "activation", [xk_, rk_], [xk_], out=xt[:], in_=xt[:], func=ACT.Copy, scale=rs[:, 0:1])
            for g in range(2):
                ps, pk = nextps()
                for kq in range(4):
                    k = g * 4 + kq
                    T("pe", "transpose", [xk_, "ident"], [pk], out=ps[:, kq * 128:(kq + 1) * 128], in_=xt[:, k * 128:(k + 1) * 128], identity=ident[:])
                for kq in range(4):
                    k = g * 4 + kq
                    T("dve", "tensor_scalar", [pk, "v128"], [tkey], out=xnT[:, k, i * 128:(i + 1) * 128], in0=ps[:, kq * 128:(kq + 1) * 128], scalar1=pv(gname, k), scalar2=None, op0=ALU.mult)
                    if f32dst is not None:
                        T("dve", "tensor_scalar", [pk, "v128"], ["xnF"], out=f32dst[:, k, :], in0=ps[:, kq * 128:(kq + 1) * 128], scalar1=pv(gname, k), scalar2=None, op0=ALU.mult)

        def route_tile(i):
            norm_T(i, "g_moe", xnF, "X1:%d" % i, "xnT:%d" % i)
            ps, pk = nextps()
            for k in range(8):
                T("pe", "matmul", ["xnF", "wrt"], [pk], out=ps[:, 0:36], lhsT=xnF[:, k, :], rhs=wrt[:, k, :], start=(k == 0), stop=(k == 7))
            T("dve", "tensor_tensor", [pk, "rowt"], ["lg"], out=lg[:], in0=ps[:, 0:36], in1=rv("br"), op=ALU.add)
            T("dve", "reduce_max", ["lg"], ["sm"], out=sm[:, 0:1], in_=lg[:, 0:4], axis=AX.X)
            T("dve", "tensor_scalar", ["lg", "sm"], ["gm"], out=gm[:], in0=lg[:, 0:4], scalar1=sm[:, 0:1], scalar2=None, op0=ALU.subtract)
            T("act", "activation", ["gm"], ["e1"], out=e1[:, 0:4], in_=gm[:], func=ACT.Exp)
            T("dve", "reduce_sum", ["e1"], ["sm"], out=sm[:, 1:2], in_=e1[:, 0:4], axis=AX.X)
            T("dve", "reciprocal", ["sm"], ["sm"], out=sm[:, 2:3], in_=sm[:, 1:2])
            T("dve", "tensor_scalar", ["gm"], ["gm"], out=gm[:], in0=gm[:], scalar1=0.0, scalar2=BIG, op0=ALU.is_lt, op1=ALU.mult)
            T("dve", "tensor_tensor", ["lg", "gm"], ["e1"], out=e1[:, :].rearrange("p (g e) -> p g e", e=8), in0=lg[:, 4:36].rearrange("p (g e) -> p g e", e=8),
              in1=gm[:, :].unsqueeze(2).broadcast_to([128, 4, 8]), op=ALU.subtract)
            T("dve", "reduce_max", ["e1"], ["sm"], out=sm[:, 3:4], in_=e1[:], axis=AX.X)
            T("dve", "tensor_scalar", ["e1", "sm"], ["s1"], out=s1[:], in0=e1[:], scalar1=sm[:, 3:4], scalar2=None, op0=ALU.is_ge)
            T("dve", "scalar_tensor_tensor", ["s1", "e1"], ["e2"], out=e2[:], in0=s1[:], scalar=-BIG, in1=e1[:], op0=ALU.mult, op1=ALU.add)
            T("dve", "reduce_max", ["e2"], ["sm"], out=sm[:, 4:5], in_=e2[:], axis=AX.X)
            T("dve", "tensor_scalar", ["e2", "sm"], ["s2"], out=s2[:], in0=e2[:], scalar1=sm[:, 4:5], scalar2=None, op0=ALU.is_ge)
            T("dve", "tensor_tensor", ["sm"], ["sm"], out=sm[:, 5:6], in0=sm[:, 4:5], in1=sm[:, 3:4], op=ALU.subtract)
            T("act", "activation", ["sm"], ["sm"], out=sm[:, 6:7], in_=sm[:, 5:6], func=ACT.Exp)
            T("dve", "tensor_scalar", ["sm"], ["sm"], out=sm[:, 6:7], in0=sm[:, 6:7], scalar1=1.0, scalar2=None, op0=ALU.add)
            T("dve", "reciprocal", ["sm"], ["sm"], out=sm[:, 7:8], in_=sm[:, 6:7])
            T("dve", "tensor_tensor", ["sm"], ["sm"], out=sm[:, 8:9], in0=sm[:, 7:8], in1=sm[:, 2:3], op=ALU.mult)
            T("dve", "tensor_tensor", ["sm"], ["sm"], out=sm[:, 9:10], in0=sm[:, 2:3], in1=sm[:, 8:9], op=ALU.subtract)
            T("dve", "tensor_scalar", ["s1", "sm"], ["s1"], out=s1[:], in0=s1[:], scalar1=sm[:, 8:9], scalar2=None, op0=ALU.mult)
            T("dve", "scalar_tensor_tensor", ["s2", "sm", "s1"], ["Wt:%d" % i], out=Wt[:, i, :], in0=s2[:], scalar=sm[:, 9:10], in1=s1[:], op0=ALU.mult, op1=ALU.add)

        wfl = sbF("wfl", [128, 4096])
        wgs = wfl[:, :].rearrange("p (k n) -> p k n", n=512)
        wds = wfl[:, :].rearrange("p (k n) -> p k n", n=D)
        wgb = sbF("wgb", [128, 8, 512], BF16)
        wub = sbF("wub", [128, 8, 512], BF16)
        wdb = sbF("wdb", [128, 4, D], BF16)
        hmTs = [sbF("hmT0", [128, 4, LO], BF16), sbF("hmT1", [128, 4, LO], BF16)]
        sg = sbF("sg", [128, 512])

        def experts_gen():
          for ex in range(32):
            hmT, hmk = hmTs[ex % 2], "hmT%d" % (ex % 2)
            load(wgs, wgate[ex].rearrange("(k p) n -> p k n", p=128), "wgs")
            T("act", "activation", ["wgs"], ["wgb"], out=wgb[:], in_=wgs, func=ACT.Copy)
            load(wgs, wup[ex].rearrange("(k p) n -> p k n", p=128), "wgs")
            T("act", "activation", ["wgs"], ["wub"], out=wub[:], in_=wgs, func=ACT.Copy)
            load(wds, wdown[ex].rearrange("(k p) n -> p k n", p=128), "wgs")
            T("act", "activation", ["wgs"], ["wdb"], out=wdb[:], in_=wds, func=ACT.Copy)
            for tcn in range(4):
                xk4 = ["xnT:%d" % j for j in range(tcn * 4, tcn * 4 + 4)]
                for ft in range(4):
                    yield (4 * tcn + 4) if ex == 0 else NO
                    sl = slice(tcn * 512, (tcn + 1) * 512)
                    psg, pkg = nextps()
                    psu, pku = nextps()
                    for k in range(8):
                        T("pe", "matmul", xk4 + ["wgb"], [pkg], out=psg[:, :], lhsT=wgb[:, k, ft * 128:(ft + 1) * 128], rhs=xnT[:, k, sl], start=(k == 0), stop=(k == 7))
                    for k in range(8):
                        T("pe", "matmul", xk4 + ["wub"], [pku], out=psu[:, :], lhsT=wub[:, k, ft * 128:(ft + 1) * 128], rhs=xnT[:, k, sl], start=(k == 0), stop=(k == 7))
                    T("act", "activation", [pkg], ["sg"], out=sg[:], in_=psg[:, :], func=ACT.Silu)
                    T("dve", "tensor_tensor", ["sg", pku], [hmk], out=hmT[:, ft, sl], in0=sg[:], in1=psu[:, :], op=ALU.mult)
            for i in range(NO):
                yield NO
                for nh in range(2):
                    ps, pk = nextps()
                    for ft in range(4):
                        T("pe", "matmul", [hmk, "wdb"], [pk], out=ps[:, :], lhsT=hmT[:, ft, i * 128:(i + 1) * 128], rhs=wdb[:, ft, nh * 512:(nh + 1) * 512], start=(ft == 0), stop=(ft == 3))
                    T("dve", "scalar_tensor_tensor", [pk, "Wt:%d" % i, "X1:%d" % i], ["X1:%d" % i], out=X1[:, i, nh * 512:(nh + 1) * 512], in0=ps[:, :], scalar=Wt[:, i, ex:ex + 1],
                      in1=X1[:, i, nh * 512:(nh + 1) * 512], op0=ALU.mult, op1=ALU.add)

        routed = 0
        cnt_y = 0
        for need in experts_gen():
            while routed < need:
                route_tile(routed)
                routed += 1
            cnt_y += 1
            if routed < NO and cnt_y % 2 == 0:
                route_tile(routed)
                routed += 1
        S.barrier()
        phF.close()

        phG = ExitStack()
        sbG = lambda name, shape, dt=F32: phG.enter_context(nc.sbuf_tensor("s_" + name, list(shape), dt))
        wstE = sbG("wstG", [128, 8, 256])
        wbig = sbG("wbigG", [128, 8, D], BF16)
        load_w8(wpg)
        wps = sbG("wps", [128, 2, D])
        wpb = sbG("wpb", [128, 2, D], BF16)
        load(wps[:], wple.rearrange("(k p) n -> p k n", p=128), "wps")
        T("act", "activation", ["wps"], ["wpb"], out=wpb[:], in_=wps[:], func=ACT.Copy)
        pt_ = sbG("pt", [128, NO, 256])
        load(pt_[:], pb.rearrange("(n p) f -> p n f", p=128), "pt")
        pT = sbG("pT", [128, 2, LO], BF16)
        gt = sbG("gt", [128, 512])
        ot = sbG("ot", [128, D])
        for i in range(NO):
            norm_T(i, "g_ple", None, "X1", "xnT:%d" % i)
            ps, pk = nextps()
            for kq in range(2):
                T("pe", "transpose", ["pt", "ident"], [pk], out=ps[:, kq * 128:(kq + 1) * 128], in_=pt_[:, i, kq * 128:(kq + 1) * 128], identity=ident[:])
            T("act", "activation", [pk], ["pT"], out=pT[:, :, i * 128:(i + 1) * 128], in_=ps[:, 0:256].rearrange("p (k t) -> p k t", t=128), func=ACT.Copy)
        rowG = sbG("rowG", [128, 2048])
        load(rowG[:], rowv[:, ROWV["g_final"][0]:ROWV["g_final"][0] + 2048].partition_broadcast(128), "rowG")
        o_bpg = 1024
        for i in range(NO):
            for nh in range(2):
                sl = slice(nh * 512, (nh + 1) * 512)
                ps, pk = nextps()
                for k in range(8):
                    T("pe", "matmul", ["xnT:%d" % i, "wbig"], [pk], out=ps[:, :], lhsT=xnT[:, k, i * 128:(i + 1) * 128], rhs=wbig[:, k, sl], start=(k == 0), stop=(k == 7))
                T("dve", "tensor_tensor", [pk, "rowG"], ["gt"], out=gt[:], in0=ps[:, :], in1=rowG[:, o_bpg + nh * 512:o_bpg + (nh + 1) * 512], op=ALU.add)
                T("act", "activation", ["gt"], ["gt"], out=gt[:], in_=gt[:], func=ACT.Sigmoid)
                ps2, pk2 = nextps()
                for k in range(2):
                    T("pe", "matmul", ["pT", "wpb"], [pk2], out=ps2[:, :], lhsT=pT[:, k, i * 128:(i + 1) * 128], rhs=wpb[:, k, sl], start=(k == 0), stop=(k == 1))
                T("dve", "tensor_tensor", [pk2, "gt"], ["gt"], out=gt[:], in0=gt[:], in1=ps2[:, :], op=ALU.mult)
                T("dve", "tensor_tensor", ["gt", "X1"], ["X1"], out=X1[:, i, sl], in0=X1[:, i, sl], in1=gt[:], op=ALU.add)
            rstd_of(X1[:, i, :], D, "X1", rs[:], "rs0")
            T("act", "activation", ["X1", "rs0"], ["ot"], out=ot[:], in_=X1[:, i, :], func=ACT.Copy, scale=rs[:, 0:1])
            T("dve", "tensor_tensor", ["ot", "rowG"], ["ot"], out=ot[:], in0=ot[:], in1=rowG[:, 0:1024], op=ALU.mult)
            DMA("sp", ["ot"], ["out"], out=out_d[i * 128:(i + 1) * 128, :], in_=ot[:])
        S.final_wait("sp", ["out"])
        S.barrier()
        S.emit()
        phG.close()
        phX.close()
    return nc


def prep_inputs(inputs):
    c = host_consts()
    g = lambda k: np.asarray(inputs[k], dtype=np.float32)
    x = g("x"); p = g("p")[0]
    w_in = g("w_in")[0]
    conv_w = g("conv_w")[0]; conv_b = g("conv_b")[0]
    wf3 = g("w_f3")[0]
    in_maps = []
    for core in range(8):
        b, half = core // 2, core % 2
        rev = (half == 1)
        xb = x[b][::-1] if rev else x[b]
        pb = p[b][::-1][:LO] if rev else p[b][:LO]
        cw = conv_w[::-1] if rev else conv_w
        if rev:
            w3 = wf3.reshape(64, 2, 2, 512)[:, :, ::-1, :].reshape(64, 2048)
        else:
            w3 = wf3
        v128 = np.zeros((128, V128_N), np.float32)
        v128[:, 0:8] = g("g_mix")[0].reshape(8, 128).T
        v128[:, 8:16] = g("g_moe")[0].reshape(8, 128).T
        v128[:, 16:24] = g("g_ple")[0].reshape(8, 128).T
        v128[:, 24:60] = cw.reshape(3, 12, 128).transpose(2, 1, 0).reshape(128, 36)
        v128[:, 60:72] = conv_b.reshape(12, 128).T
        v128[:, 72:76] = g("g_hyena_out")[0].reshape(4, 128).T
        v64 = np.stack([g("b_f1")[0], g("freq1")[0], g("b_f2")[0], g("freq2")[0]], axis=1)
        rowv = np.concatenate([g("q_gain")[0], g("k_gain")[0], g("g_attn_out")[0], g("g_final"), g("b_ple_gate")[0],
                               g("b_group")[0], g("b_router")[0], g("filt_bias")[0].reshape(-1)])[None, :]
        m = {
            "xb": xb, "pb": pb, "w_in": w_in, "w_out": g("w_out")[0], "wgate": g("w_gate")[0], "wup": g("w_up")[0],
            "wdown": g("w_down")[0], "wpg": g("w_ple_gate")[0], "wple": g("w_ple")[0], "wf1": g("w_f1")[0], "wf2": g("w_f2")[0],
            "wf3": w3, "wr": np.concatenate([g("w_group")[0], g("w_router")[0]], axis=1), "v128": v128, "v64": v64, "rowv": rowv,
            "ropeC": c["ropeC"][::-1] if rev else c["ropeC"], "ropeS": c["ropeS"][::-1] if rev else c["ropeS"],
        }
        for k in ("ident", "bones", "ones32", "zT", "dec", "F64", "Gr", "Gi", "Wi1", "Wi2", "GiC"):
            m[k] = c[k]
        in_maps.append({k: np.ascontiguousarray(v) for k, v in m.items()})
    return in_maps


_NC = None


def kernel(**inputs):
    global _NC
    in_maps = prep_inputs(inputs)
    if _NC is None:
        _NC = build_nc()
    res = run_bass_kernel_spmd(_NC, in_maps, core_ids=list(range(8)))
    out = np.zeros((4, L, D), np.float32)
    for core in range(8):
        b, half = core // 2, core % 2
        o = np.asarray(res.results[core]["out"], dtype=np.float32)
        if half == 0:
            out[b, :LO] = o
        else:
            out[b, LO:] = o[::-1]
    return out
```
